# Optimizing a Trainium2 kernel written in Bass

```python
import math
import jax, jax.numpy as jnp
from jax import lax
import numpy as np

D_MODEL = 1024
BATCH = 16
SEQ = 2048
DEPTH = 2

MIX_WIDTH = D_MODEL
RET_HEADS = 4
RET_V_DIM = 128
RET_QK_DIM = RET_V_DIM // 2
RET_CHUNK = 128
RET_THETA = 10000.0
MOBA_HEADS = 8
MOBA_HEAD_DIM = 64
MOBA_BLOCK = 256
MOBA_TOP_K = 3
MOBA_Q_BLOCK = 64
ROPE_THETA = 500000.0
ROPE_FRACTION = 4
D_FF = 4 * D_MODEL
NORM_EPS = 1e-6
GN_EPS = 1e-5
NEG_INF = -1e30

RET_Q_W = RET_HEADS * RET_QK_DIM
RET_V_W = RET_HEADS * RET_V_DIM
MOBA_W = MOBA_HEADS * MOBA_HEAD_DIM
IN_WIDTH = 2 * RET_Q_W + 2 * RET_V_W + 3 * MOBA_W

kernel_name = "hymba_retnet_moba_sandwich"


def rmsnorm(x, g):
    x32 = x.astype(jnp.float32)
    y = x32 * lax.rsqrt(jnp.mean(x32 * x32, axis=-1, keepdims=True) + NORM_EPS)
    return (y * g.astype(jnp.float32)).astype(x.dtype)


def apply_rotary(x, rot_dim, theta):
    S = x.shape[2]
    half = rot_dim // 2
    pos = jnp.arange(S, dtype=jnp.float32)
    freqs = theta ** (-jnp.arange(0, rot_dim, 2, dtype=jnp.float32) / rot_dim)
    ang = pos[:, None] * freqs[None, :]
    cos = jnp.cos(ang).astype(x.dtype)
    sin = jnp.sin(ang).astype(x.dtype)
    x1 = x[..., :half]
    x2 = x[..., half:rot_dim]
    return jnp.concatenate([x1 * cos - x2 * sin, x1 * sin + x2 * cos, x[..., rot_dim:]], axis=-1)


def retention(q, k, v, g):
    B, H, S, dk = q.shape
    dv = v.shape[-1]
    C = RET_CHUNK
    N = S // C
    dt = q.dtype
    k = k * jnp.asarray(dk ** -0.5, dt)
    log_gamma = jnp.log(1.0 - 2.0 ** (-5.0 - jnp.arange(H, dtype=jnp.float32)))
    idx = jnp.arange(C, dtype=jnp.float32)
    diff = idx[:, None] - idx[None, :]
    decay_intra = jnp.where(diff >= 0,
                            jnp.exp(log_gamma[:, None, None] * jnp.maximum(diff, 0.0)),
                            0.0).astype(dt)
    q_decay = jnp.exp(log_gamma[:, None] * (idx + 1.0)).astype(dt)
    k_decay = jnp.exp(log_gamma[:, None] * (C - 1.0 - idx)).astype(dt)
    chunk_decay = jnp.exp(log_gamma * C).astype(dt)

    qc = q.reshape(B, H, N, C, dk)
    kc = k.reshape(B, H, N, C, dk)
    vc = v.reshape(B, H, N, C, dv)
    scores = jnp.einsum('bhncd,bhnmd->bhncm', qc, kc) * decay_intra[None, :, None]
    intra = jnp.einsum('bhncm,bhnme->bhnce', scores, vc)
    kv = jnp.einsum('bhncd,bhnce->nbhde', kc * k_decay[None, :, None, :, None], vc)

    def step(state, kv_n):
        return state * chunk_decay[None, :, None, None] + kv_n, state

    _, prev = lax.scan(step, jnp.zeros((B, H, dk, dv), dt), kv)
    cross = jnp.einsum('bhncd,nbhde->bhnce', qc * q_decay[None, :, None, :, None], prev)
    o = (intra + cross).reshape(B, H, S, dv)
    o32 = o.astype(jnp.float32)
    mu = jnp.mean(o32, axis=-1, keepdims=True)
    var = jnp.mean(jnp.square(o32 - mu), axis=-1, keepdims=True)
    o = ((o32 - mu) * lax.rsqrt(var + GN_EPS)).astype(dt)
    o = o.transpose(0, 2, 1, 3).reshape(B, S, H * dv)
    return jax.nn.silu(g) * o


def moba_attention(q, k, v):
    B, H, S, dh = q.shape
    BS = MOBA_BLOCK
    QB = MOBA_Q_BLOCK
    nkb = -(-S // BS)
    pad = nkb * BS - S
    kp = jnp.pad(k, ((0, 0), (0, 0), (0, pad), (0, 0)))
    vp = jnp.pad(v, ((0, 0), (0, 0), (0, pad), (0, 0)))
    k_blocks = kp.reshape(B, H, nkb, BS, dh)
    v_blocks = vp.reshape(B, H, nkb, BS, dh)
    k_mean = jnp.mean(k_blocks.astype(jnp.float32), axis=3)
    top_k = min(MOBA_TOP_K, nkb)
    nqb = S // QB
    q_blocks = q.reshape(B, H, nqb, QB, dh).transpose(2, 0, 1, 3, 4)
    scale = dh ** -0.5
    b_idx = jnp.arange(B)[:, None, None, None]
    h_idx = jnp.arange(H)[None, :, None, None]
    blk_ids = jnp.arange(nkb)

    def one_block(args):
        qb, i = args
        own = (i * QB) // BS
        q_pos = i * QB + jnp.arange(QB)
        gate = jnp.einsum('bhqd,bhnd->bhqn', qb.astype(jnp.float32), k_mean)
        gate = jnp.where((blk_ids < own)[None, None, None, :], gate, NEG_INF)
        _, sel = lax.top_k(gate, top_k)
        sel_valid = sel < own
        k_sel = k_blocks[b_idx, h_idx, sel]
        v_sel = v_blocks[b_idx, h_idx, sel]
        s_sel = jnp.einsum('bhqd,bhqkcd->bhqkc', qb, k_sel).astype(jnp.float32) * scale
        s_sel = jnp.where(sel_valid[..., None], s_sel, NEG_INF).reshape(B, H, QB, top_k * BS)
        k_own = lax.dynamic_index_in_dim(k_blocks, own, axis=2, keepdims=False)
        v_own = lax.dynamic_index_in_dim(v_blocks, own, axis=2, keepdims=False)
        s_own = jnp.einsum('bhqd,bhcd->bhqc', qb, k_own).astype(jnp.float32) * scale
        key_pos = own * BS + jnp.arange(BS)
        s_own = jnp.where((key_pos[None, :] <= q_pos[:, None])[None, None], s_own, NEG_INF)
        p = jax.nn.softmax(jnp.concatenate([s_sel, s_own], axis=-1), axis=-1).astype(qb.dtype)
        p_sel = p[..., :top_k * BS].reshape(B, H, QB, top_k, BS)
        p_own = p[..., top_k * BS:]
        return (jnp.einsum('bhqkc,bhqkcd->bhqd', p_sel, v_sel)
                + jnp.einsum('bhqc,bhcd->bhqd', p_own, v_own))

    out = lax.map(one_block, (q_blocks, jnp.arange(nqb)))
    return out.transpose(1, 2, 0, 3, 4).reshape(B, H, S, dh)


def hybrid_mixer(h, w_in, w_out):
    B, S, _ = h.shape
    p = h @ w_in
    splits = np.cumsum([RET_Q_W, RET_Q_W, RET_V_W, RET_V_W, MOBA_W, MOBA_W])
    rq, rk, rv, rg, mq, mk, mv = jnp.split(p, splits, axis=-1)

    def heads(t, n):
        return t.reshape(B, S, n, -1).transpose(0, 2, 1, 3)

    rq = apply_rotary(heads(rq, RET_HEADS), RET_QK_DIM, RET_THETA)
    rk = apply_rotary(heads(rk, RET_HEADS), RET_QK_DIM, RET_THETA)
    ret = retention(rq, rk, heads(rv, RET_HEADS), rg)
    rot = MOBA_HEAD_DIM // ROPE_FRACTION
    mq = apply_rotary(heads(mq, MOBA_HEADS), rot, ROPE_THETA)
    mk = apply_rotary(heads(mk, MOBA_HEADS), rot, ROPE_THETA)
    mob = moba_attention(mq, mk, heads(mv, MOBA_HEADS))
    mob = mob.transpose(0, 2, 1, 3).reshape(B, S, MOBA_W)
    return jnp.concatenate([ret, mob], axis=-1) @ w_out


def setup_inputs(seed: int = 0) -> dict:
    key = jax.random.key(seed)
    ks = jax.random.split(key, 10)
    f32 = jnp.float32
    x = jax.random.normal(ks[0], (BATCH, SEQ, D_MODEL), f32)
    w_in = jax.random.normal(ks[1], (DEPTH, D_MODEL, IN_WIDTH), f32) * D_MODEL ** -0.5
    w_out = jax.random.normal(ks[2], (DEPTH, MIX_WIDTH, D_MODEL), f32) * MIX_WIDTH ** -0.5
    w_up = jax.random.normal(ks[3], (DEPTH, D_MODEL, D_FF), f32) * D_MODEL ** -0.5
    w_down = jax.random.normal(ks[4], (DEPTH, D_FF, D_MODEL), f32) * D_FF ** -0.5
    g_mix_pre = 1.0 + 0.05 * jax.random.normal(ks[5], (DEPTH, D_MODEL), f32)
    g_mix_post = 1.0 + 0.05 * jax.random.normal(ks[6], (DEPTH, D_MODEL), f32)
    g_mlp_pre = 1.0 + 0.05 * jax.random.normal(ks[7], (DEPTH, D_MODEL), f32)
    g_mlp_post = 1.0 + 0.05 * jax.random.normal(ks[8], (DEPTH, D_MODEL), f32)
    return {"x": x, "w_in": w_in, "w_out": w_out, "w_up": w_up, "w_down": w_down,
            "g_mix_pre": g_mix_pre, "g_mix_post": g_mix_post,
            "g_mlp_pre": g_mlp_pre, "g_mlp_post": g_mlp_post}


def reference(x, w_in, w_out, w_up, w_down, g_mix_pre, g_mix_post, g_mlp_pre, g_mlp_post):
    for l in range(DEPTH):
        h = rmsnorm(x, g_mix_pre[l])
        x = x + rmsnorm(hybrid_mixer(h, w_in[l], w_out[l]), g_mix_post[l])
        h = rmsnorm(x, g_mlp_pre[l])
        y = jnp.square(jax.nn.relu(h @ w_up[l])) @ w_down[l]
        x = x + rmsnorm(y, g_mlp_post[l])
    return x
```

```python
import math
from contextlib import ExitStack
import numpy as np
import ml_dtypes
import concourse.bass as bass
import concourse.mybir as mybir
from concourse.bass_utils import run_bass_kernel_spmd

F32 = mybir.dt.float32
BF16 = mybir.dt.bfloat16
ALU = mybir.AluOpType
AF = mybir.ActivationFunctionType
AX = mybir.AxisListType

PE, ACT, DVE, POOL, SP = "pe", "act", "dve", "pool", "sp"
ENGS = [PE, ACT, DVE, POOL, SP]
SAME_ENGINE_SYNC = {PE: False, ACT: True, DVE: True, POOL: True, SP: False}
N_DMA_SEMS = 24


class Region:
    __slots__ = ("name", "last_w", "reads")

    def __init__(self, name):
        self.name = name
        self.last_w = None
        self.reads = []


class Op:
    __slots__ = ("eng", "idx", "fn", "deps", "is_dma", "dma_slot", "dma_val", "signal", "semval")

    def __init__(self, eng, idx, fn, is_dma):
        self.eng = eng
        self.idx = idx
        self.fn = fn
        self.deps = []
        self.is_dma = is_dma
        self.dma_slot = None
        self.dma_val = None
        self.signal = False
        self.semval = None


class Prog:
    def __init__(self, nc):
        self.nc = nc
        self.ops = {e: [] for e in ENGS}
        self.known = {e: {} for e in ENGS}
        self.n_dma = 0
        self.dma_last = [None] * N_DMA_SEMS

    def _add(self, eng, fn, r, w, is_dma):
        op = Op(eng, self._next_idx(eng), fn, is_dma)
        deps = []
        for reg in r:
            if reg.last_w is not None:
                deps.append(reg.last_w)
        for reg in w:
            if reg.last_w is not None:
                deps.append(reg.last_w)
            deps.extend(reg.reads)
        if is_dma:
            slot = self.n_dma % N_DMA_SEMS
            op.dma_slot = slot
            op.dma_val = 16 * (self.n_dma // N_DMA_SEMS + 1)
            if self.dma_last[slot] is not None:
                deps.append(self.dma_last[slot])
            self.dma_last[slot] = op
            self.n_dma += 1
            op.signal = True
        kn = self.known[eng]
        for d in deps:
            if d.is_dma:
                key = ("dma", d.dma_slot)
                val = d.dma_val
            else:
                if d.eng == eng and not SAME_ENGINE_SYNC[eng]:
                    continue
                key = d.eng
                val = d.idx
            if kn.get(key, -1) >= val:
                continue
            kn[key] = val
            d.signal = True
            op.deps.append(d)
        for reg in r:
            reg.reads.append(op)
        for reg in w:
            reg.last_w = op
            reg.reads = []
        self.ops[eng].append(op)
        return op

    def _next_idx(self, eng):
        return len(self.ops[eng])

    def op(self, eng, fn, r=(), w=()):
        return self._add(eng, fn, r, w, False)

    def dma(self, eng, fn, r=(), w=()):
        return self._add(eng, fn, r, w, True)

    def emit(self, final_regions=()):
        nc = self.nc
        self._add(SP, None, list(final_regions), [], False)
        with ExitStack() as st:
            sems = {e: st.enter_context(nc.semaphore("sem_" + e)) for e in ENGS}
            dsems = [st.enter_context(nc.semaphore("sem_dma%d" % i)) for i in range(N_DMA_SEMS)]
            for e in ENGS:
                c = 0
                for op in self.ops[e]:
                    if op.is_dma:
                        continue
                    if op.signal:
                        c += 1
                        op.semval = c
            block = st.enter_context(nc.Block())

            def run(e):
                def body(eng):
                    for op in self.ops[e]:
                        for d in op.deps:
                            if d.is_dma:
                                eng.wait_ge(dsems[d.dma_slot], d.dma_val)
                            else:
                                eng.wait_ge(sems[d.eng], d.semval)
                        if op.fn is None:
                            continue
                        ins = op.fn(eng)
                        if op.is_dma:
                            ins.then_inc(dsems[op.dma_slot], 16)
                        elif op.signal:
                            ins.then_inc(sems[e], 1)
                return body

            block.tensor(run(PE))
            block.scalar(run(ACT))
            block.vector(run(DVE))
            block.gpsimd(run(POOL))
            block.sync(run(SP))


class Prog2(Prog):
    def __init__(self, nc, stack):
        super().__init__(nc)
        self.nidx = {e: 0 for e in ENGS}
        self.semcnt = {e: 0 for e in ENGS}
        self.last_compute = {e: None for e in ENGS}
        self.sems = {e: stack.enter_context(nc.semaphore("sem_" + e)) for e in ENGS}
        self.dsems = [stack.enter_context(nc.semaphore("sem_dma%d" % i)) for i in range(N_DMA_SEMS)]

    def _next_idx(self, eng):
        i = self.nidx[eng]
        self.nidx[eng] += 1
        return i

    def _add(self, eng, fn, r, w, is_dma):
        op = super()._add(eng, fn, r, w, is_dma)
        if not is_dma and fn is not None:
            self.last_compute[eng] = op
        return op

    def barrier(self):
        lasts = dict(self.last_compute)
        dl = list(self.dma_last)
        for e in ENGS:
            op = Op(e, self.nidx[e], None, False)
            self.nidx[e] += 1
            kn = self.known[e]
            for e2 in ENGS:
                d = lasts[e2]
                if d is None:
                    continue
                if kn.get(e2, -1) >= d.idx:
                    continue
                kn[e2] = d.idx
                d.signal = True
                op.deps.append(d)
            for d in dl:
                if d is None:
                    continue
                key = ("dma", d.dma_slot)
                if kn.get(key, -1) >= d.dma_val:
                    continue
                kn[key] = d.dma_val
                op.deps.append(d)
            self.ops[e].append(op)

    def emit_block(self):
        nc = self.nc
        for e in ENGS:
            for op in self.ops[e]:
                if op.is_dma:
                    continue
                if op.signal and op.semval is None:
                    self.semcnt[e] += 1
                    op.semval = self.semcnt[e]
        sems, dsems = self.sems, self.dsems
        ops = {e: self.ops[e] for e in ENGS}
        self.ops = {e: [] for e in ENGS}
        with nc.Block() as block:
            def run(e):
                def body(eng):
                    for op in ops[e]:
                        for d in op.deps:
                            if d.is_dma:
                                eng.wait_ge(dsems[d.dma_slot], d.dma_val)
                            else:
                                assert d.semval is not None
                                eng.wait_ge(sems[d.eng], d.semval)
                        if op.fn is None:
                            continue
                        ins = op.fn(eng)
                        if op.is_dma:
                            ins.then_inc(dsems[op.dma_slot], 16)
                        elif op.signal:
                            ins.then_inc(sems[e], 1)
                return body

            block.tensor(run(PE))
            block.scalar(run(ACT))
            block.vector(run(DVE))
            block.gpsimd(run(POOL))
            block.sync(run(SP))


NCORES = 8
SEQ = 2048
DM = 1024
NSEQ = 2
TOK = NSEQ * SEQ
NTILE = TOK // 128
DEPTH = 2
NORM_EPS = 1e-6
GN_EPS = 1e-5
BIGM = 32768.0

C_ROT = 0
C_DEC = C_ROT + 16 * 160
C_QDEC = C_DEC + 512
C_KDEC = C_QDEC + 256
C_GB = C_KDEC + 4
C_END = C_GB + 256
B_ID = 0
B_TRI = 128
B_IDB = 256
B_END = 384


def host_constants():
    cF = np.zeros((128, C_END), np.float32)
    p = np.arange(128)
    fr = (np.float32(10000.0) ** (-np.arange(0, 64, 2, dtype=np.float32) / np.float32(64))).astype(np.float32)
    fm = (np.float32(500000.0) ** (-np.arange(0, 16, 2, dtype=np.float32) / np.float32(16))).astype(np.float32)
    rot = np.zeros((128, 16, 160), np.float32)
    for t in range(16):
        pos = (t * 128 + p).astype(np.float32)
        ar = (pos[:, None] * fr[None, :]).astype(np.float32)
        am = (pos[:, None] * fm[None, :]).astype(np.float32)
        cr, sr = np.cos(ar).astype(np.float32), np.sin(ar).astype(np.float32)
        cm, sm = np.cos(am).astype(np.float32), np.sin(am).astype(np.float32)
        rot[:, t, 0:32] = cr
        rot[:, t, 32:64] = cr
        rot[:, t, 64:96] = -sr
        rot[:, t, 96:128] = sr
        rot[:, t, 128:136] = cm
        rot[:, t, 136:144] = cm
        rot[:, t, 144:152] = -sm
        rot[:, t, 152:160] = sm
    cF[:, C_ROT:C_DEC] = rot.reshape(128, -1)
    lg = np.log(1.0 - 2.0 ** (-5.0 - np.arange(4, dtype=np.float64)))
    m = np.arange(128)[:, None].astype(np.float64)
    c = np.arange(128)[None, :].astype(np.float64)
    dec = np.zeros((128, 4, 128), np.float64)
    for h in range(4):
        dec[:, h, :] = np.where(c >= m, np.exp(lg[h] * np.maximum(c - m, 0.0)), 0.0) * 0.125
    cF[:, C_DEC:C_QDEC] = dec.reshape(128, -1)
    qd = np.zeros((128, 2, 128), np.float64)
    for pr in range(2):
        for hh in range(2):
            h = pr * 2 + hh
            qd[hh * 64:(hh + 1) * 64, pr, :] = np.exp(lg[h] * (np.arange(128) + 1.0))[None, :]
    cF[:, C_QDEC:C_KDEC] = qd.reshape(128, -1)
    for h in range(4):
        cF[:, C_KDEC + h] = np.exp(lg[h] * (127.0 - np.arange(128))) * 0.125
    gb = np.zeros((4, 8, 8), np.float32)
    for j in range(4, 8):
        gb[j - 4, :, j:] = -1e30
    cF[:, C_GB:C_END] = gb.reshape(1, -1)
    cd = [float(np.exp(lg[h] * 128.0)) for h in range(4)]
    cB = np.zeros((128, B_END), np.float32)
    cB[:, B_ID:B_ID + 128] = np.eye(128)
    cB[:, B_TRI:B_TRI + 128] = (np.arange(128)[:, None] <= np.arange(128)[None, :]).astype(np.float32)
    cB[:, B_IDB:B_IDB + 128] = np.eye(128) * BIGM
    return cF, cB.astype(ml_dtypes.bfloat16), cd


def bc(ap, shape):
    return ap.broadcast_to(list(shape))


def build_program(n_layers=DEPTH, nseq=NSEQ, do_ffn=True, nblk=8, stages="armd", debug=False):
    nc = bass.Bass("TRN2", target_bir_lowering=False)
    cF_np, cB_np, cd = host_constants()
    tok = nseq * SEQ
    ntile = tok // 128
    x_d = nc.dram_tensor("x", [tok, DM], F32, kind="ExternalInput").ap()
    w_in_d = nc.dram_tensor("w_in", [DEPTH, DM, 3072], F32, kind="ExternalInput").ap()
    w_out_d = nc.dram_tensor("w_out", [DEPTH, DM, DM], F32, kind="ExternalInput").ap()
    w_up_d = nc.dram_tensor("w_up", [DEPTH, DM, 4096], F32, kind="ExternalInput").ap()
    w_dn_d = nc.dram_tensor("w_down", [DEPTH, 4096, DM], F32, kind="ExternalInput").ap()
    g_d = {n: nc.dram_tensor(n, [DEPTH, DM], F32, kind="ExternalInput").ap()
           for n in ["g_mix_pre", "g_mix_post", "g_mlp_pre", "g_mlp_post"]}
    cF_d = nc.dram_tensor("cF", [128, C_END], F32, kind="ExternalInput").ap()
    cB_d = nc.dram_tensor("cB", [128, B_END], BF16, kind="ExternalInput").ap()
    y_d = nc.dram_tensor("y", [tok, DM], F32, kind="ExternalOutput").ap()
    dbg_d = nc.dram_tensor("dbg", [tok, DM], BF16, kind="ExternalOutput").ap() if debug else None

    with ExitStack() as top:
        P = Prog2(nc, top)
        _cnt = [0]

        def sbt(st, n, s, d):
            _cnt[0] += 1
            return st.enter_context(nc.sbuf_tensor("sb_%s_%d" % (n, _cnt[0]), s, d))
        pB = [top.enter_context(nc.psum_tensor("pB%d" % i, [128, 512], F32)) for i in range(3)]
        pT = top.enter_context(nc.psum_tensor("pT", [128, 1024], BF16))
        pR0 = top.enter_context(nc.psum_tensor("pR0", [128, 512], F32))
        pR1 = top.enter_context(nc.psum_tensor("pR1", [128, 512], F32))
        pM0 = top.enter_context(nc.psum_tensor("pM0", [128, 512], F32))
        pM1 = top.enter_context(nc.psum_tensor("pM1", [128, 512], F32))
        R_pB = [Region("pB%d" % i) for i in range(3)]
        _rpt = Region("pT")
        R_pT = [_rpt, _rpt]
        R_R0a = Region("R0")
        R_R0b = R_R0a
        R_R1 = Region("R1")
        R_M0 = [Region("M0"), R_R0a]
        R_O = [Region("M1"), R_R1]
        R_G = R_pB[2]
        pT2 = pM1[:, :].bitcast(BF16)
        R_pT2 = R_O[0]
        cB = sbt(top, "cB", [128, B_END], BF16)
        gA = sbt(top, "gA", [128, DM], F32)
        gB = sbt(top, "gB", [128, DM], F32)
        R_cB, R_gA, R_gB = Region("cB"), Region("gA"), Region("gB")
        ident = cB[:, B_ID:B_ID + 128]
        tri = cB[:, B_TRI:B_TRI + 128]
        identBig = cB[:, B_IDB:B_IDB + 128]
        P.dma(SP, lambda e: e.dma_start(out=cB[:], in_=cB_d[:, :]), w=[R_cB])
        R_y = [Region("y%d" % i) for i in range(ntile)]

        def rstd_from_ssq(stat, col_in, col_tmp, col_out, R_stat, eps, inv_n):
            P.op(ACT, lambda e: e.activation(out=stat[:, col_tmp:col_tmp + 1], in_=stat[:, col_in:col_in + 1],
                                             func=AF.Ln, scale=inv_n, bias=eps), r=[R_stat], w=[R_stat])
            P.op(ACT, lambda e: e.activation(out=stat[:, col_out:col_out + 1], in_=stat[:, col_tmp:col_tmp + 1],
                                             func=AF.Exp, scale=-0.5), r=[R_stat], w=[R_stat])

        for l in range(n_layers):
            src_d = x_d if l == 0 else y_d
            with ExitStack() as ph:
                WB = sbt(ph, "WBm", [128, 32768], BF16)
                w_in = WB[:, 0:24576].rearrange("p (k n) -> p k n", k=8)
                w_out = WB[:, 24576:32768].rearrange("p (k n) -> p k n", k=8)
                R_win = [Region("win%d" % k) for k in range(8)]
                R_wout = [Region("wout%d" % k) for k in range(8)]
                cF = sbt(ph, "cF", [128, C_END], F32)
                R_cF = Region("cF")
                rot = cF[:, C_ROT:C_DEC].rearrange("p (t n) -> p t n", t=16)
                dec = cF[:, C_DEC:C_QDEC].rearrange("p (h n) -> p h n", h=4)
                qdec = cF[:, C_QDEC:C_KDEC].rearrange("p (h n) -> p h n", h=2)
                kdec = cF[:, C_KDEC:C_KDEC + 4]
                gbias = cF[:, C_GB:C_END].rearrange("p (j n) -> p j n", j=4)
                mkT = sbt(ph, "mkT", [128, 4, SEQ], BF16)
                mv = sbt(ph, "mv", [128, 16, 8, 65], BF16)
                kmT = sbt(ph, "kmT", [128, 4, 8], BF16)
                km32 = sbt(ph, "km32", [128, 4], F32)
                MT = sbt(ph, "MT", [128, 256], BF16)
                R_mkT = [Region("mkT%d" % i) for i in range(16)]
                R_mv = [Region("mv%d" % i) for i in range(16)]
                R_kmT, R_km32, R_MT = Region("kmT"), Region("km32"), Region("MT")
                xt = sbt(ph, "xt", [128, 4, DM], F32)
                R_xt = [Region("xt%d" % i) for i in range(4)]
                hb = sbt(ph, "hb", [128, 2, DM], BF16)
                R_hb = [Region("hb0"), Region("hb1")]
                hT = sbt(ph, "hT", [128, 2, DM], BF16)
                R_hT = [Region("hT0"), Region("hT1")]
                stat = sbt(ph, "stat", [128, 2, 8], F32)
                R_stat = [Region("stat0"), Region("stat1")]
                stat2 = sbt(ph, "stat2", [128, 2, 8], F32)
                R_stat2 = [Region("stat2_0"), Region("stat2_1")]
                rqa = sbt(ph, "rqa", [128, 512], F32)
                rqb = sbt(ph, "rqb", [128, 512], F32)
                rqk = sbt(ph, "rqk", [128, 512], BF16)
                rkd = sbt(ph, "rkd", [128, 2, 256], BF16)
                R_rqa, R_rqb, R_rqk = Region("rqa"), Region("rqb"), Region("rqk")
                R_rkd = [Region("rkd0"), Region("rkd1")]
                rv = sbt(ph, "rv", [128, 2, 512], BF16)
                sg = sbt(ph, "sg", [128, 2, 512], BF16)
                R_rv = [Region("rv0"), Region("rv1")]
                R_sg = [Region("sg0"), Region("sg1")]
                sgt = sbt(ph, "sgt", [128, 512], F32)
                R_sgt = Region("sgt")
                gs = sbt(ph, "gs", [128, 512], F32)
                R_gs = Region("gs")
                mqs = sbt(ph, "mqs", [128, 2, 512], BF16)
                R_mqs = [Region("mqs0"), Region("mqs1")]
                mta = sbt(ph, "mta", [128, 2, 128], F32)
                mtb = sbt(ph, "mtb", [128, 2, 128], F32)
                R_mta = [Region("mta0"), Region("mta1")]
                R_mtb = [Region("mtb0"), Region("mtb1")]
                rqT = sbt(ph, "rqT", [128, 2, 256], BF16)
                rqdT = sbt(ph, "rqdT", [128, 2, 256], BF16)
                rkT = sbt(ph, "rkT", [128, 2, 256], BF16)
                mqT = sbt(ph, "mqT", [128, 4, 256], BF16)
                R_rqT = [Region("rqT0"), Region("rqT1")]
                R_rqdT = [Region("rqdT0"), Region("rqdT1")]
                R_rkT = [Region("rkT0"), Region("rkT1")]
                R_mqT = [Region("mqT0"), Region("mqT1")]
                ST = sbt(ph, "ST", [128, 4, 128], BF16)
                R_ST = [Region("ST0"), Region("ST1")]
                state = sbt(ph, "state", [128, 2, 128], F32)
                stateb = sbt(ph, "stateb", [128, 2, 2, 128], BF16)
                R_state = [Region("state0"), Region("state1")]
                R_stateb = [Region("stateb0"), Region("stateb1")]
                bst = sbt(ph, "bst", [128, 4, 6], F32)
                bag = sbt(ph, "bag", [128, 4, 2], F32)
                rs4 = sbt(ph, "rs4", [128, 8], F32)
                R_bst, R_bag, R_rs4 = Region("bst"), Region("bag"), Region("rs4")
                on = sbt(ph, "on", [128, 512], F32)
                R_on = Region("on")
                mix = sbt(ph, "mix", [128, 2, DM], BF16)
                R_mixr = [Region("mixr0"), Region("mixr1")]
                R_mixm = [Region("mixm0"), Region("mixm1")]
                mixT = sbt(ph, "mixT", [128, DM], BF16)
                R_mixT = Region("mixT")
                PT = sbt(ph, "PT", [128, 3, 256], BF16)
                R_PT = [Region("PT%d" % i) for i in range(3)]
                g2 = sbt(ph, "g2", [128, 2, 64], F32)
                m8 = sbt(ph, "m8", [128, 2, 64], F32)
                Mp = sbt(ph, "Mp", [128, 2, 128], BF16)
                rc = sbt(ph, "rc", [128, 2, 2], F32)
                R_g2 = [Region("g2_0"), Region("g2_1")]
                R_m8 = [Region("m8_0"), Region("m8_1")]
                R_Mp = [Region("Mp0"), Region("Mp1")]
                R_rc = [Region("rc0"), Region("rc1")]
                ytmp = sbt(ph, "ytmp", [128, DM], F32)
                R_ytmp = Region("ytmp")

                P.dma(SP, lambda e: e.dma_start(out=cF[:], in_=cF_d[:, :]), w=[R_cF])
                P.dma(SP, lambda e: e.dma_start(out=gA[:], in_=g_d["g_mix_pre"][l].partition_broadcast(128)), w=[R_gA])
                P.dma(SP, lambda e: e.dma_start(out=gB[:], in_=g_d["g_mix_post"][l].partition_broadcast(128)), w=[R_gB])
                for k in range(8):
                    for hh in range(2):
                        P.dma(POOL, lambda e, k=k, hh=hh: e.dma_start(
                            out=w_in[:, k, hh * 1536:(hh + 1) * 1536],
                            in_=w_in_d[l, k * 128:(k + 1) * 128, hh * 1536:(hh + 1) * 1536]), w=[R_win[k]])
                for k in range(8):
                    P.dma(POOL, lambda e, k=k: e.dma_start(out=w_out[:, k, :], in_=w_out_d[l, k * 128:(k + 1) * 128, :]),
                          w=[R_wout[k]])
                P.op(POOL, lambda e: e.memset(mv[:, :, :, 64:65], 1.0), w=R_mv)
                P.op(POOL, lambda e: e.memset(kmT[:], 0.0), w=[R_kmT])
                P.op(POOL, lambda e: e.memset(Mp[:], 0.0), w=R_Mp)

                def phase_a_pre(s, b, i):
                    tt = 2 * b + i
                    gt = s * 16 + tt
                    dbk = ((s * 8 + b) % 2) * 2 + i
                    X = xt[:, dbk, :]
                    RX = R_xt[dbk]
                    sl = gt % 2
                    P.dma(SP, lambda e: e.dma_start(out=X, in_=src_d[gt * 128:(gt + 1) * 128, :]),
                          r=([R_y[gt]] if l > 0 else []), w=[RX])
                    stt_ = stat[:, sl, :]
                    P.op(POOL, lambda e: e.memset(stt_, 0.0), w=[R_stat[sl]])
                    P.op(ACT, lambda e: e.activation(out=hb[:, sl, :], in_=X, func=AF.Square, accum_out=stt_[:, 0:1]),
                         r=[RX], w=[R_stat[sl], R_hb[sl]])
                    rstd_from_ssq(stt_, 0, 1, 2, R_stat[sl], NORM_EPS, 1.0 / DM)
                    P.op(DVE, lambda e: e.scalar_tensor_tensor(out=hb[:, sl, :], in0=X, scalar=stt_[:, 2:3], in1=gA[:],
                                                               op0=ALU.mult, op1=ALU.mult),
                         r=[RX, R_stat[sl], R_gA], w=[R_hb[sl]])
                    for k in range(8):
                        P.op(PE, lambda e, k=k: e.transpose(out=pT[:, k * 128:(k + 1) * 128], in_=hb[:, sl, k * 128:(k + 1) * 128],
                                                            identity=ident), r=[R_hb[sl], R_cB], w=[R_pT[k // 4]])
                    P.op(ACT, lambda e: e.copy(out=hT[:, sl, :], in_=pT[:, :]), r=R_pT, w=[R_hT[sl]])

                def phase_a(s, b, i):
                    tt = 2 * b + i
                    gt = s * 16 + tt
                    sl = gt % 2
                    import os
                    KCUT = int(os.environ.get("KCUT", "99"))
                    hTv = hT[:, sl, :].rearrange("p (k n) -> p k n", k=8)
                    rt = rot[:, tt, :]
                    for ci, cb in enumerate([3, 4, 0, 5, 1, 2]):
                        bank = pB[ci % 3]
                        Rb = R_pB[ci % 3]
                        for k in range(8):
                            P.op(PE, lambda e, k=k, cb=cb, bank=bank: e.matmul(
                                bank[:, :], lhsT=hTv[:, k, :], rhs=w_in[:, k, cb * 512:(cb + 1) * 512],
                                start=(k == 0), stop=(k == 7)), r=[R_hT[sl], R_win[k]], w=[Rb])
                        if cb == 0:
                            ps = bank[:, :].rearrange("p (a n) -> p a n", a=8)
                            av = rqa[:, :].rearrange("p (a n) -> p a n", a=8)
                            bv = rqb[:, :].rearrange("p (a n) -> p a n", a=8)
                            P.op(ACT, lambda e, bank=bank: e.copy(out=rqa[:, :], in_=bank[:, :]), r=[Rb], w=[R_rqa])
                            P.op(DVE, lambda e, av=av, bv=bv: e.tensor_tensor(
                                out=bv[:, :, 0:32], in0=av[:, :, 32:64], in1=bc(rt[:, 64:96].unsqueeze(1), [128, 8, 32]),
                                op=ALU.mult), r=[R_rqa, R_cF], w=[R_rqb])
                            P.op(DVE, lambda e, av=av, bv=bv: e.tensor_tensor(
                                out=bv[:, :, 32:64], in0=av[:, :, 0:32], in1=bc(rt[:, 96:128].unsqueeze(1), [128, 8, 32]),
                                op=ALU.mult), r=[R_rqa, R_cF], w=[R_rqb])
                            P.op(DVE, lambda e, av=av: e.tensor_tensor(
                                out=av, in0=av, in1=bc(rt[:, 0:64].unsqueeze(1), [128, 8, 64]), op=ALU.mult),
                                r=[R_rqa, R_cF], w=[R_rqa])
                            P.op(POOL, lambda e: e.tensor_tensor(out=rqk[:, :], in0=rqa[:, :], in1=rqb[:, :], op=ALU.add),
                                 r=[R_rqa, R_rqb], w=[R_rqk])
                            P.op(POOL, lambda e: e.tensor_tensor(
                                out=rkd[:, i, :].rearrange("p (h n) -> p h n", h=4),
                                in0=rqk[:, 256:512].rearrange("p (h n) -> p h n", h=4),
                                in1=bc(kdec.unsqueeze(2), [128, 4, 64]), op=ALU.mult), r=[R_rqk, R_cF], w=[R_rkd[i]])
                        elif cb == 1:
                            P.op(ACT, lambda e, bank=bank: e.copy(out=rv[:, i, :], in_=bank[:, :]), r=[Rb], w=[R_rv[i]])
                        elif cb == 2:
                            P.op(ACT, lambda e, bank=bank: e.activation(out=sgt[:, :], in_=bank[:, :], func=AF.Exp, scale=-1.0),
                                 r=[Rb], w=[R_sgt])
                            P.op(ACT, lambda e, bank=bank: e.copy(out=gs[:, :], in_=bank[:, :]), r=[Rb], w=[R_gs])
                            P.op(POOL, lambda e: e.tensor_scalar_add(out=sgt[:, :], in0=sgt[:, :], scalar1=1.0),
                                 r=[R_sgt], w=[R_sgt])
                            P.op(DVE, lambda e: e.reciprocal(out=sgt[:, :], in_=sgt[:, :]), r=[R_sgt], w=[R_sgt])
                            P.op(DVE, lambda e: e.tensor_tensor(out=sg[:, i, :], in0=gs[:, :], in1=sgt[:, :],
                                                                op=ALU.mult), r=[R_gs, R_sgt], w=[R_sg[i]])
                        elif cb in (3, 4):
                            j = cb - 3
                            ps = bank[:, :].rearrange("p (a n) -> p a n", a=8)
                            ov = mqs[:, j, :].rearrange("p (a n) -> p a n", a=8)
                            av = mta[:, j, :].rearrange("p (a n) -> p a n", a=8)
                            bv = mtb[:, j, :].rearrange("p (a n) -> p a n", a=8)
                            P.op(ACT, lambda e, bank=bank, j=j: e.copy(out=mqs[:, j, :], in_=bank[:, :]), r=[Rb], w=[R_mqs[j]])
                            P.op(DVE, lambda e, ov=ov, av=av: e.tensor_tensor(
                                out=av, in0=ov[:, :, 0:16], in1=bc(rt[:, 128:144].unsqueeze(1), [128, 8, 16]), op=ALU.mult),
                                r=[R_mqs[j], R_cF], w=[R_mta[j]])
                            P.op(DVE, lambda e, ov=ov, bv=bv: e.tensor_tensor(
                                out=bv[:, :, 0:8], in0=ov[:, :, 8:16], in1=bc(rt[:, 144:152].unsqueeze(1), [128, 8, 8]),
                                op=ALU.mult), r=[R_mqs[j], R_cF], w=[R_mtb[j]])
                            P.op(DVE, lambda e, ov=ov, bv=bv: e.tensor_tensor(
                                out=bv[:, :, 8:16], in0=ov[:, :, 0:8], in1=bc(rt[:, 152:160].unsqueeze(1), [128, 8, 8]),
                                op=ALU.mult), r=[R_mqs[j], R_cF], w=[R_mtb[j]])
                            P.op(POOL, lambda e, ov=ov, av=av, bv=bv: e.tensor_tensor(
                                out=ov[:, :, 0:16], in0=av, in1=bv, op=ALU.add),
                                r=[R_mta[j], R_mtb[j], R_mqs[j]], w=[R_mqs[j]])
                        else:
                            P.op(ACT, lambda e, bank=bank: e.copy(
                                out=mv[:, tt, :, 0:64], in_=bank[:, :].rearrange("p (a n) -> p a n", a=8)),
                                r=[Rb], w=[R_mv[tt]])
                    if KCUT <= 9:
                        return
                    cs = slice(i * 128, (i + 1) * 128)
                    KSUB = int(os.environ.get("KSUB", "0"))
                    for c4 in range(4):
                        if KSUB == 2:
                            break
                        P.op(PE, lambda e, c4=c4: e.transpose(out=pT2[:, c4 * 128:(c4 + 1) * 128],
                                                              in_=rqk[:, c4 * 128:(c4 + 1) * 128], identity=ident),
                             r=[R_rqk, R_cB], w=[R_pT2])
                    if KSUB == 1:
                        return
                    for c4 in range(4):
                        P.op(PE, lambda e, c4=c4: e.transpose(out=pT2[:, 512 + c4 * 128:512 + (c4 + 1) * 128],
                                                              in_=mqs[:, 0, c4 * 128:(c4 + 1) * 128], identity=ident),
                             r=[R_mqs[0], R_cB], w=[R_pT2])
                    if KSUB == 3:
                        return
                    P.op(DVE, lambda e: e.tensor_copy(out=rqT[:, :, cs], in_=pT2[:, 0:256].rearrange("p (a n) -> p a n", a=2)),
                         r=[R_pT2], w=[R_rqT[i]])
                    P.op(DVE, lambda e: e.tensor_copy(out=rkT[:, :, cs], in_=pT2[:, 256:512].rearrange("p (a n) -> p a n", a=2)),
                         r=[R_pT2], w=[R_rkT[i]])
                    if KSUB == 4:
                        return
                    P.op(ACT, lambda e: e.copy(out=mqT[:, :, cs], in_=pT2[:, 512:1024].rearrange("p (a n) -> p a n", a=4)),
                         r=[R_pT2], w=[R_mqT[i]])
                    if KCUT <= 10:
                        return
                    for c4 in range(4):
                        P.op(PE, lambda e, c4=c4: e.transpose(out=pT[:, c4 * 128:(c4 + 1) * 128],
                                                              in_=mqs[:, 1, c4 * 128:(c4 + 1) * 128], identity=ident),
                             r=[R_mqs[1], R_cB], w=[R_pT[0]])
                    P.op(ACT, lambda e: e.copy(out=mkT[:, :, tt * 128:(tt + 1) * 128],
                                               in_=pT[:, 0:512].rearrange("p (a n) -> p a n", a=4)),
                         r=[R_pT[0]], w=[R_mkT[tt]])
                    if KCUT <= 11:
                        return
                    P.op(POOL, lambda e: e.tensor_tensor(out=rqdT[:, :, cs], in0=rqT[:, :, cs], in1=qdec, op=ALU.mult),
                         r=[R_rqT[i], R_cF], w=[R_rqdT[i]])

                def gate(s, b, i):
                    if b < 4:
                        return
                    cs = slice(i * 128, (i + 1) * 128)
                    gbk = [pR0, pM0]
                    Rgb = [R_R0a, R_M0[0]]
                    for h in range(8):
                        pr, hh = divmod(h, 2)
                        hf = slice(hh * 64, (hh + 1) * 64)
                        P.op(PE, lambda e, h=h, pr=pr, hh=hh, hf=hf: e.matmul(
                            gbk[hh][:, pr * 8:(pr + 1) * 8], lhsT=mqT[hf, pr, cs], rhs=kmT[hf, pr, :],
                            start=True, stop=True), r=[R_mqT[i], R_kmT], w=[Rgb[hh]])
                    g2v = g2[:, i, :].rearrange("p (a c n) -> p a c n", a=4, c=2)
                    gbv = gbias[:, b - 4, :].rearrange("p (a c n) -> p a c n", a=4, c=2)
                    for hh in range(2):
                        P.op(DVE, lambda e, hh=hh: e.tensor_tensor(
                            out=g2v[:, :, hh, :], in0=gbk[hh][:, 0:32].rearrange("p (a n) -> p a n", a=4),
                            in1=gbv[:, :, hh, :], op=ALU.add), r=[Rgb[hh], R_cF], w=[R_g2[i]])
                    for h in range(8):
                        P.op(DVE, lambda e, h=h: e.max(out=m8[:, i, h * 8:(h + 1) * 8], in_=g2[:, i, h * 8:(h + 1) * 8]),
                             r=[R_g2[i]], w=[R_m8[i]])
                    for h in range(8):
                        P.op(DVE, lambda e, h=h: e.tensor_scalar(
                            out=Mp[:, i, (h % 2) * 64 + (h // 2) * 8:(h % 2) * 64 + (h // 2) * 8 + 8],
                            in0=g2[:, i, h * 8:(h + 1) * 8],
                            scalar1=m8[:, i, h * 8 + 2:h * 8 + 3], scalar2=1.0, op0=ALU.is_ge, op1=ALU.subtract),
                            r=[R_g2[i], R_m8[i]], w=[R_Mp[i]])

                def retention(s, b, i):
                    import os
                    RC = int(os.environ.get("RCUT", "99"))
                    cs = slice(i * 128, (i + 1) * 128)
                    sbk = [[pR0, pM0], [pB[0], pB[1]]]
                    Rsb = [[R_R0a, R_M0[0]], [R_pB[0], R_pB[1]]]
                    kvb = [pR0[:, 256:512], pB[2][:, 0:256]]
                    Rkv = [R_R0b, R_pB[2]]
                    for pr in range(2):
                        for hh in range(2):
                            hf = slice(hh * 64, (hh + 1) * 64)
                            P.op(PE, lambda e, hh=hh, hf=hf, pr=pr: e.matmul(
                                sbk[pr][hh][:, 0:128], lhsT=rkT[hf, pr, cs], rhs=rqT[hf, pr, cs],
                                start=True, stop=True), r=[R_rkT[i], R_rqT[i]], w=[Rsb[pr][hh]])
                    for pr in range(2):
                        for hh in range(2):
                            P.op(DVE, lambda e, pr=pr, hh=hh: e.tensor_tensor(
                                out=ST[:, pr * 2 + hh, :], in0=sbk[pr][hh][:, 0:128],
                                in1=dec[:, pr * 2 + hh, :], op=ALU.mult),
                                r=[Rsb[pr][hh], R_cF], w=[R_ST[pr]])
                    for pr in range(2):
                        for hh in range(2):
                            h = pr * 2 + hh
                            P.op(PE, lambda e, h=h: e.matmul(pR1[:, h * 128:(h + 1) * 128], lhsT=ST[:, h, :],
                                                             rhs=rv[:, i, h * 128:(h + 1) * 128], start=True, stop=False),
                                 r=[R_ST[pr], R_rv[i]], w=[R_R1])
                            P.op(PE, lambda e, h=h, hh=hh, pr=pr: e.matmul(
                                pR1[:, h * 128:(h + 1) * 128], lhsT=rqdT[:, pr, cs], rhs=stateb[:, pr, hh, :],
                                start=False, stop=True), r=[R_rqdT[i], R_stateb[pr]], w=[R_R1])
                    for pr in range(2):
                        P.op(PE, lambda e, pr=pr: e.matmul(kvb[pr], lhsT=rkd[:, i, pr * 128:(pr + 1) * 128],
                                                           rhs=rv[:, i, pr * 256:(pr + 1) * 256], start=True, stop=True),
                             r=[R_rkd[i], R_rv[i]], w=[Rkv[pr]])
                    for pr in range(2):
                        for hh in range(2):
                            h = pr * 2 + hh
                            hf = slice(hh * 64, (hh + 1) * 64)
                            P.op(DVE, lambda e, h=h, hh=hh, hf=hf, pr=pr: e.scalar_tensor_tensor(
                                out=state[hf, pr, :], in0=state[hf, pr, :], scalar=cd[h],
                                in1=kvb[pr][hf, hh * 128:(hh + 1) * 128], op0=ALU.mult, op1=ALU.add),
                                r=[Rkv[pr], R_state[pr]], w=[R_state[pr]])
                        for hh in range(2):
                            hf = slice(hh * 64, (hh + 1) * 64)
                            P.op(ACT, lambda e, pr=pr, hh=hh, hf=hf: e.copy(out=stateb[hf, pr, hh, :], in_=state[hf, pr, :]),
                                 r=[R_state[pr]], w=[R_stateb[pr]])
                    if RC <= 3:
                        return
                    for h in range(4):
                        P.op(DVE, lambda e, h=h: e.bn_stats(out=bst[:, h, :], in_=pR1[:, h * 128:(h + 1) * 128]),
                             r=[R_R1], w=[R_bst])
                    for h in range(4):
                        P.op(DVE, lambda e, h=h: e.bn_aggr(out=bag[:, h, :], in_=bst[:, h, :]), r=[R_bst], w=[R_bag])
                    if RC <= 4:
                        return
                    P.op(ACT, lambda e: e.activation(out=rs4[:, 0:4], in_=bag[:, :, 1], func=AF.Ln, bias=GN_EPS),
                         r=[R_bag], w=[R_rs4])
                    P.op(ACT, lambda e: e.activation(out=rs4[:, 4:8], in_=rs4[:, 0:4], func=AF.Exp, scale=-0.5),
                         r=[R_rs4], w=[R_rs4])
                    if RC <= 5:
                        return
                    for h in range(4):
                        P.op(DVE, lambda e, h=h: e.tensor_scalar(
                            out=on[:, h * 128:(h + 1) * 128], in0=pR1[:, h * 128:(h + 1) * 128],
                            scalar1=bag[:, h, 0:1], scalar2=rs4[:, 4 + h:5 + h], op0=ALU.subtract, op1=ALU.mult),
                            r=[R_R1, R_bag, R_rs4], w=[R_on])
                    P.op(POOL, lambda e: e.tensor_tensor(out=mix[:, i, 0:512], in0=on[:, :], in1=sg[:, i, :], op=ALU.mult),
                         r=[R_on, R_sg[i]], w=[R_mixr[i]])

                def moba(s, b):
                    bs = slice(b * 256, (b + 1) * 256)
                    if b < 7:
                        P.op(DVE, lambda e: e.tensor_reduce(out=km32[:, :], in_=mkT[:, :, bs], axis=AX.X, op=ALU.add),
                             r=[R_mkT[2 * b], R_mkT[2 * b + 1]], w=[R_km32])
                        P.op(ACT, lambda e: e.mul(out=kmT[:, :, b], in_=km32[:, :], mul=1.0 / 256), r=[R_km32], w=[R_kmT])
                    if b >= 4:
                        for i in range(2):
                            cs = slice(i * 128, (i + 1) * 128)
                            P.op(PE, lambda e, i=i: e.transpose(out=pT[:, 0:128], in_=Mp[:, i, :], identity=ident),
                                 r=[R_Mp[i], R_cB], w=[R_pT[0]])
                            P.op(ACT, lambda e, cs=cs: e.copy(out=MT[:, cs], in_=pT[:, 0:128]), r=[R_pT[0]], w=[R_MT])
                    nk = 2 * b + 2
                    units = [(h, kt) for h in range(8) for kt in range(nk)]
                    Ob = [[pM1, pR1], [pB[0], pB[1]]]
                    R_Ob = [[R_O[0], R_R1], [R_pB[0], R_pB[1]]]
                    scb = [pM0, pR0]

                    def scores(u):
                        h, kt = units[u]
                        pr, hh = divmod(h, 2)
                        hf = slice(hh * 64, (hh + 1) * 64)
                        q0 = 128 if kt == 2 * b + 1 else 0
                        slot = u % 2
                        sc = scb[slot][:, q0:256]
                        masked = (b >= 4 and kt < 2 * b)
                        P.op(PE, lambda e: e.matmul(sc, lhsT=mkT[hf, pr, kt * 128:(kt + 1) * 128], rhs=mqT[hf, pr, q0:256],
                                                    start=True, stop=not masked),
                             r=[R_mkT[kt], R_mqT[0], R_mqT[1]], w=[R_M0[slot]])
                        if masked:
                            rr = hh * 64 + pr * 8 + kt // 2
                            P.op(PE, lambda e: e.matmul(sc, lhsT=bc(identBig[hf, rr:rr + 1], [64, 128]),
                                                        rhs=MT[hf, q0:256], start=False, stop=True),
                                 r=[R_MT, R_cB], w=[R_M0[slot]])
                        pt = PT[:, u % 3, :]
                        P.op(ACT, lambda e: e.activation(out=pt[:, q0:256], in_=sc, func=AF.Exp, scale=0.125),
                             r=[R_M0[slot]], w=[R_PT[u % 3]])
                        if kt >= 2 * b:
                            P.op(POOL, lambda e: e.tensor_tensor(out=pt[:, q0:q0 + 128], in0=pt[:, q0:q0 + 128], in1=tri,
                                                                 op=ALU.mult), r=[R_PT[u % 3], R_cB], w=[R_PT[u % 3]])

                    def pv(u):
                        h, kt = units[u]
                        q0 = 128 if kt == 2 * b + 1 else 0
                        pt = PT[:, u % 3, :]
                        for qh in range(q0 // 128, 2):
                            last = (2 * b) if qh == 0 else (2 * b + 1)
                            P.op(PE, lambda e, qh=qh, last=last: e.matmul(
                                Ob[h % 2][qh][:, 0:65], lhsT=pt[:, qh * 128:(qh + 1) * 128], rhs=mv[:, kt, h, :],
                                start=(kt == 0), stop=(kt == last)), r=[R_PT[u % 3], R_mv[kt]], w=[R_Ob[h % 2][qh]])
                        if kt == nk - 1:
                            for qh in range(2):
                                P.op(DVE, lambda e, qh=qh: e.reciprocal(out=rc[:, h % 2, qh:qh + 1], in_=Ob[h % 2][qh][:, 64:65]),
                                     r=[R_Ob[h % 2][qh]], w=[R_rc[h % 2]])
                                P.op(DVE, lambda e, qh=qh: e.tensor_scalar(
                                    out=mix[:, qh, 512 + h * 64:512 + (h + 1) * 64], in0=Ob[h % 2][qh][:, 0:64],
                                    scalar1=rc[:, h % 2, qh:qh + 1], scalar2=None, op0=ALU.mult),
                                    r=[R_Ob[h % 2][qh], R_rc[h % 2]], w=[R_mixm[qh]])

                    scores(0)
                    for u in range(len(units)):
                        if u + 1 < len(units):
                            scores(u + 1)
                        pv(u)

                def phase_d(s, b, i):
                    tt = 2 * b + i
                    gt = s * 16 + tt
                    dbk = ((s * 8 + b) % 2) * 2 + i
                    X = xt[:, dbk, :]
                    RX = R_xt[dbk]
                    sl = gt % 2
                    if debug and l == 0:
                        P.dma(SP, lambda e: e.dma_start(out=dbg_d[gt * 128:(gt + 1) * 128, :], in_=mix[:, i, :]),
                              r=[R_mixr[i], R_mixm[i]], w=[Region("dbg%d" % gt)])
                    for k in range(8):
                        P.op(PE, lambda e, k=k: e.transpose(out=pT[:, k * 128:(k + 1) * 128], in_=mix[:, i, k * 128:(k + 1) * 128],
                                                            identity=ident),
                             r=[R_mixr[i], R_mixm[i], R_cB], w=[R_pT[k // 4]])
                    P.op(ACT, lambda e: e.copy(out=mixT[:, :], in_=pT[:, :]), r=R_pT, w=[R_mixT])
                    mixTv = mixT[:, :].rearrange("p (k n) -> p k n", k=8)
                    s2 = stat2[:, sl, :]
                    P.op(POOL, lambda e: e.memset(s2, 0.0), w=[R_stat2[sl]])
                    for hf in range(2):
                        for k in range(8):
                            P.op(PE, lambda e, k=k, hf=hf: e.matmul(pB[hf][:, :], lhsT=mixTv[:, k, :],
                                                                    rhs=w_out[:, k, hf * 512:(hf + 1) * 512],
                                                                    start=(k == 0), stop=(k == 7)),
                                 r=[R_mixT, R_wout[k]], w=[R_pB[hf]])
                        P.op(ACT, lambda e, hf=hf: e.activation(out=ytmp[:, hf * 512:(hf + 1) * 512], in_=pB[hf][:, :],
                                                                func=AF.Square, accum_out=s2[:, hf:hf + 1]),
                             r=[R_pB[hf]], w=[R_stat2[sl], R_ytmp])
                    P.op(DVE, lambda e: e.tensor_tensor(out=s2[:, 2:3], in0=s2[:, 0:1], in1=s2[:, 1:2], op=ALU.add),
                         r=[R_stat2[sl]], w=[R_stat2[sl]])
                    rstd_from_ssq(s2, 2, 3, 4, R_stat2[sl], NORM_EPS, 1.0 / DM)
                    for hf in range(2):
                        P.op(DVE, lambda e, hf=hf: e.scalar_tensor_tensor(
                            out=ytmp[:, hf * 512:(hf + 1) * 512], in0=pB[hf][:, :], scalar=s2[:, 4:5],
                            in1=gB[:, hf * 512:(hf + 1) * 512], op0=ALU.mult, op1=ALU.mult),
                            r=[R_pB[hf], R_stat2[sl], R_gB], w=[R_ytmp])
                    P.op(POOL, lambda e: e.tensor_tensor(out=X, in0=X, in1=ytmp[:, :], op=ALU.add), r=[RX, R_ytmp], w=[RX])
                    P.dma(SP, lambda e: e.dma_start(out=y_d[gt * 128:(gt + 1) * 128, :], in_=X), r=[RX], w=[R_y[gt]])

                for s in range(nseq):
                    P.op(POOL, lambda e: e.memset(state[:], 0.0), w=R_state)
                    P.op(POOL, lambda e: e.memset(stateb[:], 0.0), w=R_stateb)
                    for b in range(nblk):
                        if s == 0 and b == 0:
                            for i in range(2):
                                phase_a_pre(s, b, i)
                        if "a" in stages:
                            for i in range(2):
                                phase_a(s, b, i)
                                if "m" in stages:
                                    gate(s, b, i)
                        if "r" in stages:
                            for i in range(2):
                                retention(s, b, i)
                        nb_ = s * nblk + b + 1
                        if nb_ < nseq * nblk:
                            for i in range(2):
                                phase_a_pre(nb_ // nblk, nb_ % nblk, i)
                        if "m" in stages:
                            moba(s, b)
                        if "d" in stages:
                            for i in range(2):
                                phase_d(s, b, i)
                if debug and l == 0:
                    P.dma(SP, lambda e: e.dma_start(out=dbg_d[0:128, 0:512], in_=stateb[:].rearrange("p a b n -> p (a b n)")),
                          r=R_stateb, w=[Region("dbgs")])
                    P.dma(SP, lambda e: e.dma_start(out=dbg_d[128:256, 0:512], in_=rqdT[:].rearrange("p a n -> p (a n)")),
                          r=R_rqdT, w=[Region("dbgs3")])
                P.barrier()
                P.emit_block()

            if not do_ffn:
                continue
            with ExitStack() as ph:
                WB = sbt(ph, "WBf", [128, 65536], BF16)
                w_up = WB[:, 0:32768].rearrange("p (k n) -> p k n", k=8)
                w_dn = WB[:, 32768:65536].rearrange("p (c n) -> p c n", c=32)
                R_wup = [Region("wup%d" % k) for k in range(8)]
                R_wdn = [Region("wdn%d" % k) for k in range(8)]
                xt = sbt(ph, "xtf", [128, 4, DM], F32)
                R_xt = [Region("xtf%d" % i) for i in range(4)]
                hb = sbt(ph, "hbf", [128, 2, DM], BF16)
                R_hb = [Region("hbf0"), Region("hbf1")]
                hT = sbt(ph, "hTf", [128, 2, 8, 256], BF16)
                R_hT = [[Region("hTf%d_%d" % (d, i)) for i in range(2)] for d in range(2)]
                stat = sbt(ph, "statf", [128, 2, 8], F32)
                R_stat = [Region("statf0"), Region("statf1")]
                stat2 = sbt(ph, "stat2f", [128, 2, 8], F32)
                R_stat2 = [Region("stat2f0"), Region("stat2f1")]
                rl = sbt(ph, "rl", [128, 3, 256], BF16)
                R_rl = [Region("rl%d" % i) for i in range(3)]
                aT = sbt(ph, "aT", [128, 32, 256], BF16)
                R_aT = [Region("aT%d" % i) for i in range(32)]
                ytmp = sbt(ph, "ytmpf", [128, DM], F32)
                R_ytmp = Region("ytmpf")
                P.dma(SP, lambda e: e.dma_start(out=gA[:], in_=g_d["g_mlp_pre"][l].partition_broadcast(128)), w=[R_gA])
                P.dma(SP, lambda e: e.dma_start(out=gB[:], in_=g_d["g_mlp_post"][l].partition_broadcast(128)), w=[R_gB])
                for k in range(8):
                    for hh in range(2):
                        P.dma(POOL, lambda e, k=k, hh=hh: e.dma_start(
                            out=w_up[:, k, hh * 2048:(hh + 1) * 2048],
                            in_=w_up_d[l, k * 128:(k + 1) * 128, hh * 2048:(hh + 1) * 2048]), w=[R_wup[k]])
                wdv = w_dn_d[l].rearrange("(c p) n -> p c n", p=128)
                for k in range(8):
                    P.dma(POOL, lambda e, k=k: e.dma_start(out=w_dn[:, k * 4:(k + 1) * 4, :], in_=wdv[:, k * 4:(k + 1) * 4, :]),
                          w=[R_wdn[k]])
                upb = [pB[0][:, 0:256], pB[1][:, 0:256], pB[2][:, 0:256]]
                R_upb = R_pB
                dnb = [pR0, pR1, pM0, pM1]
                R_dnb = [Region("dnb%d" % i) for i in range(4)]
                ngrp = ntile // 2

                def prep(G):
                    db = G % 2
                    for i in range(2):
                        gt = G * 2 + i
                        dbk = db * 2 + i
                        X = xt[:, dbk, :]
                        RX = R_xt[dbk]
                        sl = i
                        P.dma(SP, lambda e, X=X, gt=gt: e.dma_start(out=X, in_=y_d[gt * 128:(gt + 1) * 128, :]),
                              r=[R_y[gt]], w=[RX])
                        stt_ = stat[:, sl, :]
                        P.op(POOL, lambda e, stt_=stt_: e.memset(stt_, 0.0), w=[R_stat[sl]])
                        P.op(ACT, lambda e, X=X, stt_=stt_, sl=sl: e.activation(out=hb[:, sl, :], in_=X, func=AF.Square,
                                                                                accum_out=stt_[:, 0:1]),
                             r=[RX], w=[R_stat[sl], R_hb[sl]])
                        rstd_from_ssq(stt_, 0, 1, 2, R_stat[sl], NORM_EPS, 1.0 / DM)
                        P.op(DVE, lambda e, X=X, stt_=stt_, sl=sl: e.scalar_tensor_tensor(
                            out=hb[:, sl, :], in0=X, scalar=stt_[:, 2:3], in1=gA[:], op0=ALU.mult, op1=ALU.mult),
                            r=[RX, R_stat[sl], R_gA], w=[R_hb[sl]])
                        for k in range(8):
                            P.op(PE, lambda e, k=k, sl=sl: e.transpose(out=pT[:, k * 128:(k + 1) * 128],
                                                                       in_=hb[:, sl, k * 128:(k + 1) * 128], identity=ident),
                                 r=[R_hb[sl], R_cB], w=[R_pT[k // 4]])
                        P.op(ACT, lambda e, db=db, i=i: e.copy(out=hT[:, db, :, i * 128:(i + 1) * 128],
                                                               in_=pT[:, :].rearrange("p (k n) -> p k n", k=8)),
                             r=R_pT, w=[R_hT[db][i]])

                def up(G):
                    db = G % 2
                    for fc in range(32):
                        bank = upb[fc % 3]
                        Rb = R_upb[fc % 3]
                        for k in range(8):
                            P.op(PE, lambda e, k=k, fc=fc, bank=bank, db=db: e.matmul(
                                bank, lhsT=w_up[:, k, fc * 128:(fc + 1) * 128], rhs=hT[:, db, k, :],
                                start=(k == 0), stop=(k == 7)), r=[R_wup[k], R_hT[db][0], R_hT[db][1]], w=[Rb])
                        P.op(ACT, lambda e, fc=fc, bank=bank: e.activation(out=rl[:, fc % 3, :], in_=bank, func=AF.Relu),
                             r=[Rb], w=[R_rl[fc % 3]])
                        P.op(POOL, lambda e, fc=fc: e.tensor_tensor(out=aT[:, fc, :], in0=rl[:, fc % 3, :], in1=rl[:, fc % 3, :],
                                                                    op=ALU.mult), r=[R_rl[fc % 3]], w=[R_aT[fc]])

                def down(G):
                    for fc in range(32):
                        for i in range(2):
                            for hf in range(2):
                                bi = i * 2 + hf
                                P.op(PE, lambda e, fc=fc, i=i, hf=hf, bi=bi: e.matmul(
                                    dnb[bi][:, :], lhsT=aT[:, fc, i * 128:(i + 1) * 128], rhs=w_dn[:, fc, hf * 512:(hf + 1) * 512],
                                    start=(fc == 0), stop=(fc == 31)), r=[R_aT[fc], R_wdn[fc // 4]], w=[R_dnb[bi]])

                def post(G):
                    db = G % 2
                    for i in range(2):
                        gt = G * 2 + i
                        dbk = db * 2 + i
                        X = xt[:, dbk, :]
                        RX = R_xt[dbk]
                        s2 = stat2[:, i, :]
                        R2 = R_stat2[i]
                        P.op(POOL, lambda e, s2=s2: e.memset(s2, 0.0), w=[R2])
                        for hf in range(2):
                            bi = i * 2 + hf
                            P.op(ACT, lambda e, hf=hf, bi=bi, s2=s2: e.activation(
                                out=ytmp[:, hf * 512:(hf + 1) * 512], in_=dnb[bi][:, :], func=AF.Square,
                                accum_out=s2[:, hf:hf + 1]), r=[R_dnb[bi]], w=[R2, R_ytmp])
                        P.op(DVE, lambda e, s2=s2: e.tensor_tensor(out=s2[:, 2:3], in0=s2[:, 0:1], in1=s2[:, 1:2], op=ALU.add),
                             r=[R2], w=[R2])
                        rstd_from_ssq(s2, 2, 3, 4, R2, NORM_EPS, 1.0 / DM)
                        for hf in range(2):
                            bi = i * 2 + hf
                            P.op(DVE, lambda e, hf=hf, bi=bi, s2=s2: e.scalar_tensor_tensor(
                                out=ytmp[:, hf * 512:(hf + 1) * 512], in0=dnb[bi][:, :], scalar=s2[:, 4:5],
                                in1=gB[:, hf * 512:(hf + 1) * 512], op0=ALU.mult, op1=ALU.mult),
                                r=[R_dnb[bi], R2, R_gB], w=[R_ytmp])
                        P.op(POOL, lambda e, X=X: e.tensor_tensor(out=X, in0=X, in1=ytmp[:, :], op=ALU.add),
                             r=[RX, R_ytmp], w=[RX])
                        P.dma(SP, lambda e, X=X, gt=gt: e.dma_start(out=y_d[gt * 128:(gt + 1) * 128, :], in_=X),
                              r=[RX], w=[R_y[gt]])

                prep(0)
                for G in range(ngrp):
                    up(G)
                    if G + 1 < ngrp:
                        prep(G + 1)
                    down(G)
                    post(G)
                P.barrier()
                P.emit_block()
    return nc


_NC_CACHE = {}


def kernel(x, w_in, w_out, w_up, w_down, g_mix_pre, g_mix_post, g_mlp_pre, g_mlp_post):
    if "nc" not in _NC_CACHE:
        _NC_CACHE["nc"] = build_program()
    nc = _NC_CACHE["nc"]
    cF, cB, _ = host_constants()
    x = np.ascontiguousarray(x, dtype=np.float32)
    B = x.shape[0]
    per = B // NCORES
    common = {
        "w_in": np.ascontiguousarray(w_in, np.float32), "w_out": np.ascontiguousarray(w_out, np.float32),
        "w_up": np.ascontiguousarray(w_up, np.float32), "w_down": np.ascontiguousarray(w_down, np.float32),
        "g_mix_pre": np.ascontiguousarray(g_mix_pre, np.float32), "g_mix_post": np.ascontiguousarray(g_mix_post, np.float32),
        "g_mlp_pre": np.ascontiguousarray(g_mlp_pre, np.float32), "g_mlp_post": np.ascontiguousarray(g_mlp_post, np.float32),
        "cF": cF, "cB": cB,
    }
    in_maps = []
    for c in range(NCORES):
        d = dict(common)
        d["x"] = x[c * per:(c + 1) * per].reshape(per * SEQ, DM)
        in_maps.append(d)
    res = run_bass_kernel_spmd(nc, in_maps, core_ids=list(range(NCORES)))
    out = np.stack([np.asarray(r["y"]).reshape(per, SEQ, DM) for r in res.results], axis=0)
    return out.reshape(B, SEQ, DM).astype(np.float32)
```

```python
import math
from contextlib import ExitStack
import numpy as np
import ml_dtypes
import concourse.bass as bass
import concourse.mybir as mybir
from concourse.bass_utils import run_bass_kernel_spmd

F32 = mybir.dt.float32
BF16 = mybir.dt.bfloat16
ALU = mybir.AluOpType
AF = mybir.ActivationFunctionType
AX = mybir.AxisListType

PE, ACT, DVE, POOL, SP = "pe", "act", "dve", "pool", "sp"
ENGS = [PE, ACT, DVE, POOL, SP]
SAME_ENGINE_SYNC = {PE: False, ACT: True, DVE: True, POOL: True, SP: False}
N_DMA_SEMS = 24


class Region:
    __slots__ = ("name", "last_w", "reads")

    def __init__(self, name):
        self.name = name
        self.last_w = None
        self.reads = []


class Op:
    __slots__ = ("eng", "idx", "fn", "deps", "is_dma", "dma_slot", "dma_val", "signal", "semval")

    def __init__(self, eng, idx, fn, is_dma):
        self.eng = eng
        self.idx = idx
        self.fn = fn
        self.deps = []
        self.is_dma = is_dma
        self.dma_slot = None
        self.dma_val = None
        self.signal = False
        self.semval = None


class Prog:
    def __init__(self, nc):
        self.nc = nc
        self.ops = {e: [] for e in ENGS}
        self.known = {e: {} for e in ENGS}
        self.n_dma = 0
        self.dma_last = [None] * N_DMA_SEMS

    def _add(self, eng, fn, r, w, is_dma):
        op = Op(eng, self._next_idx(eng), fn, is_dma)
        deps = []
        for reg in r:
            if reg.last_w is not None:
                deps.append(reg.last_w)
        for reg in w:
            if reg.last_w is not None:
                deps.append(reg.last_w)
            deps.extend(reg.reads)
        if is_dma:
            slot = self.n_dma % N_DMA_SEMS
            op.dma_slot = slot
            op.dma_val = 16 * (self.n_dma // N_DMA_SEMS + 1)
            if self.dma_last[slot] is not None:
                deps.append(self.dma_last[slot])
            self.dma_last[slot] = op
            self.n_dma += 1
            op.signal = True
        kn = self.known[eng]
        for d in deps:
            if d.is_dma:
                key = ("dma", d.dma_slot)
                val = d.dma_val
            else:
                if d.eng == eng and not SAME_ENGINE_SYNC[eng]:
                    continue
                key = d.eng
                val = d.idx
            if kn.get(key, -1) >= val:
                continue
            kn[key] = val
            d.signal = True
            op.deps.append(d)
        for reg in r:
            reg.reads.append(op)
        for reg in w:
            reg.last_w = op
            reg.reads = []
        self.ops[eng].append(op)
        return op

    def _next_idx(self, eng):
        return len(self.ops[eng])

    def op(self, eng, fn, r=(), w=()):
        return self._add(eng, fn, r, w, False)

    def dma(self, eng, fn, r=(), w=()):
        return self._add(eng, fn, r, w, True)

    def emit(self, final_regions=()):
        nc = self.nc
        self._add(SP, None, list(final_regions), [], False)
        with ExitStack() as st:
            sems = {e: st.enter_context(nc.semaphore("sem_" + e)) for e in ENGS}
            dsems = [st.enter_context(nc.semaphore("sem_dma%d" % i)) for i in range(N_DMA_SEMS)]
            for e in ENGS:
                c = 0
                for op in self.ops[e]:
                    if op.is_dma:
                        continue
                    if op.signal:
                        c += 1
                        op.semval = c
            block = st.enter_context(nc.Block())

            def run(e):
                def body(eng):
                    for op in self.ops[e]:
                        for d in op.deps:
                            if d.is_dma:
                                eng.wait_ge(dsems[d.dma_slot], d.dma_val)
                            else:
                                eng.wait_ge(sems[d.eng], d.semval)
                        if op.fn is None:
                            continue
                        ins = op.fn(eng)
                        if op.is_dma:
                            ins.then_inc(dsems[op.dma_slot], 16)
                        elif op.signal:
                            ins.then_inc(sems[e], 1)
                return body

            block.tensor(run(PE))
            block.scalar(run(ACT))
            block.vector(run(DVE))
            block.gpsimd(run(POOL))
            block.sync(run(SP))


class Prog2(Prog):
    def __init__(self, nc, stack):
        super().__init__(nc)
        self.nidx = {e: 0 for e in ENGS}
        self.semcnt = {e: 0 for e in ENGS}
        self.last_compute = {e: None for e in ENGS}
        self.sems = {e: stack.enter_context(nc.semaphore("sem_" + e)) for e in ENGS}
        self.dsems = [stack.enter_context(nc.semaphore("sem_dma%d" % i)) for i in range(N_DMA_SEMS)]

    def _next_idx(self, eng):
        i = self.nidx[eng]
        self.nidx[eng] += 1
        return i

    def _add(self, eng, fn, r, w, is_dma):
        op = super()._add(eng, fn, r, w, is_dma)
        if not is_dma and fn is not None:
            self.last_compute[eng] = op
        return op

    def barrier(self):
        lasts = dict(self.last_compute)
        dl = list(self.dma_last)
        for e in ENGS:
            op = Op(e, self.nidx[e], None, False)
            self.nidx[e] += 1
            kn = self.known[e]
            for e2 in ENGS:
                d = lasts[e2]
                if d is None:
                    continue
                if kn.get(e2, -1) >= d.idx:
                    continue
                kn[e2] = d.idx
                d.signal = True
                op.deps.append(d)
            for d in dl:
                if d is None:
                    continue
                key = ("dma", d.dma_slot)
                if kn.get(key, -1) >= d.dma_val:
                    continue
                kn[key] = d.dma_val
                op.deps.append(d)
            self.ops[e].append(op)

    def emit_block(self):
        nc = self.nc
        for e in ENGS:
            for op in self.ops[e]:
                if op.is_dma:
                    continue
                if op.signal and op.semval is None:
                    self.semcnt[e] += 1
                    op.semval = self.semcnt[e]
        sems, dsems = self.sems, self.dsems
        ops = {e: self.ops[e] for e in ENGS}
        self.ops = {e: [] for e in ENGS}
        with nc.Block() as block:
            def run(e):
                def body(eng):
                    for op in ops[e]:
                        for d in op.deps:
                            if d.is_dma:
                                eng.wait_ge(dsems[d.dma_slot], d.dma_val)
                            else:
                                assert d.semval is not None
                                eng.wait_ge(sems[d.eng], d.semval)
                        if op.fn is None:
                            continue
                        ins = op.fn(eng)
                        if op.is_dma:
                            ins.then_inc(dsems[op.dma_slot], 16)
                        elif op.signal:
                            ins.then_inc(sems[e], 1)
                return body

            block.tensor(run(PE))
            block.scalar(run(ACT))
            block.vector(run(DVE))
            block.gpsimd(run(POOL))
            block.sync(run(SP))


NCORES = 8
SEQ = 2048
DM = 1024
NSEQ = 2
TOK = NSEQ * SEQ
NTILE = TOK // 128
DEPTH = 2
NORM_EPS = 1e-6
GN_EPS = 1e-5
BIGM = 32768.0

C_ROT = 0
C_DEC = C_ROT + 16 * 160
C_QDEC = C_DEC + 512
C_KDEC = C_QDEC + 256
C_GB = C_KDEC + 4
C_END = C_GB + 256
B_ID = 0
B_TRI = 128
B_IDB = 256
B_END = 384


def host_constants():
    cF = np.zeros((128, C_END), np.float32)
    p = np.arange(128)
    fr = (np.float32(10000.0) ** (-np.arange(0, 64, 2, dtype=np.float32) / np.float32(64))).astype(np.float32)
    fm = (np.float32(500000.0) ** (-np.arange(0, 16, 2, dtype=np.float32) / np.float32(16))).astype(np.float32)
    rot = np.zeros((128, 16, 160), np.float32)
    for t in range(16):
        pos = (t * 128 + p).astype(np.float32)
        ar = (pos[:, None] * fr[None, :]).astype(np.float32)
        am = (pos[:, None] * fm[None, :]).astype(np.float32)
        cr, sr = np.cos(ar).astype(np.float32), np.sin(ar).astype(np.float32)
        cm, sm = np.cos(am).astype(np.float32), np.sin(am).astype(np.float32)
        rot[:, t, 0:32] = cr
        rot[:, t, 32:64] = cr
        rot[:, t, 64:96] = -sr
        rot[:, t, 96:128] = sr
        rot[:, t, 128:136] = cm
        rot[:, t, 136:144] = cm
        rot[:, t, 144:152] = -sm
        rot[:, t, 152:160] = sm
    cF[:, C_ROT:C_DEC] = rot.reshape(128, -1)
    lg = np.log(1.0 - 2.0 ** (-5.0 - np.arange(4, dtype=np.float64)))
    m = np.arange(128)[:, None].astype(np.float64)
    c = np.arange(128)[None, :].astype(np.float64)
    dec = np.zeros((128, 4, 128), np.float64)
    for h in range(4):
        dec[:, h, :] = np.where(c >= m, np.exp(lg[h] * np.maximum(c - m, 0.0)), 0.0) * 0.125
    cF[:, C_DEC:C_QDEC] = dec.reshape(128, -1)
    qd = np.zeros((128, 2, 128), np.float64)
    for pr in range(2):
        for hh in range(2):
            h = pr * 2 + hh
            qd[hh * 64:(hh + 1) * 64, pr, :] = np.exp(lg[h] * (np.arange(128) + 1.0))[None, :]
    cF[:, C_QDEC:C_KDEC] = qd.reshape(128, -1)
    for h in range(4):
        cF[:, C_KDEC + h] = np.exp(lg[h] * (127.0 - np.arange(128))) * 0.125
    gb = np.zeros((4, 8, 8), np.float32)
    for j in range(4, 8):
        gb[j - 4, :, j:] = -1e30
    cF[:, C_GB:C_END] = gb.reshape(1, -1)
    cd = [float(np.exp(lg[h] * 128.0)) for h in range(4)]
    cB = np.zeros((128, B_END), np.float32)
    cB[:, B_ID:B_ID + 128] = np.eye(128)
    cB[:, B_TRI:B_TRI + 128] = (np.arange(128)[:, None] <= np.arange(128)[None, :]).astype(np.float32)
    cB[:, B_IDB:B_IDB + 128] = np.eye(128) * BIGM
    return cF, cB.astype(ml_dtypes.bfloat16), cd


def bc(ap, shape):
    return ap.broadcast_to(list(shape))


def build_program(n_layers=DEPTH, nseq=NSEQ, do_ffn=True, nblk=8, stages="armd", debug=False):
    nc = bass.Bass("TRN2", target_bir_lowering=False)
    cF_np, cB_np, cd = host_constants()
    tok = nseq * SEQ
    ntile = tok // 128
    x_d = nc.dram_tensor("x", [tok, DM], F32, kind="ExternalInput").ap()
    w_in_d = nc.dram_tensor("w_in", [DEPTH, DM, 3072], F32, kind="ExternalInput").ap()
    w_out_d = nc.dram_tensor("w_out", [DEPTH, DM, DM], F32, kind="ExternalInput").ap()
    w_up_d = nc.dram_tensor("w_up", [DEPTH, DM, 4096], F32, kind="ExternalInput").ap()
    w_dn_d = nc.dram_tensor("w_down", [DEPTH, 4096, DM], F32, kind="ExternalInput").ap()
    g_d = {n: nc.dram_tensor(n, [DEPTH, DM], F32, kind="ExternalInput").ap()
           for n in ["g_mix_pre", "g_mix_post", "g_mlp_pre", "g_mlp_post"]}
    cF_d = nc.dram_tensor("cF", [128, C_END], F32, kind="ExternalInput").ap()
    cB_d = nc.dram_tensor("cB", [128, B_END], BF16, kind="ExternalInput").ap()
    y_d = nc.dram_tensor("y", [tok, DM], F32, kind="ExternalOutput").ap()
    dbg_d = nc.dram_tensor("dbg", [tok, DM], BF16, kind="ExternalOutput").ap() if debug else None

    with ExitStack() as top:
        P = Prog2(nc, top)
        _cnt = [0]

        def sbt(st, n, s, d):
            _cnt[0] += 1
            return st.enter_context(nc.sbuf_tensor("sb_%s_%d" % (n, _cnt[0]), s, d))
        pB = [top.enter_context(nc.psum_tensor("pB%d" % i, [128, 512], F32)) for i in range(3)]
        pT = top.enter_context(nc.psum_tensor("pT", [128, 1024], BF16))
        pR0 = top.enter_context(nc.psum_tensor("pR0", [128, 512], F32))
        pR1 = top.enter_context(nc.psum_tensor("pR1", [128, 512], F32))
        pM0 = top.enter_context(nc.psum_tensor("pM0", [128, 512], F32))
        pM1 = top.enter_context(nc.psum_tensor("pM1", [128, 512], F32))
        R_pB = [Region("pB%d" % i) for i in range(3)]
        _rpt = Region("pT")
        R_pT = [_rpt, _rpt]
        R_R0a = Region("R0")
        R_R0b = R_R0a
        R_R1 = Region("R1")
        R_M0 = [Region("M0"), R_R0a]
        R_O = [Region("M1"), R_R1]
        R_G = R_pB[2]
        pT2 = pM1[:, :].bitcast(BF16)
        R_pT2 = R_O[0]
        cB = sbt(top, "cB", [128, B_END], BF16)
        gA = sbt(top, "gA", [128, DM], F32)
        gB = sbt(top, "gB", [128, DM], F32)
        R_cB, R_gA, R_gB = Region("cB"), Region("gA"), Region("gB")
        ident = cB[:, B_ID:B_ID + 128]
        tri = cB[:, B_TRI:B_TRI + 128]
        identBig = cB[:, B_IDB:B_IDB + 128]
        P.dma(SP, lambda e: e.dma_start(out=cB[:], in_=cB_d[:, :]), w=[R_cB])
        R_y = [Region("y%d" % i) for i in range(ntile)]

        def rstd_from_ssq(stat, col_in, col_tmp, col_out, R_stat, eps, inv_n):
            P.op(ACT, lambda e: e.activation(out=stat[:, col_tmp:col_tmp + 1], in_=stat[:, col_in:col_in + 1],
                                             func=AF.Ln, scale=inv_n, bias=eps), r=[R_stat], w=[R_stat])
            P.op(ACT, lambda e: e.activation(out=stat[:, col_out:col_out + 1], in_=stat[:, col_tmp:col_tmp + 1],
                                             func=AF.Exp, scale=-0.5), r=[R_stat], w=[R_stat])

        for l in range(n_layers):
            src_d = x_d if l == 0 else y_d
            with ExitStack() as ph:
                WB = sbt(ph, "WBm", [128, 32768], BF16)
                w_in = WB[:, 0:24576].rearrange("p (k n) -> p k n", k=8)
                w_out = WB[:, 24576:32768].rearrange("p (k n) -> p k n", k=8)
                R_win = [Region("win%d" % k) for k in range(8)]
                R_wout = [Region("wout%d" % k) for k in range(8)]
                cF = sbt(ph, "cF", [128, C_END], F32)
                R_cF = Region("cF")
                rot = cF[:, C_ROT:C_DEC].rearrange("p (t n) -> p t n", t=16)
                dec = cF[:, C_DEC:C_QDEC].rearrange("p (h n) -> p h n", h=4)
                qdec = cF[:, C_QDEC:C_KDEC].rearrange("p (h n) -> p h n", h=2)
                kdec = cF[:, C_KDEC:C_KDEC + 4]
                gbias = cF[:, C_GB:C_END].rearrange("p (j n) -> p j n", j=4)
                mkT = sbt(ph, "mkT", [128, 4, SEQ], BF16)
                mv = sbt(ph, "mv", [128, 16, 8, 65], BF16)
                kmT = sbt(ph, "kmT", [128, 4, 8], BF16)
                km32 = sbt(ph, "km32", [128, 4], F32)
                MT = sbt(ph, "MT", [128, 256], BF16)
                R_mkT = [Region("mkT%d" % i) for i in range(16)]
                R_mv = [Region("mv%d" % i) for i in range(16)]
                R_kmT, R_km32, R_MT = Region("kmT"), Region("km32"), Region("MT")
                xt = sbt(ph, "xt", [128, 4, DM], F32)
                R_xt = [Region("xt%d" % i) for i in range(4)]
                hb = sbt(ph, "hb", [128, 2, DM], BF16)
                R_hb = [Region("hb0"), Region("hb1")]
                hT = sbt(ph, "hT", [128, 2, DM], BF16)
                R_hT = [Region("hT0"), Region("hT1")]
                stat = sbt(ph, "stat", [128, 2, 8], F32)
                R_stat = [Region("stat0"), Region("stat1")]
                stat2 = sbt(ph, "stat2", [128, 2, 8], F32)
                R_stat2 = [Region("stat2_0"), Region("stat2_1")]
                rqa = sbt(ph, "rqa", [128, 512], F32)
                rqb = sbt(ph, "rqb", [128, 512], F32)
                rqk = sbt(ph, "rqk", [128, 512], BF16)
                rkd = sbt(ph, "rkd", [128, 2, 256], BF16)
                R_rqa, R_rqb, R_rqk = Region("rqa"), Region("rqb"), Region("rqk")
                R_rkd = [Region("rkd0"), Region("rkd1")]
                rv = sbt(ph, "rv", [128, 2, 512], BF16)
                sg = sbt(ph, "sg", [128, 2, 512], BF16)
                R_rv = [Region("rv0"), Region("rv1")]
                R_sg = [Region("sg0"), Region("sg1")]
                sgt = sbt(ph, "sgt", [128, 512], F32)
                R_sgt = Region("sgt")
                gs = sbt(ph, "gs", [128, 512], F32)
                R_gs = Region("gs")
                mqs = sbt(ph, "mqs", [128, 2, 512], BF16)
                R_mqs = [Region("mqs0"), Region("mqs1")]
                mta = sbt(ph, "mta", [128, 2, 128], F32)
                mtb = sbt(ph, "mtb", [128, 2, 128], F32)
                R_mta = [Region("mta0"), Region("mta1")]
                R_mtb = [Region("mtb0"), Region("mtb1")]
                rqT = sbt(ph, "rqT", [128, 2, 256], BF16)
                rqdT = sbt(ph, "rqdT", [128, 2, 256], BF16)
                rkT = sbt(ph, "rkT", [128, 2, 256], BF16)
                mqT = sbt(ph, "mqT", [128, 4, 256], BF16)
                R_rqT = [Region("rqT0"), Region("rqT1")]
                R_rqdT = [Region("rqdT0"), Region("rqdT1")]
                R_rkT = [Region("rkT0"), Region("rkT1")]
                R_mqT = [Region("mqT0"), Region("mqT1")]
                ST = sbt(ph, "ST", [128, 4, 128], BF16)
                R_ST = [Region("ST0"), Region("ST1")]
                state = sbt(ph, "state", [128, 2, 128], F32)
                stateb = sbt(ph, "stateb", [128, 2, 2, 128], BF16)
                R_state = [Region("state0"), Region("state1")]
                R_stateb = [Region("stateb0"), Region("stateb1")]
                bst = sbt(ph, "bst", [128, 4, 6], F32)
                bag = sbt(ph, "bag", [128, 4, 2], F32)
                rs4 = sbt(ph, "rs4", [128, 8], F32)
                R_bst, R_bag, R_rs4 = Region("bst"), Region("bag"), Region("rs4")
                on = sbt(ph, "on", [128, 512], F32)
                R_on = Region("on")
                mix = sbt(ph, "mix", [128, 2, DM], BF16)
                R_mixr = [Region("mixr0"), Region("mixr1")]
                R_mixm = [Region("mixm0"), Region("mixm1")]
                mixT = sbt(ph, "mixT", [128, DM], BF16)
                R_mixT = Region("mixT")
                PT = sbt(ph, "PT", [128, 3, 256], BF16)
                R_PT = [Region("PT%d" % i) for i in range(3)]
                g2 = sbt(ph, "g2", [128, 2, 64], F32)
                m8 = sbt(ph, "m8", [128, 2, 64], F32)
                Mp = sbt(ph, "Mp", [128, 2, 128], BF16)
                rc = sbt(ph, "rc", [128, 2, 2], F32)
                R_g2 = [Region("g2_0"), Region("g2_1")]
                R_m8 = [Region("m8_0"), Region("m8_1")]
                R_Mp = [Region("Mp0"), Region("Mp1")]
                R_rc = [Region("rc0"), Region("rc1")]
                ytmp = sbt(ph, "ytmp", [128, DM], F32)
                R_ytmp = Region("ytmp")

                P.dma(SP, lambda e: e.dma_start(out=cF[:], in_=cF_d[:, :]), w=[R_cF])
                P.dma(SP, lambda e: e.dma_start(out=gA[:], in_=g_d["g_mix_pre"][l].partition_broadcast(128)), w=[R_gA])
                P.dma(SP, lambda e: e.dma_start(out=gB[:], in_=g_d["g_mix_post"][l].partition_broadcast(128)), w=[R_gB])
                for k in range(8):
                    for hh in range(2):
                        P.dma(POOL, lambda e, k=k, hh=hh: e.dma_start(
                            out=w_in[:, k, hh * 1536:(hh + 1) * 1536],
                            in_=w_in_d[l, k * 128:(k + 1) * 128, hh * 1536:(hh + 1) * 1536]), w=[R_win[k]])
                for k in range(8):
                    P.dma(POOL, lambda e, k=k: e.dma_start(out=w_out[:, k, :], in_=w_out_d[l, k * 128:(k + 1) * 128, :]),
                          w=[R_wout[k]])
                P.op(POOL, lambda e: e.memset(mv[:, :, :, 64:65], 1.0), w=R_mv)
                P.op(POOL, lambda e: e.memset(kmT[:], 0.0), w=[R_kmT])
                P.op(POOL, lambda e: e.memset(Mp[:], 0.0), w=R_Mp)

                def phase_a_pre(s, b, i):
                    tt = 2 * b + i
                    gt = s * 16 + tt
                    dbk = ((s * 8 + b) % 2) * 2 + i
                    X = xt[:, dbk, :]
                    RX = R_xt[dbk]
                    sl = gt % 2
                    P.dma(SP, lambda e: e.dma_start(out=X, in_=src_d[gt * 128:(gt + 1) * 128, :]),
                          r=([R_y[gt]] if l > 0 else []), w=[RX])
                    stt_ = stat[:, sl, :]
                    P.op(POOL, lambda e: e.memset(stt_, 0.0), w=[R_stat[sl]])
                    P.op(ACT, lambda e: e.activation(out=hb[:, sl, :], in_=X, func=AF.Square, accum_out=stt_[:, 0:1]),
                         r=[RX], w=[R_stat[sl], R_hb[sl]])
                    rstd_from_ssq(stt_, 0, 1, 2, R_stat[sl], NORM_EPS, 1.0 / DM)
                    P.op(DVE, lambda e: e.scalar_tensor_tensor(out=hb[:, sl, :], in0=X, scalar=stt_[:, 2:3], in1=gA[:],
                                                               op0=ALU.mult, op1=ALU.mult),
                         r=[RX, R_stat[sl], R_gA], w=[R_hb[sl]])
                    for k in range(8):
                        P.op(PE, lambda e, k=k: e.transpose(out=pT[:, k * 128:(k + 1) * 128], in_=hb[:, sl, k * 128:(k + 1) * 128],
                                                            identity=ident), r=[R_hb[sl], R_cB], w=[R_pT[k // 4]])
                    P.op(ACT, lambda e: e.copy(out=hT[:, sl, :], in_=pT[:, :]), r=R_pT, w=[R_hT[sl]])

                def phase_a(s, b, i):
                    tt = 2 * b + i
                    gt = s * 16 + tt
                    sl = gt % 2
                    import os
                    KCUT = int(os.environ.get("KCUT", "99"))
                    hTv = hT[:, sl, :].rearrange("p (k n) -> p k n", k=8)
                    rt = rot[:, tt, :]
                    for ci, cb in enumerate([3, 4, 0, 5, 1, 2]):
                        bank = pB[ci % 3]
                        Rb = R_pB[ci % 3]
                        for k in range(8):
                            P.op(PE, lambda e, k=k, cb=cb, bank=bank: e.matmul(
                                bank[:, :], lhsT=hTv[:, k, :], rhs=w_in[:, k, cb * 512:(cb + 1) * 512],
                                start=(k == 0), stop=(k == 7)), r=[R_hT[sl], R_win[k]], w=[Rb])
                        if cb == 0:
                            ps = bank[:, :].rearrange("p (a n) -> p a n", a=8)
                            av = rqa[:, :].rearrange("p (a n) -> p a n", a=8)
                            bv = rqb[:, :].rearrange("p (a n) -> p a n", a=8)
                            P.op(ACT, lambda e, bank=bank: e.copy(out=rqa[:, :], in_=bank[:, :]), r=[Rb], w=[R_rqa])
                            P.op(DVE, lambda e, av=av, bv=bv: e.tensor_tensor(
                                out=bv[:, :, 0:32], in0=av[:, :, 32:64], in1=bc(rt[:, 64:96].unsqueeze(1), [128, 8, 32]),
                                op=ALU.mult), r=[R_rqa, R_cF], w=[R_rqb])
                            P.op(DVE, lambda e, av=av, bv=bv: e.tensor_tensor(
                                out=bv[:, :, 32:64], in0=av[:, :, 0:32], in1=bc(rt[:, 96:128].unsqueeze(1), [128, 8, 32]),
                                op=ALU.mult), r=[R_rqa, R_cF], w=[R_rqb])
                            P.op(DVE, lambda e, av=av: e.tensor_tensor(
                                out=av, in0=av, in1=bc(rt[:, 0:64].unsqueeze(1), [128, 8, 64]), op=ALU.mult),
                                r=[R_rqa, R_cF], w=[R_rqa])
                            P.op(POOL, lambda e: e.tensor_tensor(out=rqk[:, :], in0=rqa[:, :], in1=rqb[:, :], op=ALU.add),
                                 r=[R_rqa, R_rqb], w=[R_rqk])
                            P.op(POOL, lambda e: e.tensor_tensor(
                                out=rkd[:, i, :].rearrange("p (h n) -> p h n", h=4),
                                in0=rqk[:, 256:512].rearrange("p (h n) -> p h n", h=4),
                                in1=bc(kdec.unsqueeze(2), [128, 4, 64]), op=ALU.mult), r=[R_rqk, R_cF], w=[R_rkd[i]])
                        elif cb == 1:
                            P.op(ACT, lambda e, bank=bank: e.copy(out=rv[:, i, :], in_=bank[:, :]), r=[Rb], w=[R_rv[i]])
                        elif cb == 2:
                            P.op(ACT, lambda e, bank=bank: e.activation(out=sgt[:, :], in_=bank[:, :], func=AF.Exp, scale=-1.0),
                                 r=[Rb], w=[R_sgt])
                            P.op(ACT, lambda e, bank=bank: e.copy(out=gs[:, :], in_=bank[:, :]), r=[Rb], w=[R_gs])
                            P.op(POOL, lambda e: e.tensor_scalar_add(out=sgt[:, :], in0=sgt[:, :], scalar1=1.0),
                                 r=[R_sgt], w=[R_sgt])
                            P.op(DVE, lambda e: e.reciprocal(out=sgt[:, :], in_=sgt[:, :]), r=[R_sgt], w=[R_sgt])
                            P.op(DVE, lambda e: e.tensor_tensor(out=sg[:, i, :], in0=gs[:, :], in1=sgt[:, :],
                                                                op=ALU.mult), r=[R_gs, R_sgt], w=[R_sg[i]])
                        elif cb in (3, 4):
                            j = cb - 3
                            ps = bank[:, :].rearrange("p (a n) -> p a n", a=8)
                            ov = mqs[:, j, :].rearrange("p (a n) -> p a n", a=8)
                            av = mta[:, j, :].rearrange("p (a n) -> p a n", a=8)
                            bv = mtb[:, j, :].rearrange("p (a n) -> p a n", a=8)
                            P.op(ACT, lambda e, bank=bank, j=j: e.copy(out=mqs[:, j, :], in_=bank[:, :]), r=[Rb], w=[R_mqs[j]])
                            P.op(DVE, lambda e, ov=ov, av=av: e.tensor_tensor(
                                out=av, in0=ov[:, :, 0:16], in1=bc(rt[:, 128:144].unsqueeze(1), [128, 8, 16]), op=ALU.mult),
                                r=[R_mqs[j], R_cF], w=[R_mta[j]])
                            P.op(DVE, lambda e, ov=ov, bv=bv: e.tensor_tensor(
                                out=bv[:, :, 0:8], in0=ov[:, :, 8:16], in1=bc(rt[:, 144:152].unsqueeze(1), [128, 8, 8]),
                                op=ALU.mult), r=[R_mqs[j], R_cF], w=[R_mtb[j]])
                            P.op(DVE, lambda e, ov=ov, bv=bv: e.tensor_tensor(
                                out=bv[:, :, 8:16], in0=ov[:, :, 0:8], in1=bc(rt[:, 152:160].unsqueeze(1), [128, 8, 8]),
                                op=ALU.mult), r=[R_mqs[j], R_cF], w=[R_mtb[j]])
                            P.op(POOL, lambda e, ov=ov, av=av, bv=bv: e.tensor_tensor(
                                out=ov[:, :, 0:16], in0=av, in1=bv, op=ALU.add),
                                r=[R_mta[j], R_mtb[j], R_mqs[j]], w=[R_mqs[j]])
                        else:
                            P.op(ACT, lambda e, bank=bank: e.copy(
                                out=mv[:, tt, :, 0:64], in_=bank[:, :].rearrange("p (a n) -> p a n", a=8)),
                                r=[Rb], w=[R_mv[tt]])
                    if KCUT <= 9:
                        return
                    cs = slice(i * 128, (i + 1) * 128)
                    KSUB = int(os.environ.get("KSUB", "0"))
                    for c4 in range(4):
                        if KSUB == 2:
                            break
                        P.op(PE, lambda e, c4=c4: e.transpose(out=pT2[:, c4 * 128:(c4 + 1) * 128],
                                                              in_=rqk[:, c4 * 128:(c4 + 1) * 128], identity=ident),
                             r=[R_rqk, R_cB], w=[R_pT2])
                    if KSUB == 1:
                        return
                    for c4 in range(4):
                        P.op(PE, lambda e, c4=c4: e.transpose(out=pT2[:, 512 + c4 * 128:512 + (c4 + 1) * 128],
                                                              in_=mqs[:, 0, c4 * 128:(c4 + 1) * 128], identity=ident),
                             r=[R_mqs[0], R_cB], w=[R_pT2])
                    if KSUB == 3:
                        return
                    P.op(DVE, lambda e: e.tensor_copy(out=rqT[:, :, cs], in_=pT2[:, 0:256].rearrange("p (a n) -> p a n", a=2)),
                         r=[R_pT2], w=[R_rqT[i]])
                    P.op(DVE, lambda e: e.tensor_copy(out=rkT[:, :, cs], in_=pT2[:, 256:512].rearrange("p (a n) -> p a n", a=2)),
                         r=[R_pT2], w=[R_rkT[i]])
                    if KSUB == 4:
                        return
                    P.op(ACT, lambda e: e.copy(out=mqT[:, :, cs], in_=pT2[:, 512:1024].rearrange("p (a n) -> p a n", a=4)),
                         r=[R_pT2], w=[R_mqT[i]])
                    if KCUT <= 10:
                        return
                    for c4 in range(4):
                        P.op(PE, lambda e, c4=c4: e.transpose(out=pT[:, c4 * 128:(c4 + 1) * 128],
                                                              in_=mqs[:, 1, c4 * 128:(c4 + 1) * 128], identity=ident),
                             r=[R_mqs[1], R_cB], w=[R_pT[0]])
                    P.op(ACT, lambda e: e.copy(out=mkT[:, :, tt * 128:(tt + 1) * 128],
                                               in_=pT[:, 0:512].rearrange("p (a n) -> p a n", a=4)),
                         r=[R_pT[0]], w=[R_mkT[tt]])
                    if KCUT <= 11:
                        return
                    P.op(POOL, lambda e: e.tensor_tensor(out=rqdT[:, :, cs], in0=rqT[:, :, cs], in1=qdec, op=ALU.mult),
                         r=[R_rqT[i], R_cF], w=[R_rqdT[i]])

                def gate(s, b, i):
                    if b < 4:
                        return
                    cs = slice(i * 128, (i + 1) * 128)
                    gbk = [pR0, pM0]
                    Rgb = [R_R0a, R_M0[0]]
                    for h in range(8):
                        pr, hh = divmod(h, 2)
                        hf = slice(hh * 64, (hh + 1) * 64)
                        P.op(PE, lambda e, h=h, pr=pr, hh=hh, hf=hf: e.matmul(
                            gbk[hh][:, pr * 8:(pr + 1) * 8], lhsT=mqT[hf, pr, cs], rhs=kmT[hf, pr, :],
                            start=True, stop=True), r=[R_mqT[i], R_kmT], w=[Rgb[hh]])
                    g2v = g2[:, i, :].rearrange("p (a c n) -> p a c n", a=4, c=2)
                    gbv = gbias[:, b - 4, :].rearrange("p (a c n) -> p a c n", a=4, c=2)
                    for hh in range(2):
                        P.op(DVE, lambda e, hh=hh: e.tensor_tensor(
                            out=g2v[:, :, hh, :], in0=gbk[hh][:, 0:32].rearrange("p (a n) -> p a n", a=4),
                            in1=gbv[:, :, hh, :], op=ALU.add), r=[Rgb[hh], R_cF], w=[R_g2[i]])
                    for h in range(8):
                        P.op(DVE, lambda e, h=h: e.max(out=m8[:, i, h * 8:(h + 1) * 8], in_=g2[:, i, h * 8:(h + 1) * 8]),
                             r=[R_g2[i]], w=[R_m8[i]])
                    for h in range(8):
                        P.op(DVE, lambda e, h=h: e.tensor_scalar(
                            out=Mp[:, i, (h % 2) * 64 + (h // 2) * 8:(h % 2) * 64 + (h // 2) * 8 + 8],
                            in0=g2[:, i, h * 8:(h + 1) * 8],
                            scalar1=m8[:, i, h * 8 + 2:h * 8 + 3], scalar2=1.0, op0=ALU.is_ge, op1=ALU.subtract),
                            r=[R_g2[i], R_m8[i]], w=[R_Mp[i]])

                def retention(s, b, i):
                    import os
                    RC = int(os.environ.get("RCUT", "99"))
                    cs = slice(i * 128, (i + 1) * 128)
                    sbk = [[pR0, pM0], [pB[0], pB[1]]]
                    Rsb = [[R_R0a, R_M0[0]], [R_pB[0], R_pB[1]]]
                    kvb = [pR0[:, 256:512], pB[2][:, 0:256]]
                    Rkv = [R_R0b, R_pB[2]]
                    for pr in range(2):
                        for hh in range(2):
                            hf = slice(hh * 64, (hh + 1) * 64)
                            P.op(PE, lambda e, hh=hh, hf=hf, pr=pr: e.matmul(
                                sbk[pr][hh][:, 0:128], lhsT=rkT[hf, pr, cs], rhs=rqT[hf, pr, cs],
                                start=True, stop=True), r=[R_rkT[i], R_rqT[i]], w=[Rsb[pr][hh]])
                    for pr in range(2):
                        for hh in range(2):
                            P.op(DVE, lambda e, pr=pr, hh=hh: e.tensor_tensor(
                                out=ST[:, pr * 2 + hh, :], in0=sbk[pr][hh][:, 0:128],
                                in1=dec[:, pr * 2 + hh, :], op=ALU.mult),
                                r=[Rsb[pr][hh], R_cF], w=[R_ST[pr]])
                    for pr in range(2):
                        for hh in range(2):
                            h = pr * 2 + hh
                            P.op(PE, lambda e, h=h: e.matmul(pR1[:, h * 128:(h + 1) * 128], lhsT=ST[:, h, :],
                                                             rhs=rv[:, i, h * 128:(h + 1) * 128], start=True, stop=False),
                                 r=[R_ST[pr], R_rv[i]], w=[R_R1])
                            P.op(PE, lambda e, h=h, hh=hh, pr=pr: e.matmul(
                                pR1[:, h * 128:(h + 1) * 128], lhsT=rqdT[:, pr, cs], rhs=stateb[:, pr, hh, :],
                                start=False, stop=True), r=[R_rqdT[i], R_stateb[pr]], w=[R_R1])
                    for pr in range(2):
                        P.op(PE, lambda e, pr=pr: e.matmul(kvb[pr], lhsT=rkd[:, i, pr * 128:(pr + 1) * 128],
                                                           rhs=rv[:, i, pr * 256:(pr + 1) * 256], start=True, stop=True),
                             r=[R_rkd[i], R_rv[i]], w=[Rkv[pr]])
                    for pr in range(2):
                        for hh in range(2):
                            h = pr * 2 + hh
                            hf = slice(hh * 64, (hh + 1) * 64)
                            P.op(DVE, lambda e, h=h, hh=hh, hf=hf, pr=pr: e.scalar_tensor_tensor(
                                out=state[hf, pr, :], in0=state[hf, pr, :], scalar=cd[h],
                                in1=kvb[pr][hf, hh * 128:(hh + 1) * 128], op0=ALU.mult, op1=ALU.add),
                                r=[Rkv[pr], R_state[pr]], w=[R_state[pr]])
                        for hh in range(2):
                            hf = slice(hh * 64, (hh + 1) * 64)
                            P.op(ACT, lambda e, pr=pr, hh=hh, hf=hf: e.copy(out=stateb[hf, pr, hh, :], in_=state[hf, pr, :]),
                                 r=[R_state[pr]], w=[R_stateb[pr]])
                    if RC <= 3:
                        return
                    for h in range(4):
                        P.op(DVE, lambda e, h=h: e.bn_stats(out=bst[:, h, :], in_=pR1[:, h * 128:(h + 1) * 128]),
                             r=[R_R1], w=[R_bst])
                    for h in range(4):
                        P.op(DVE, lambda e, h=h: e.bn_aggr(out=bag[:, h, :], in_=bst[:, h, :]), r=[R_bst], w=[R_bag])
                    if RC <= 4:
                        return
                    P.op(ACT, lambda e: e.activation(out=rs4[:, 0:4], in_=bag[:, :, 1], func=AF.Ln, bias=GN_EPS),
                         r=[R_bag], w=[R_rs4])
                    P.op(ACT, lambda e: e.activation(out=rs4[:, 4:8], in_=rs4[:, 0:4], func=AF.Exp, scale=-0.5),
                         r=[R_rs4], w=[R_rs4])
                    if RC <= 5:
                        return
                    for h in range(4):
                        P.op(DVE, lambda e, h=h: e.tensor_scalar(
                            out=on[:, h * 128:(h + 1) * 128], in0=pR1[:, h * 128:(h + 1) * 128],
                            scalar1=bag[:, h, 0:1], scalar2=rs4[:, 4 + h:5 + h], op0=ALU.subtract, op1=ALU.mult),
                            r=[R_R1, R_bag, R_rs4], w=[R_on])
                    P.op(POOL, lambda e: e.tensor_tensor(out=mix[:, i, 0:512], in0=on[:, :], in1=sg[:, i, :], op=ALU.mult),
                         r=[R_on, R_sg[i]], w=[R_mixr[i]])

                def moba(s, b):
                    bs = slice(b * 256, (b + 1) * 256)
                    if b < 7:
                        P.op(DVE, lambda e: e.tensor_reduce(out=km32[:, :], in_=mkT[:, :, bs], axis=AX.X, op=ALU.add),
                             r=[R_mkT[2 * b], R_mkT[2 * b + 1]], w=[R_km32])
                        P.op(ACT, lambda e: e.mul(out=kmT[:, :, b], in_=km32[:, :], mul=1.0 / 256), r=[R_km32], w=[R_kmT])
                    if b >= 4:
                        for i in range(2):
                            cs = slice(i * 128, (i + 1) * 128)
                            P.op(PE, lambda e, i=i: e.transpose(out=pT[:, 0:128], in_=Mp[:, i, :], identity=ident),
                                 r=[R_Mp[i], R_cB], w=[R_pT[0]])
                            P.op(ACT, lambda e, cs=cs: e.copy(out=MT[:, cs], in_=pT[:, 0:128]), r=[R_pT[0]], w=[R_MT])
                    nk = 2 * b + 2
                    units = [(h, kt) for h in range(8) for kt in range(nk)]
                    Ob = [[pM1, pR1], [pB[0], pB[1]]]
                    R_Ob = [[R_O[0], R_R1], [R_pB[0], R_pB[1]]]
                    scb = [pM0, pR0]

                    def scores(u):
                        h, kt = units[u]
                        pr, hh = divmod(h, 2)
                        hf = slice(hh * 64, (hh + 1) * 64)
                        q0 = 128 if kt == 2 * b + 1 else 0
                        slot = u % 2
                        sc = scb[slot][:, q0:256]
                        masked = (b >= 4 and kt < 2 * b)
                        P.op(PE, lambda e: e.matmul(sc, lhsT=mkT[hf, pr, kt * 128:(kt + 1) * 128], rhs=mqT[hf, pr, q0:256],
                                                    start=True, stop=not masked),
                             r=[R_mkT[kt], R_mqT[0], R_mqT[1]], w=[R_M0[slot]])
                        if masked:
                            rr = hh * 64 + pr * 8 + kt // 2
                            P.op(PE, lambda e: e.matmul(sc, lhsT=bc(identBig[hf, rr:rr + 1], [64, 128]),
                                                        rhs=MT[hf, q0:256], start=False, stop=True),
                                 r=[R_MT, R_cB], w=[R_M0[slot]])
                        pt = PT[:, u % 3, :]
                        P.op(ACT, lambda e: e.activation(out=pt[:, q0:256], in_=sc, func=AF.Exp, scale=0.125),
                             r=[R_M0[slot]], w=[R_PT[u % 3]])
                        if kt >= 2 * b:
                            P.op(POOL, lambda e: e.tensor_tensor(out=pt[:, q0:q0 + 128], in0=pt[:, q0:q0 + 128], in1=tri,
                                                                 op=ALU.mult), r=[R_PT[u % 3], R_cB], w=[R_PT[u % 3]])

                    def pv(u):
                        h, kt = units[u]
                        q0 = 128 if kt == 2 * b + 1 else 0
                        pt = PT[:, u % 3, :]
                        for qh in range(q0 // 128, 2):
                            last = (2 * b) if qh == 0 else (2 * b + 1)
                            P.op(PE, lambda e, qh=qh, last=last: e.matmul(
                                Ob[h % 2][qh][:, 0:65], lhsT=pt[:, qh * 128:(qh + 1) * 128], rhs=mv[:, kt, h, :],
                                start=(kt == 0), stop=(kt == last)), r=[R_PT[u % 3], R_mv[kt]], w=[R_Ob[h % 2][qh]])
                        if kt == nk - 1:
                            for qh in range(2):
                                P.op(DVE, lambda e, qh=qh: e.reciprocal(out=rc[:, h % 2, qh:qh + 1], in_=Ob[h % 2][qh][:, 64:65]),
                                     r=[R_Ob[h % 2][qh]], w=[R_rc[h % 2]])
                                P.op(DVE, lambda e, qh=qh: e.tensor_scalar(
                                    out=mix[:, qh, 512 + h * 64:512 + (h + 1) * 64], in0=Ob[h % 2][qh][:, 0:64],
                                    scalar1=rc[:, h % 2, qh:qh + 1], scalar2=None, op0=ALU.mult),
                                    r=[R_Ob[h % 2][qh], R_rc[h % 2]], w=[R_mixm[qh]])

                    scores(0)
                    for u in range(len(units)):
                        if u + 1 < len(units):
                            scores(u + 1)
                        pv(u)

                def phase_d(s, b, i):
                    tt = 2 * b + i
                    gt = s * 16 + tt
                    dbk = ((s * 8 + b) % 2) * 2 + i
                    X = xt[:, dbk, :]
                    RX = R_xt[dbk]
                    sl = gt % 2
                    if debug and l == 0:
                        P.dma(SP, lambda e: e.dma_start(out=dbg_d[gt * 128:(gt + 1) * 128, :], in_=mix[:, i, :]),
                              r=[R_mixr[i], R_mixm[i]], w=[Region("dbg%d" % gt)])
                    for k in range(8):
                        P.op(PE, lambda e, k=k: e.transpose(out=pT[:, k * 128:(k + 1) * 128], in_=mix[:, i, k * 128:(k + 1) * 128],
                                                            identity=ident),
                             r=[R_mixr[i], R_mixm[i], R_cB], w=[R_pT[k // 4]])
                    P.op(ACT, lambda e: e.copy(out=mixT[:, :], in_=pT[:, :]), r=R_pT, w=[R_mixT])
                    mixTv = mixT[:, :].rearrange("p (k n) -> p k n", k=8)
                    s2 = stat2[:, sl, :]
                    P.op(POOL, lambda e: e.memset(s2, 0.0), w=[R_stat2[sl]])
                    for hf in range(2):
                        for k in range(8):
                            P.op(PE, lambda e, k=k, hf=hf: e.matmul(pB[hf][:, :], lhsT=mixTv[:, k, :],
                                                                    rhs=w_out[:, k, hf * 512:(hf + 1) * 512],
                                                                    start=(k == 0), stop=(k == 7)),
                                 r=[R_mixT, R_wout[k]], w=[R_pB[hf]])
                        P.op(ACT, lambda e, hf=hf: e.activation(out=ytmp[:, hf * 512:(hf + 1) * 512], in_=pB[hf][:, :],
                                                                func=AF.Square, accum_out=s2[:, hf:hf + 1]),
                             r=[R_pB[hf]], w=[R_stat2[sl], R_ytmp])
                    P.op(DVE, lambda e: e.tensor_tensor(out=s2[:, 2:3], in0=s2[:, 0:1], in1=s2[:, 1:2], op=ALU.add),
                         r=[R_stat2[sl]], w=[R_stat2[sl]])
                    rstd_from_ssq(s2, 2, 3, 4, R_stat2[sl], NORM_EPS, 1.0 / DM)
                    for hf in range(2):
                        P.op(DVE, lambda e, hf=hf: e.scalar_tensor_tensor(
                            out=ytmp[:, hf * 512:(hf + 1) * 512], in0=pB[hf][:, :], scalar=s2[:, 4:5],
                            in1=gB[:, hf * 512:(hf + 1) * 512], op0=ALU.mult, op1=ALU.mult),
                            r=[R_pB[hf], R_stat2[sl], R_gB], w=[R_ytmp])
                    P.op(POOL, lambda e: e.tensor_tensor(out=X, in0=X, in1=ytmp[:, :], op=ALU.add), r=[RX, R_ytmp], w=[RX])
                    P.dma(SP, lambda e: e.dma_start(out=y_d[gt * 128:(gt + 1) * 128, :], in_=X), r=[RX], w=[R_y[gt]])

                for s in range(nseq):
                    P.op(POOL, lambda e: e.memset(state[:], 0.0), w=R_state)
                    P.op(POOL, lambda e: e.memset(stateb[:], 0.0), w=R_stateb)
                    for b in range(nblk):
                        if s == 0 and b == 0:
                            for i in range(2):
                                phase_a_pre(s, b, i)
                        if "a" in stages:
                            for i in range(2):
                                phase_a(s, b, i)
                                if "m" in stages:
                                    gate(s, b, i)
                        if "r" in stages:
                            for i in range(2):
                                retention(s, b, i)
                        nb_ = s * nblk + b + 1
                        if nb_ < nseq * nblk:
                            for i in range(2):
                                phase_a_pre(nb_ // nblk, nb_ % nblk, i)
                        if "m" in stages:
                            moba(s, b)
                        if "d" in stages:
                            for i in range(2):
                                phase_d(s, b, i)
                if debug and l == 0:
                    P.dma(SP, lambda e: e.dma_start(out=dbg_d[0:128, 0:512], in_=stateb[:].rearrange("p a b n -> p (a b n)")),
                          r=R_stateb, w=[Region("dbgs")])
                    P.dma(SP, lambda e: e.dma_start(out=dbg_d[128:256, 0:512], in_=rqdT[:].rearrange("p a n -> p (a n)")),
                          r=R_rqdT, w=[Region("dbgs3")])
                P.barrier()
                P.emit_block()

            if not do_ffn:
                continue
            with ExitStack() as ph:
                WB = sbt(ph, "WBf", [128, 65536], BF16)
                w_up = WB[:, 0:32768].rearrange("p (k n) -> p k n", k=8)
                w_dn = WB[:, 32768:65536].rearrange("p (c n) -> p c n", c=32)
                R_wup = [Region("wup%d" % k) for k in range(8)]
                R_wdn = [Region("wdn%d" % k) for k in range(8)]
                xt = sbt(ph, "xtf", [128, 6, DM], F32)
                R_xt = [Region("xtf%d" % i) for i in range(6)]
                hb = sbt(ph, "hbf", [128, 2, DM], BF16)
                R_hb = [Region("hbf0"), Region("hbf1")]
                hT = sbt(ph, "hTf", [128, 2, 8, 256], BF16)
                R_hT = [[Region("hTf%d_%d" % (d, i)) for i in range(2)] for d in range(2)]
                stat = sbt(ph, "statf", [128, 2, 8], F32)
                R_stat = [Region("statf0"), Region("statf1")]
                stat2 = sbt(ph, "stat2f", [128, 2, 8], F32)
                R_stat2 = [Region("stat2f0"), Region("stat2f1")]
                rl = sbt(ph, "rl", [128, 3, 256], BF16)
                R_rl = [Region("rl%d" % i) for i in range(3)]
                aT = sbt(ph, "aT", [128, 32, 256], BF16)
                R_aT = [Region("aT%d" % i) for i in range(32)]
                ytmp = sbt(ph, "ytmpf", [128, DM], F32)
                R_ytmp = Region("ytmpf")
                P.dma(SP, lambda e: e.dma_start(out=gA[:], in_=g_d["g_mlp_pre"][l].partition_broadcast(128)), w=[R_gA])
                P.dma(SP, lambda e: e.dma_start(out=gB[:], in_=g_d["g_mlp_post"][l].partition_broadcast(128)), w=[R_gB])
                for k in range(8):
                    for hh in range(2):
                        P.dma(POOL, lambda e, k=k, hh=hh: e.dma_start(
                            out=w_up[:, k, hh * 2048:(hh + 1) * 2048],
                            in_=w_up_d[l, k * 128:(k + 1) * 128, hh * 2048:(hh + 1) * 2048]), w=[R_wup[k]])
                wdv = w_dn_d[l].rearrange("(c p) n -> p c n", p=128)
                for k in range(8):
                    P.dma(POOL, lambda e, k=k: e.dma_start(out=w_dn[:, k * 4:(k + 1) * 4, :], in_=wdv[:, k * 4:(k + 1) * 4, :]),
                          w=[R_wdn[k]])
                upb = [pB[0][:, 0:256], pB[1][:, 0:256], pB[2][:, 0:256]]
                R_upb = R_pB
                dnb = [pR0, pR1, pM0, pM1]
                R_dnb = [Region("dnb%d" % i) for i in range(4)]
                ngrp = ntile // 2

                def prep_norm(G):
                    db = G % 2
                    for i in range(2):
                        gt = G * 2 + i
                        dbk = (G % 3) * 2 + i
                        X = xt[:, dbk, :]
                        RX = R_xt[dbk]
                        sl = i
                        P.dma(SP, lambda e, X=X, gt=gt: e.dma_start(out=X, in_=y_d[gt * 128:(gt + 1) * 128, :]),
                              r=[R_y[gt]], w=[RX])
                        stt_ = stat[:, sl, :]
                        P.op(POOL, lambda e, stt_=stt_: e.memset(stt_, 0.0), w=[R_stat[sl]])
                        P.op(ACT, lambda e, X=X, stt_=stt_, sl=sl: e.activation(out=hb[:, sl, :], in_=X, func=AF.Square,
                                                                                accum_out=stt_[:, 0:1]),
                             r=[RX], w=[R_stat[sl], R_hb[sl]])
                        rstd_from_ssq(stt_, 0, 1, 2, R_stat[sl], NORM_EPS, 1.0 / DM)
                        P.op(DVE, lambda e, X=X, stt_=stt_, sl=sl: e.scalar_tensor_tensor(
                            out=hb[:, sl, :], in0=X, scalar=stt_[:, 2:3], in1=gA[:], op0=ALU.mult, op1=ALU.mult),
                            r=[RX, R_stat[sl], R_gA], w=[R_hb[sl]])

                def prep_tr(G):
                    db = G % 2
                    for i in range(2):
                        sl = i
                        for k in range(8):
                            P.op(PE, lambda e, k=k, sl=sl: e.transpose(out=pT[:, k * 128:(k + 1) * 128],
                                                                       in_=hb[:, sl, k * 128:(k + 1) * 128], identity=ident),
                                 r=[R_hb[sl], R_cB], w=[R_pT[k // 4]])
                        P.op(ACT, lambda e, db=db, i=i: e.copy(out=hT[:, db, :, i * 128:(i + 1) * 128],
                                                               in_=pT[:, :].rearrange("p (k n) -> p k n", k=8)),
                             r=R_pT, w=[R_hT[db][i]])

                def up(G):
                    db = G % 2
                    for fc in range(32):
                        if fc == 4 and G + 1 < ngrp:
                            prep_norm(G + 1)
                        if fc == 24 and G + 1 < ngrp:
                            prep_tr(G + 1)
                        bank = upb[fc % 3]
                        Rb = R_upb[fc % 3]
                        for k in range(8):
                            P.op(PE, lambda e, k=k, fc=fc, bank=bank, db=db: e.matmul(
                                bank, lhsT=w_up[:, k, fc * 128:(fc + 1) * 128], rhs=hT[:, db, k, :],
                                start=(k == 0), stop=(k == 7)), r=[R_wup[k], R_hT[db][0], R_hT[db][1]], w=[Rb])
                        P.op(ACT, lambda e, fc=fc, bank=bank: e.activation(out=rl[:, fc % 3, :], in_=bank, func=AF.Relu),
                             r=[Rb], w=[R_rl[fc % 3]])
                        P.op(POOL, lambda e, fc=fc: e.tensor_tensor(out=aT[:, fc, :], in0=rl[:, fc % 3, :], in1=rl[:, fc % 3, :],
                                                                    op=ALU.mult), r=[R_rl[fc % 3]], w=[R_aT[fc]])

                def down(G):
                    for fc in range(32):
                        for i in range(2):
                            for hf in range(2):
                                bi = i * 2 + hf
                                P.op(PE, lambda e, fc=fc, i=i, hf=hf, bi=bi: e.matmul(
                                    dnb[bi][:, :], lhsT=aT[:, fc, i * 128:(i + 1) * 128], rhs=w_dn[:, fc, hf * 512:(hf + 1) * 512],
                                    start=(fc == 0), stop=(fc == 31)), r=[R_aT[fc], R_wdn[fc // 4]], w=[R_dnb[bi]])

                def post(G):
                    db = G % 2
                    for i in range(2):
                        gt = G * 2 + i
                        dbk = (G % 3) * 2 + i
                        X = xt[:, dbk, :]
                        RX = R_xt[dbk]
                        s2 = stat2[:, i, :]
                        R2 = R_stat2[i]
                        P.op(POOL, lambda e, s2=s2: e.memset(s2, 0.0), w=[R2])
                        for hf in range(2):
                            bi = i * 2 + hf
                            P.op(ACT, lambda e, hf=hf, bi=bi, s2=s2: e.activation(
                                out=ytmp[:, hf * 512:(hf + 1) * 512], in_=dnb[bi][:, :], func=AF.Square,
                                accum_out=s2[:, hf:hf + 1]), r=[R_dnb[bi]], w=[R2, R_ytmp])
                        P.op(DVE, lambda e, s2=s2: e.tensor_tensor(out=s2[:, 2:3], in0=s2[:, 0:1], in1=s2[:, 1:2], op=ALU.add),
                             r=[R2], w=[R2])
                        rstd_from_ssq(s2, 2, 3, 4, R2, NORM_EPS, 1.0 / DM)
                        for hf in range(2):
                            bi = i * 2 + hf
                            P.op(DVE, lambda e, hf=hf, bi=bi, s2=s2: e.scalar_tensor_tensor(
                                out=ytmp[:, hf * 512:(hf + 1) * 512], in0=dnb[bi][:, :], scalar=s2[:, 4:5],
                                in1=gB[:, hf * 512:(hf + 1) * 512], op0=ALU.mult, op1=ALU.mult),
                                r=[R_dnb[bi], R2, R_gB], w=[R_ytmp])
                        P.op(POOL, lambda e, X=X: e.tensor_tensor(out=X, in0=X, in1=ytmp[:, :], op=ALU.add),
                             r=[RX, R_ytmp], w=[RX])
                        P.dma(SP, lambda e, X=X, gt=gt: e.dma_start(out=y_d[gt * 128:(gt + 1) * 128, :], in_=X),
                              r=[RX], w=[R_y[gt]])

                prep_norm(0)
                prep_tr(0)
                for G in range(ngrp):
                    up(G)
                    down(G)
                    post(G)
                P.barrier()
                P.emit_block()
    return nc


_NC_CACHE = {}


def kernel(x, w_in, w_out, w_up, w_down, g_mix_pre, g_mix_post, g_mlp_pre, g_mlp_post):
    if "nc" not in _NC_CACHE:
        _NC_CACHE["nc"] = build_program()
    nc = _NC_CACHE["nc"]
    cF, cB, _ = host_constants()
    x = np.ascontiguousarray(x, dtype=np.float32)
    B = x.shape[0]
    per = B // NCORES
    common = {
        "w_in": np.ascontiguousarray(w_in, np.float32), "w_out": np.ascontiguousarray(w_out, np.float32),
        "w_up": np.ascontiguousarray(w_up, np.float32), "w_down": np.ascontiguousarray(w_down, np.float32),
        "g_mix_pre": np.ascontiguousarray(g_mix_pre, np.float32), "g_mix_post": np.ascontiguousarray(g_mix_post, np.float32),
        "g_mlp_pre": np.ascontiguousarray(g_mlp_pre, np.float32), "g_mlp_post": np.ascontiguousarray(g_mlp_post, np.float32),
        "cF": cF, "cB": cB,
    }
    in_maps = []
    for c in range(NCORES):
        d = dict(common)
        d["x"] = x[c * per:(c + 1) * per].reshape(per * SEQ, DM)
        in_maps.append(d)
    res = run_bass_kernel_spmd(nc, in_maps, core_ids=list(range(NCORES)))
    out = np.stack([np.asarray(r["y"]).reshape(per, SEQ, DM) for r in res.results], axis=0)
    return out.reshape(B, SEQ, DM).astype(np.float32)
```

```python
import math
from contextlib import ExitStack
import numpy as np
import ml_dtypes
import concourse.bass as bass
import concourse.mybir as mybir
from concourse.bass_utils import run_bass_kernel_spmd

F32 = mybir.dt.float32
BF16 = mybir.dt.bfloat16
ALU = mybir.AluOpType
AF = mybir.ActivationFunctionType
AX = mybir.AxisListType

PE, ACT, DVE, POOL, SP = "pe", "act", "dve", "pool", "sp"
ENGS = [PE, ACT, DVE, POOL, SP]
SAME_ENGINE_SYNC = {PE: False, ACT: True, DVE: True, POOL: True, SP: False}
N_DMA_SEMS = 24


class Region:
    __slots__ = ("name", "last_w", "reads")

    def __init__(self, name):
        self.name = name
        self.last_w = None
        self.reads = []


class Op:
    __slots__ = ("eng", "idx", "fn", "deps", "is_dma", "dma_slot", "dma_val", "signal", "semval")

    def __init__(self, eng, idx, fn, is_dma):
        self.eng = eng
        self.idx = idx
        self.fn = fn
        self.deps = []
        self.is_dma = is_dma
        self.dma_slot = None
        self.dma_val = None
        self.signal = False
        self.semval = None


class Prog:
    def __init__(self, nc):
        self.nc = nc
        self.ops = {e: [] for e in ENGS}
        self.known = {e: {} for e in ENGS}
        self.n_dma = 0
        self.dma_last = [None] * N_DMA_SEMS

    def _add(self, eng, fn, r, w, is_dma):
        op = Op(eng, self._next_idx(eng), fn, is_dma)
        deps = []
        for reg in r:
            if reg.last_w is not None:
                deps.append(reg.last_w)
        for reg in w:
            if reg.last_w is not None:
                deps.append(reg.last_w)
            deps.extend(reg.reads)
        if is_dma:
            slot = self.n_dma % N_DMA_SEMS
            op.dma_slot = slot
            op.dma_val = 16 * (self.n_dma // N_DMA_SEMS + 1)
            if self.dma_last[slot] is not None:
                deps.append(self.dma_last[slot])
            self.dma_last[slot] = op
            self.n_dma += 1
            op.signal = True
        kn = self.known[eng]
        for d in deps:
            if d.is_dma:
                key = ("dma", d.dma_slot)
                val = d.dma_val
            else:
                if d.eng == eng and not SAME_ENGINE_SYNC[eng]:
                    continue
                key = d.eng
                val = d.idx
            if kn.get(key, -1) >= val:
                continue
            kn[key] = val
            d.signal = True
            op.deps.append(d)
        for reg in r:
            reg.reads.append(op)
        for reg in w:
            reg.last_w = op
            reg.reads = []
        self.ops[eng].append(op)
        return op

    def _next_idx(self, eng):
        return len(self.ops[eng])

    def op(self, eng, fn, r=(), w=()):
        return self._add(eng, fn, r, w, False)

    def dma(self, eng, fn, r=(), w=()):
        return self._add(eng, fn, r, w, True)

    def emit(self, final_regions=()):
        nc = self.nc
        self._add(SP, None, list(final_regions), [], False)
        with ExitStack() as st:
            sems = {e: st.enter_context(nc.semaphore("sem_" + e)) for e in ENGS}
            dsems = [st.enter_context(nc.semaphore("sem_dma%d" % i)) for i in range(N_DMA_SEMS)]
            for e in ENGS:
                c = 0
                for op in self.ops[e]:
                    if op.is_dma:
                        continue
                    if op.signal:
                        c += 1
                        op.semval = c
            block = st.enter_context(nc.Block())

            def run(e):
                def body(eng):
                    for op in self.ops[e]:
                        for d in op.deps:
                            if d.is_dma:
                                eng.wait_ge(dsems[d.dma_slot], d.dma_val)
                            else:
                                eng.wait_ge(sems[d.eng], d.semval)
                        if op.fn is None:
                            continue
                        ins = op.fn(eng)
                        if op.is_dma:
                            ins.then_inc(dsems[op.dma_slot], 16)
                        elif op.signal:
                            ins.then_inc(sems[e], 1)
                return body

            block.tensor(run(PE))
            block.scalar(run(ACT))
            block.vector(run(DVE))
            block.gpsimd(run(POOL))
            block.sync(run(SP))


class Prog2(Prog):
    def __init__(self, nc, stack):
        super().__init__(nc)
        self.nidx = {e: 0 for e in ENGS}
        self.semcnt = {e: 0 for e in ENGS}
        self.last_compute = {e: None for e in ENGS}
        self.sems = {e: stack.enter_context(nc.semaphore("sem_" + e)) for e in ENGS}
        self.dsems = [stack.enter_context(nc.semaphore("sem_dma%d" % i)) for i in range(N_DMA_SEMS)]

    def _next_idx(self, eng):
        i = self.nidx[eng]
        self.nidx[eng] += 1
        return i

    def _add(self, eng, fn, r, w, is_dma):
        op = super()._add(eng, fn, r, w, is_dma)
        if not is_dma and fn is not None:
            self.last_compute[eng] = op
        return op

    def barrier(self):
        lasts = dict(self.last_compute)
        dl = list(self.dma_last)
        for e in ENGS:
            op = Op(e, self.nidx[e], None, False)
            self.nidx[e] += 1
            kn = self.known[e]
            for e2 in ENGS:
                d = lasts[e2]
                if d is None:
                    continue
                if kn.get(e2, -1) >= d.idx:
                    continue
                kn[e2] = d.idx
                d.signal = True
                op.deps.append(d)
            for d in dl:
                if d is None:
                    continue
                key = ("dma", d.dma_slot)
                if kn.get(key, -1) >= d.dma_val:
                    continue
                kn[key] = d.dma_val
                op.deps.append(d)
            self.ops[e].append(op)

    def emit_block(self):
        nc = self.nc
        for e in ENGS:
            for op in self.ops[e]:
                if op.is_dma:
                    continue
                if op.signal and op.semval is None:
                    self.semcnt[e] += 1
                    op.semval = self.semcnt[e]
        sems, dsems = self.sems, self.dsems
        ops = {e: self.ops[e] for e in ENGS}
        self.ops = {e: [] for e in ENGS}
        with nc.Block() as block:
            def run(e):
                def body(eng):
                    for op in ops[e]:
                        for d in op.deps:
                            if d.is_dma:
                                eng.wait_ge(dsems[d.dma_slot], d.dma_val)
                            else:
                                assert d.semval is not None
                                eng.wait_ge(sems[d.eng], d.semval)
                        if op.fn is None:
                            continue
                        ins = op.fn(eng)
                        if op.is_dma:
                            ins.then_inc(dsems[op.dma_slot], 16)
                        elif op.signal:
                            ins.then_inc(sems[e], 1)
                return body

            block.tensor(run(PE))
            block.scalar(run(ACT))
            block.vector(run(DVE))
            block.gpsimd(run(POOL))
            block.sync(run(SP))


NCORES = 8
SEQ = 2048
DM = 1024
NSEQ = 2
TOK = NSEQ * SEQ
NTILE = TOK // 128
DEPTH = 2
NORM_EPS = 1e-6
GN_EPS = 1e-5
BIGM = 32768.0

C_ROT = 0
C_DEC = C_ROT + 16 * 160
C_QDEC = C_DEC + 512
C_KDEC = C_QDEC + 256
C_GB = C_KDEC + 4
C_END = C_GB + 256
B_ID = 0
B_TRI = 128
B_IDB = 256
B_END = 384


def host_constants():
    cF = np.zeros((128, C_END), np.float32)
    p = np.arange(128)
    fr = (np.float32(10000.0) ** (-np.arange(0, 64, 2, dtype=np.float32) / np.float32(64))).astype(np.float32)
    fm = (np.float32(500000.0) ** (-np.arange(0, 16, 2, dtype=np.float32) / np.float32(16))).astype(np.float32)
    rot = np.zeros((128, 16, 160), np.float32)
    for t in range(16):
        pos = (t * 128 + p).astype(np.float32)
        ar = (pos[:, None] * fr[None, :]).astype(np.float32)
        am = (pos[:, None] * fm[None, :]).astype(np.float32)
        cr, sr = np.cos(ar).astype(np.float32), np.sin(ar).astype(np.float32)
        cm, sm = np.cos(am).astype(np.float32), np.sin(am).astype(np.float32)
        rot[:, t, 0:32] = cr
        rot[:, t, 32:64] = cr
        rot[:, t, 64:96] = -sr
        rot[:, t, 96:128] = sr
        rot[:, t, 128:136] = cm
        rot[:, t, 136:144] = cm
        rot[:, t, 144:152] = -sm
        rot[:, t, 152:160] = sm
    cF[:, C_ROT:C_DEC] = rot.reshape(128, -1)
    lg = np.log(1.0 - 2.0 ** (-5.0 - np.arange(4, dtype=np.float64)))
    m = np.arange(128)[:, None].astype(np.float64)
    c = np.arange(128)[None, :].astype(np.float64)
    dec = np.zeros((128, 4, 128), np.float64)
    for h in range(4):
        dec[:, h, :] = np.where(c >= m, np.exp(lg[h] * np.maximum(c - m, 0.0)), 0.0) * 0.125
    cF[:, C_DEC:C_QDEC] = dec.reshape(128, -1)
    qd = np.zeros((128, 2, 128), np.float64)
    for pr in range(2):
        for hh in range(2):
            h = pr * 2 + hh
            qd[hh * 64:(hh + 1) * 64, pr, :] = np.exp(lg[h] * (np.arange(128) + 1.0))[None, :]
    cF[:, C_QDEC:C_KDEC] = qd.reshape(128, -1)
    for h in range(4):
        cF[:, C_KDEC + h] = np.exp(lg[h] * (127.0 - np.arange(128))) * 0.125
    gb = np.zeros((4, 8, 8), np.float32)
    for j in range(4, 8):
        gb[j - 4, :, j:] = -1e30
    cF[:, C_GB:C_END] = gb.reshape(1, -1)
    cd = [float(np.exp(lg[h] * 128.0)) for h in range(4)]
    cB = np.zeros((128, B_END), np.float32)
    cB[:, B_ID:B_ID + 128] = np.eye(128)
    cB[:, B_TRI:B_TRI + 128] = (np.arange(128)[:, None] <= np.arange(128)[None, :]).astype(np.float32)
    cB[:, B_IDB:B_IDB + 128] = np.eye(128) * BIGM
    return cF, cB.astype(ml_dtypes.bfloat16), cd


def bc(ap, shape):
    return ap.broadcast_to(list(shape))


def build_program(n_layers=DEPTH, nseq=NSEQ, do_ffn=True, nblk=8, stages="armd", debug=False):
    nc = bass.Bass("TRN2", target_bir_lowering=False)
    cF_np, cB_np, cd = host_constants()
    tok = nseq * SEQ
    ntile = tok // 128
    x_d = nc.dram_tensor("x", [tok, DM], F32, kind="ExternalInput").ap()
    w_in_d = nc.dram_tensor("w_in", [DEPTH, DM, 3072], F32, kind="ExternalInput").ap()
    w_out_d = nc.dram_tensor("w_out", [DEPTH, DM, DM], F32, kind="ExternalInput").ap()
    w_up_d = nc.dram_tensor("w_up", [DEPTH, DM, 4096], F32, kind="ExternalInput").ap()
    w_dn_d = nc.dram_tensor("w_down", [DEPTH, 4096, DM], F32, kind="ExternalInput").ap()
    g_d = {n: nc.dram_tensor(n, [DEPTH, DM], F32, kind="ExternalInput").ap()
           for n in ["g_mix_pre", "g_mix_post", "g_mlp_pre", "g_mlp_post"]}
    cF_d = nc.dram_tensor("cF", [128, C_END], F32, kind="ExternalInput").ap()
    cB_d = nc.dram_tensor("cB", [128, B_END], BF16, kind="ExternalInput").ap()
    y_d = nc.dram_tensor("y", [tok, DM], F32, kind="ExternalOutput").ap()
    dbg_d = nc.dram_tensor("dbg", [tok, DM], BF16, kind="ExternalOutput").ap() if debug else None

    with ExitStack() as top:
        P = Prog2(nc, top)
        _cnt = [0]

        def sbt(st, n, s, d):
            _cnt[0] += 1
            return st.enter_context(nc.sbuf_tensor("sb_%s_%d" % (n, _cnt[0]), s, d))
        pB = [top.enter_context(nc.psum_tensor("pB%d" % i, [128, 512], F32)) for i in range(3)]
        pT = top.enter_context(nc.psum_tensor("pT", [128, 1024], BF16))
        pR0 = top.enter_context(nc.psum_tensor("pR0", [128, 512], F32))
        pR1 = top.enter_context(nc.psum_tensor("pR1", [128, 512], F32))
        pM0 = top.enter_context(nc.psum_tensor("pM0", [128, 512], F32))
        pM1 = top.enter_context(nc.psum_tensor("pM1", [128, 512], F32))
        R_pB = [Region("pB%d" % i) for i in range(3)]
        _rpt = Region("pT")
        R_pT = [_rpt, _rpt]
        R_R0a = Region("R0")
        R_R0b = R_R0a
        R_R1 = Region("R1")
        R_M0 = [Region("M0"), R_R0a]
        R_O = [Region("M1"), R_R1]
        R_G = R_pB[2]
        pT2 = pM1[:, :].bitcast(BF16)
        R_pT2 = R_O[0]
        cB = sbt(top, "cB", [128, B_END], BF16)
        gA = sbt(top, "gA", [128, DM], F32)
        gB = sbt(top, "gB", [128, DM], F32)
        R_cB, R_gA, R_gB = Region("cB"), Region("gA"), Region("gB")
        ident = cB[:, B_ID:B_ID + 128]
        tri = cB[:, B_TRI:B_TRI + 128]
        identBig = cB[:, B_IDB:B_IDB + 128]
        P.dma(SP, lambda e: e.dma_start(out=cB[:], in_=cB_d[:, :]), w=[R_cB])
        R_y = [Region("y%d" % i) for i in range(ntile)]

        def rstd_from_ssq(stat, col_in, col_tmp, col_out, R_stat, eps, inv_n):
            P.op(ACT, lambda e: e.activation(out=stat[:, col_tmp:col_tmp + 1], in_=stat[:, col_in:col_in + 1],
                                             func=AF.Ln, scale=inv_n, bias=eps), r=[R_stat], w=[R_stat])
            P.op(ACT, lambda e: e.activation(out=stat[:, col_out:col_out + 1], in_=stat[:, col_tmp:col_tmp + 1],
                                             func=AF.Exp, scale=-0.5), r=[R_stat], w=[R_stat])

        for l in range(n_layers):
            src_d = x_d if l == 0 else y_d
            with ExitStack() as ph:
                WB = sbt(ph, "WBm", [128, 32768], BF16)
                w_in = WB[:, 0:24576].rearrange("p (k n) -> p k n", k=8)
                w_out = WB[:, 24576:32768].rearrange("p (k n) -> p k n", k=8)
                R_win = [Region("win%d" % k) for k in range(8)]
                R_wout = [Region("wout%d" % k) for k in range(8)]
                cF = sbt(ph, "cF", [128, C_END], F32)
                R_cF = Region("cF")
                rot = cF[:, C_ROT:C_DEC].rearrange("p (t n) -> p t n", t=16)
                dec = cF[:, C_DEC:C_QDEC].rearrange("p (h n) -> p h n", h=4)
                qdec = cF[:, C_QDEC:C_KDEC].rearrange("p (h n) -> p h n", h=2)
                kdec = cF[:, C_KDEC:C_KDEC + 4]
                gbias = cF[:, C_GB:C_END].rearrange("p (j n) -> p j n", j=4)
                mkT = sbt(ph, "mkT", [128, 4, SEQ], BF16)
                mv = sbt(ph, "mv", [128, 16, 8, 65], BF16)
                kmT = sbt(ph, "kmT", [128, 4, 8], BF16)
                km32 = sbt(ph, "km32", [128, 4], F32)
                MT = sbt(ph, "MT", [128, 256], BF16)
                R_mkT = [Region("mkT%d" % i) for i in range(16)]
                R_mv = [Region("mv%d" % i) for i in range(16)]
                R_kmT, R_km32, R_MT = Region("kmT"), Region("km32"), Region("MT")
                xt = sbt(ph, "xt", [128, 4, DM], F32)
                R_xt = [Region("xt%d" % i) for i in range(4)]
                hb = sbt(ph, "hb", [128, 2, DM], BF16)
                R_hb = [Region("hb0"), Region("hb1")]
                hT = sbt(ph, "hT", [128, 2, DM], BF16)
                R_hT = [Region("hT0"), Region("hT1")]
                stat = sbt(ph, "stat", [128, 2, 8], F32)
                R_stat = [Region("stat0"), Region("stat1")]
                stat2 = sbt(ph, "stat2", [128, 2, 8], F32)
                R_stat2 = [Region("stat2_0"), Region("stat2_1")]
                rqa = sbt(ph, "rqa", [128, 512], F32)
                rqb = sbt(ph, "rqb", [128, 512], F32)
                rqk = sbt(ph, "rqk", [128, 512], BF16)
                rkd = sbt(ph, "rkd", [128, 2, 256], BF16)
                R_rqa, R_rqb, R_rqk = Region("rqa"), Region("rqb"), Region("rqk")
                R_rkd = [Region("rkd0"), Region("rkd1")]
                rv = sbt(ph, "rv", [128, 2, 512], BF16)
                sg = sbt(ph, "sg", [128, 2, 512], BF16)
                R_rv = [Region("rv0"), Region("rv1")]
                R_sg = [Region("sg0"), Region("sg1")]
                sgt = sbt(ph, "sgt", [128, 512], F32)
                R_sgt = Region("sgt")
                gs = sbt(ph, "gs", [128, 512], F32)
                R_gs = Region("gs")
                mqs = sbt(ph, "mqs", [128, 2, 512], BF16)
                R_mqs = [Region("mqs0"), Region("mqs1")]
                mta = sbt(ph, "mta", [128, 2, 128], F32)
                mtb = sbt(ph, "mtb", [128, 2, 128], F32)
                R_mta = [Region("mta0"), Region("mta1")]
                R_mtb = [Region("mtb0"), Region("mtb1")]
                rqT = sbt(ph, "rqT", [128, 2, 256], BF16)
                rqdT = sbt(ph, "rqdT", [128, 2, 256], BF16)
                rkT = sbt(ph, "rkT", [128, 2, 256], BF16)
                mqT = sbt(ph, "mqT", [128, 4, 256], BF16)
                R_rqT = [Region("rqT0"), Region("rqT1")]
                R_rqdT = [Region("rqdT0"), Region("rqdT1")]
                R_rkT = [Region("rkT0"), Region("rkT1")]
                R_mqT = [Region("mqT0"), Region("mqT1")]
                mqTz = sbt(ph, "mqTz", [128, 8, 256], BF16)
                ST = sbt(ph, "ST", [128, 4, 128], BF16)
                R_ST = [Region("ST0"), Region("ST1")]
                state = sbt(ph, "state", [128, 2, 128], F32)
                stateb = sbt(ph, "stateb", [128, 2, 2, 128], BF16)
                R_state = [Region("state0"), Region("state1")]
                R_stateb = [Region("stateb0"), Region("stateb1")]
                bst = sbt(ph, "bst", [128, 4, 6], F32)
                bag = sbt(ph, "bag", [128, 4, 2], F32)
                rs4 = sbt(ph, "rs4", [128, 8], F32)
                R_bst, R_bag, R_rs4 = Region("bst"), Region("bag"), Region("rs4")
                on = sbt(ph, "on", [128, 512], F32)
                R_on = Region("on")
                mix = sbt(ph, "mix", [128, 2, DM], BF16)
                R_mixr = [Region("mixr0"), Region("mixr1")]
                R_mixm = [Region("mixm0"), Region("mixm1")]
                mixT = sbt(ph, "mixT", [128, DM], BF16)
                R_mixT = Region("mixT")
                PT = sbt(ph, "PT", [128, 3, 256], BF16)
                R_PT = [Region("PT%d" % i) for i in range(3)]
                g2 = sbt(ph, "g2", [128, 2, 64], F32)
                m8 = sbt(ph, "m8", [128, 2, 64], F32)
                Mp = sbt(ph, "Mp", [128, 2, 128], BF16)
                rc = sbt(ph, "rc", [128, 2, 2], F32)
                R_g2 = [Region("g2_0"), Region("g2_1")]
                R_m8 = [Region("m8_0"), Region("m8_1")]
                R_Mp = [Region("Mp0"), Region("Mp1")]
                R_rc = [Region("rc0"), Region("rc1")]
                ytmp = sbt(ph, "ytmp", [128, DM], F32)
                R_ytmp = Region("ytmp")

                P.dma(SP, lambda e: e.dma_start(out=cF[:], in_=cF_d[:, :]), w=[R_cF])
                P.dma(SP, lambda e: e.dma_start(out=gA[:], in_=g_d["g_mix_pre"][l].partition_broadcast(128)), w=[R_gA])
                P.dma(SP, lambda e: e.dma_start(out=gB[:], in_=g_d["g_mix_post"][l].partition_broadcast(128)), w=[R_gB])
                for k in range(8):
                    for hh in range(2):
                        P.dma(POOL, lambda e, k=k, hh=hh: e.dma_start(
                            out=w_in[:, k, hh * 1536:(hh + 1) * 1536],
                            in_=w_in_d[l, k * 128:(k + 1) * 128, hh * 1536:(hh + 1) * 1536]), w=[R_win[k]])
                for k in range(8):
                    P.dma(POOL, lambda e, k=k: e.dma_start(out=w_out[:, k, :], in_=w_out_d[l, k * 128:(k + 1) * 128, :]),
                          w=[R_wout[k]])
                P.op(POOL, lambda e: e.memset(mv[:, :, :, 64:65], 1.0), w=R_mv)
                P.op(POOL, lambda e: e.memset(kmT[:], 0.0), w=[R_kmT])
                P.op(POOL, lambda e: e.memset(mqTz[:], 0.0), w=R_mqT)
                P.op(POOL, lambda e: e.memset(Mp[:], 0.0), w=R_Mp)

                def phase_a_pre(s, b, i):
                    tt = 2 * b + i
                    gt = s * 16 + tt
                    dbk = ((s * 8 + b) % 2) * 2 + i
                    X = xt[:, dbk, :]
                    RX = R_xt[dbk]
                    sl = gt % 2
                    P.dma(SP, lambda e: e.dma_start(out=X, in_=src_d[gt * 128:(gt + 1) * 128, :]),
                          r=([R_y[gt]] if l > 0 else []), w=[RX])
                    stt_ = stat[:, sl, :]
                    P.op(POOL, lambda e: e.memset(stt_, 0.0), w=[R_stat[sl]])
                    P.op(ACT, lambda e: e.activation(out=hb[:, sl, :], in_=X, func=AF.Square, accum_out=stt_[:, 0:1]),
                         r=[RX], w=[R_stat[sl], R_hb[sl]])
                    rstd_from_ssq(stt_, 0, 1, 2, R_stat[sl], NORM_EPS, 1.0 / DM)
                    P.op(DVE, lambda e: e.scalar_tensor_tensor(out=hb[:, sl, :], in0=X, scalar=stt_[:, 2:3], in1=gA[:],
                                                               op0=ALU.mult, op1=ALU.mult),
                         r=[RX, R_stat[sl], R_gA], w=[R_hb[sl]])
                    for k in range(8):
                        P.op(PE, lambda e, k=k: e.transpose(out=pT[:, k * 128:(k + 1) * 128], in_=hb[:, sl, k * 128:(k + 1) * 128],
                                                            identity=ident), r=[R_hb[sl], R_cB], w=[R_pT[k // 4]])
                    P.op(ACT, lambda e: e.copy(out=hT[:, sl, :], in_=pT[:, :]), r=R_pT, w=[R_hT[sl]])

                def phase_a(s, b, i):
                    tt = 2 * b + i
                    gt = s * 16 + tt
                    sl = gt % 2
                    import os
                    KCUT = int(os.environ.get("KCUT", "99"))
                    hTv = hT[:, sl, :].rearrange("p (k n) -> p k n", k=8)
                    rt = rot[:, tt, :]
                    for ci, cb in enumerate([3, 4, 0, 5, 1, 2]):
                        bank = pB[ci % 3]
                        Rb = R_pB[ci % 3]
                        for k in range(8):
                            P.op(PE, lambda e, k=k, cb=cb, bank=bank: e.matmul(
                                bank[:, :], lhsT=hTv[:, k, :], rhs=w_in[:, k, cb * 512:(cb + 1) * 512],
                                start=(k == 0), stop=(k == 7)), r=[R_hT[sl], R_win[k]], w=[Rb])
                        if cb == 0:
                            ps = bank[:, :].rearrange("p (a n) -> p a n", a=8)
                            av = rqa[:, :].rearrange("p (a n) -> p a n", a=8)
                            bv = rqb[:, :].rearrange("p (a n) -> p a n", a=8)
                            P.op(ACT, lambda e, bank=bank: e.copy(out=rqa[:, :], in_=bank[:, :]), r=[Rb], w=[R_rqa])
                            P.op(DVE, lambda e, av=av, bv=bv: e.tensor_tensor(
                                out=bv[:, :, 0:32], in0=av[:, :, 32:64], in1=bc(rt[:, 64:96].unsqueeze(1), [128, 8, 32]),
                                op=ALU.mult), r=[R_rqa, R_cF], w=[R_rqb])
                            P.op(DVE, lambda e, av=av, bv=bv: e.tensor_tensor(
                                out=bv[:, :, 32:64], in0=av[:, :, 0:32], in1=bc(rt[:, 96:128].unsqueeze(1), [128, 8, 32]),
                                op=ALU.mult), r=[R_rqa, R_cF], w=[R_rqb])
                            P.op(DVE, lambda e, av=av: e.tensor_tensor(
                                out=av, in0=av, in1=bc(rt[:, 0:64].unsqueeze(1), [128, 8, 64]), op=ALU.mult),
                                r=[R_rqa, R_cF], w=[R_rqa])
                            P.op(POOL, lambda e: e.tensor_tensor(out=rqk[:, :], in0=rqa[:, :], in1=rqb[:, :], op=ALU.add),
                                 r=[R_rqa, R_rqb], w=[R_rqk])
                            P.op(POOL, lambda e: e.tensor_tensor(
                                out=rkd[:, i, :].rearrange("p (h n) -> p h n", h=4),
                                in0=rqk[:, 256:512].rearrange("p (h n) -> p h n", h=4),
                                in1=bc(kdec.unsqueeze(2), [128, 4, 64]), op=ALU.mult), r=[R_rqk, R_cF], w=[R_rkd[i]])
                        elif cb == 1:
                            P.op(ACT, lambda e, bank=bank: e.copy(out=rv[:, i, :], in_=bank[:, :]), r=[Rb], w=[R_rv[i]])
                        elif cb == 2:
                            P.op(ACT, lambda e, bank=bank: e.activation(out=sgt[:, :], in_=bank[:, :], func=AF.Exp, scale=-1.0),
                                 r=[Rb], w=[R_sgt])
                            P.op(ACT, lambda e, bank=bank: e.copy(out=gs[:, :], in_=bank[:, :]), r=[Rb], w=[R_gs])
                            P.op(POOL, lambda e: e.tensor_scalar_add(out=sgt[:, :], in0=sgt[:, :], scalar1=1.0),
                                 r=[R_sgt], w=[R_sgt])
                            P.op(DVE, lambda e: e.reciprocal(out=sgt[:, :], in_=sgt[:, :]), r=[R_sgt], w=[R_sgt])
                            P.op(DVE, lambda e: e.tensor_tensor(out=sg[:, i, :], in0=gs[:, :], in1=sgt[:, :],
                                                                op=ALU.mult), r=[R_gs, R_sgt], w=[R_sg[i]])
                        elif cb in (3, 4):
                            j = cb - 3
                            ps = bank[:, :].rearrange("p (a n) -> p a n", a=8)
                            ov = mqs[:, j, :].rearrange("p (a n) -> p a n", a=8)
                            av = mta[:, j, :].rearrange("p (a n) -> p a n", a=8)
                            bv = mtb[:, j, :].rearrange("p (a n) -> p a n", a=8)
                            P.op(ACT, lambda e, bank=bank, j=j: e.copy(out=mqs[:, j, :], in_=bank[:, :]), r=[Rb], w=[R_mqs[j]])
                            P.op(DVE, lambda e, ov=ov, av=av: e.tensor_tensor(
                                out=av, in0=ov[:, :, 0:16], in1=bc(rt[:, 128:144].unsqueeze(1), [128, 8, 16]), op=ALU.mult),
                                r=[R_mqs[j], R_cF], w=[R_mta[j]])
                            P.op(DVE, lambda e, ov=ov, bv=bv: e.tensor_tensor(
                                out=bv[:, :, 0:8], in0=ov[:, :, 8:16], in1=bc(rt[:, 144:152].unsqueeze(1), [128, 8, 8]),
                                op=ALU.mult), r=[R_mqs[j], R_cF], w=[R_mtb[j]])
                            P.op(DVE, lambda e, ov=ov, bv=bv: e.tensor_tensor(
                                out=bv[:, :, 8:16], in0=ov[:, :, 0:8], in1=bc(rt[:, 152:160].unsqueeze(1), [128, 8, 8]),
                                op=ALU.mult), r=[R_mqs[j], R_cF], w=[R_mtb[j]])
                            P.op(POOL, lambda e, ov=ov, av=av, bv=bv: e.tensor_tensor(
                                out=ov[:, :, 0:16], in0=av, in1=bv, op=ALU.add),
                                r=[R_mta[j], R_mtb[j], R_mqs[j]], w=[R_mqs[j]])
                        else:
                            P.op(ACT, lambda e, bank=bank: e.copy(
                                out=mv[:, tt, :, 0:64], in_=bank[:, :].rearrange("p (a n) -> p a n", a=8)),
                                r=[Rb], w=[R_mv[tt]])
                    if KCUT <= 9:
                        return
                    cs = slice(i * 128, (i + 1) * 128)
                    KSUB = int(os.environ.get("KSUB", "0"))
                    for c4 in range(4):
                        if KSUB == 2:
                            break
                        P.op(PE, lambda e, c4=c4: e.transpose(out=pT2[:, c4 * 128:(c4 + 1) * 128],
                                                              in_=rqk[:, c4 * 128:(c4 + 1) * 128], identity=ident),
                             r=[R_rqk, R_cB], w=[R_pT2])
                    if KSUB == 1:
                        return
                    for c4 in range(4):
                        P.op(PE, lambda e, c4=c4: e.transpose(out=pT2[:, 512 + c4 * 128:512 + (c4 + 1) * 128],
                                                              in_=mqs[:, 0, c4 * 128:(c4 + 1) * 128], identity=ident),
                             r=[R_mqs[0], R_cB], w=[R_pT2])
                    if KSUB == 3:
                        return
                    P.op(DVE, lambda e: e.tensor_copy(out=rqT[:, :, cs], in_=pT2[:, 0:256].rearrange("p (a n) -> p a n", a=2)),
                         r=[R_pT2], w=[R_rqT[i]])
                    P.op(DVE, lambda e: e.tensor_copy(out=rkT[:, :, cs], in_=pT2[:, 256:512].rearrange("p (a n) -> p a n", a=2)),
                         r=[R_pT2], w=[R_rkT[i]])
                    if KSUB == 4:
                        return
                    P.op(ACT, lambda e: e.copy(out=mqT[:, :, cs], in_=pT2[:, 512:1024].rearrange("p (a n) -> p a n", a=4)),
                         r=[R_pT2], w=[R_mqT[i]])
                    mzv = mqTz[:, :, :].rearrange("p (a c) n -> p a c n", c=2)
                    for hh in range(2):
                        hf = slice(hh * 64, (hh + 1) * 64)
                        P.op(DVE if hh == 0 else ACT, lambda e, hh=hh, hf=hf: (e.tensor_copy if hh == 0 else e.copy)(
                            out=mzv[hf, :, hh, cs], in_=pT2[hf, 512:1024].rearrange("p (a n) -> p a n", a=4)),
                            r=[R_pT2], w=[R_mqT[i]])
                    if KCUT <= 10:
                        return
                    for c4 in range(4):
                        P.op(PE, lambda e, c4=c4: e.transpose(out=pT[:, c4 * 128:(c4 + 1) * 128],
                                                              in_=mqs[:, 1, c4 * 128:(c4 + 1) * 128], identity=ident),
                             r=[R_mqs[1], R_cB], w=[R_pT[0]])
                    P.op(ACT, lambda e: e.copy(out=mkT[:, :, tt * 128:(tt + 1) * 128],
                                               in_=pT[:, 0:512].rearrange("p (a n) -> p a n", a=4)),
                         r=[R_pT[0]], w=[R_mkT[tt]])
                    if KCUT <= 11:
                        return
                    P.op(POOL, lambda e: e.tensor_tensor(out=rqdT[:, :, cs], in0=rqT[:, :, cs], in1=qdec, op=ALU.mult),
                         r=[R_rqT[i], R_cF], w=[R_rqdT[i]])

                def gate(s, b, i):
                    if b < 4:
                        return
                    cs = slice(i * 128, (i + 1) * 128)
                    gbk = [pR0, pM0]
                    Rgb = [R_R0a, R_M0[0]]
                    for h in range(8):
                        pr, hh = divmod(h, 2)
                        hf = slice(hh * 64, (hh + 1) * 64)
                        P.op(PE, lambda e, h=h, pr=pr, hh=hh, hf=hf: e.matmul(
                            gbk[hh][:, pr * 8:(pr + 1) * 8], lhsT=mqTz[:, h, cs], rhs=kmT[:, pr, :],
                            start=True, stop=True), r=[R_mqT[i], R_kmT], w=[Rgb[hh]])
                    g2v = g2[:, i, :].rearrange("p (a c n) -> p a c n", a=4, c=2)
                    gbv = gbias[:, b - 4, :].rearrange("p (a c n) -> p a c n", a=4, c=2)
                    for hh in range(2):
                        P.op(DVE, lambda e, hh=hh: e.tensor_tensor(
                            out=g2v[:, :, hh, :], in0=gbk[hh][:, 0:32].rearrange("p (a n) -> p a n", a=4),
                            in1=gbv[:, :, hh, :], op=ALU.add), r=[Rgb[hh], R_cF], w=[R_g2[i]])
                    for h in range(8):
                        P.op(DVE, lambda e, h=h: e.max(out=m8[:, i, h * 8:(h + 1) * 8], in_=g2[:, i, h * 8:(h + 1) * 8]),
                             r=[R_g2[i]], w=[R_m8[i]])
                    for h in range(8):
                        P.op(DVE, lambda e, h=h: e.tensor_scalar(
                            out=Mp[:, i, (h % 2) * 64 + (h // 2) * 8:(h % 2) * 64 + (h // 2) * 8 + 8],
                            in0=g2[:, i, h * 8:(h + 1) * 8],
                            scalar1=m8[:, i, h * 8 + 2:h * 8 + 3], scalar2=1.0, op0=ALU.is_ge, op1=ALU.subtract),
                            r=[R_g2[i], R_m8[i]], w=[R_Mp[i]])

                def retention(s, b, i):
                    import os
                    RC = int(os.environ.get("RCUT", "99"))
                    cs = slice(i * 128, (i + 1) * 128)
                    sbk = [[pR0, pM0], [pB[0], pB[1]]]
                    Rsb = [[R_R0a, R_M0[0]], [R_pB[0], R_pB[1]]]
                    kvb = [pR0[:, 256:512], pB[2][:, 0:256]]
                    Rkv = [R_R0b, R_pB[2]]
                    for pr in range(2):
                        for hh in range(2):
                            hf = slice(hh * 64, (hh + 1) * 64)
                            P.op(PE, lambda e, hh=hh, hf=hf, pr=pr: e.matmul(
                                sbk[pr][hh][:, 0:128], lhsT=rkT[hf, pr, cs], rhs=rqT[hf, pr, cs],
                                start=True, stop=True), r=[R_rkT[i], R_rqT[i]], w=[Rsb[pr][hh]])
                    for pr in range(2):
                        for hh in range(2):
                            P.op(DVE, lambda e, pr=pr, hh=hh: e.tensor_tensor(
                                out=ST[:, pr * 2 + hh, :], in0=sbk[pr][hh][:, 0:128],
                                in1=dec[:, pr * 2 + hh, :], op=ALU.mult),
                                r=[Rsb[pr][hh], R_cF], w=[R_ST[pr]])
                    for pr in range(2):
                        for hh in range(2):
                            h = pr * 2 + hh
                            P.op(PE, lambda e, h=h: e.matmul(pR1[:, h * 128:(h + 1) * 128], lhsT=ST[:, h, :],
                                                             rhs=rv[:, i, h * 128:(h + 1) * 128], start=True, stop=False),
                                 r=[R_ST[pr], R_rv[i]], w=[R_R1])
                            P.op(PE, lambda e, h=h, hh=hh, pr=pr: e.matmul(
                                pR1[:, h * 128:(h + 1) * 128], lhsT=rqdT[:, pr, cs], rhs=stateb[:, pr, hh, :],
                                start=False, stop=True), r=[R_rqdT[i], R_stateb[pr]], w=[R_R1])
                    for pr in range(2):
                        P.op(PE, lambda e, pr=pr: e.matmul(kvb[pr], lhsT=rkd[:, i, pr * 128:(pr + 1) * 128],
                                                           rhs=rv[:, i, pr * 256:(pr + 1) * 256], start=True, stop=True),
                             r=[R_rkd[i], R_rv[i]], w=[Rkv[pr]])
                    for pr in range(2):
                        for hh in range(2):
                            h = pr * 2 + hh
                            hf = slice(hh * 64, (hh + 1) * 64)
                            P.op(DVE, lambda e, h=h, hh=hh, hf=hf, pr=pr: e.scalar_tensor_tensor(
                                out=state[hf, pr, :], in0=state[hf, pr, :], scalar=cd[h],
                                in1=kvb[pr][hf, hh * 128:(hh + 1) * 128], op0=ALU.mult, op1=ALU.add),
                                r=[Rkv[pr], R_state[pr]], w=[R_state[pr]])
                        for hh in range(2):
                            hf = slice(hh * 64, (hh + 1) * 64)
                            P.op(ACT, lambda e, pr=pr, hh=hh, hf=hf: e.copy(out=stateb[hf, pr, hh, :], in_=state[hf, pr, :]),
                                 r=[R_state[pr]], w=[R_stateb[pr]])
                    if RC <= 3:
                        return
                    for h in range(4):
                        P.op(DVE, lambda e, h=h: e.bn_stats(out=bst[:, h, :], in_=pR1[:, h * 128:(h + 1) * 128]),
                             r=[R_R1], w=[R_bst])
                    for h in range(4):
                        P.op(DVE, lambda e, h=h: e.bn_aggr(out=bag[:, h, :], in_=bst[:, h, :]), r=[R_bst], w=[R_bag])
                    if RC <= 4:
                        return
                    P.op(ACT, lambda e: e.activation(out=rs4[:, 0:4], in_=bag[:, :, 1], func=AF.Ln, bias=GN_EPS),
                         r=[R_bag], w=[R_rs4])
                    P.op(ACT, lambda e: e.activation(out=rs4[:, 4:8], in_=rs4[:, 0:4], func=AF.Exp, scale=-0.5),
                         r=[R_rs4], w=[R_rs4])
                    if RC <= 5:
                        return
                    for h in range(4):
                        P.op(DVE, lambda e, h=h: e.tensor_scalar(
                            out=on[:, h * 128:(h + 1) * 128], in0=pR1[:, h * 128:(h + 1) * 128],
                            scalar1=bag[:, h, 0:1], scalar2=rs4[:, 4 + h:5 + h], op0=ALU.subtract, op1=ALU.mult),
                            r=[R_R1, R_bag, R_rs4], w=[R_on])
                    P.op(POOL, lambda e: e.tensor_tensor(out=mix[:, i, 0:512], in0=on[:, :], in1=sg[:, i, :], op=ALU.mult),
                         r=[R_on, R_sg[i]], w=[R_mixr[i]])

                def moba(s, b):
                    bs = slice(b * 256, (b + 1) * 256)
                    if b < 7:
                        P.op(DVE, lambda e: e.tensor_reduce(out=km32[:, :], in_=mkT[:, :, bs], axis=AX.X, op=ALU.add),
                             r=[R_mkT[2 * b], R_mkT[2 * b + 1]], w=[R_km32])
                        P.op(ACT, lambda e: e.mul(out=kmT[:, :, b], in_=km32[:, :], mul=1.0 / 256), r=[R_km32], w=[R_kmT])
                    if b >= 4:
                        for i in range(2):
                            cs = slice(i * 128, (i + 1) * 128)
                            P.op(PE, lambda e, i=i: e.transpose(out=pT[:, 0:128], in_=Mp[:, i, :], identity=ident),
                                 r=[R_Mp[i], R_cB], w=[R_pT[0]])
                            P.op(ACT, lambda e, cs=cs: e.copy(out=MT[:, cs], in_=pT[:, 0:128]), r=[R_pT[0]], w=[R_MT])
                    nk = 2 * b + 2
                    units = [(h, kt) for h in range(8) for kt in range(nk)]
                    Ob = [[pM1, pR1], [pB[0], pB[1]]]
                    R_Ob = [[R_O[0], R_R1], [R_pB[0], R_pB[1]]]
                    scb = [pM0, pR0]

                    def scores(u):
                        h, kt = units[u]
                        pr, hh = divmod(h, 2)
                        hf = slice(hh * 64, (hh + 1) * 64)
                        q0 = 128 if kt == 2 * b + 1 else 0
                        slot = u % 2
                        sc = scb[slot][:, q0:256]
                        masked = (b >= 4 and kt < 2 * b)
                        P.op(PE, lambda e: e.matmul(sc, lhsT=mkT[:, pr, kt * 128:(kt + 1) * 128], rhs=mqTz[:, h, q0:256],
                                                    start=True, stop=not masked),
                             r=[R_mkT[kt], R_mqT[0], R_mqT[1]], w=[R_M0[slot]])
                        if masked:
                            rr = hh * 64 + pr * 8 + kt // 2
                            P.op(PE, lambda e: e.matmul(sc, lhsT=bc(identBig[:, rr:rr + 1], [128, 128]),
                                                        rhs=MT[:, q0:256], start=False, stop=True),
                                 r=[R_MT, R_cB], w=[R_M0[slot]])
                        pt = PT[:, u % 3, :]
                        P.op(ACT, lambda e: e.activation(out=pt[:, q0:256], in_=sc, func=AF.Exp, scale=0.125),
                             r=[R_M0[slot]], w=[R_PT[u % 3]])
                        if kt >= 2 * b:
                            P.op(POOL, lambda e: e.tensor_tensor(out=pt[:, q0:q0 + 128], in0=pt[:, q0:q0 + 128], in1=tri,
                                                                 op=ALU.mult), r=[R_PT[u % 3], R_cB], w=[R_PT[u % 3]])

                    def pv(u):
                        h, kt = units[u]
                        q0 = 128 if kt == 2 * b + 1 else 0
                        pt = PT[:, u % 3, :]
                        for qh in range(q0 // 128, 2):
                            last = (2 * b) if qh == 0 else (2 * b + 1)
                            P.op(PE, lambda e, qh=qh, last=last: e.matmul(
                                Ob[h % 2][qh][:, 0:65], lhsT=pt[:, qh * 128:(qh + 1) * 128], rhs=mv[:, kt, h, :],
                                start=(kt == 0), stop=(kt == last)), r=[R_PT[u % 3], R_mv[kt]], w=[R_Ob[h % 2][qh]])
                        if kt == nk - 1:
                            for qh in range(2):
                                P.op(DVE, lambda e, qh=qh: e.reciprocal(out=rc[:, h % 2, qh:qh + 1], in_=Ob[h % 2][qh][:, 64:65]),
                                     r=[R_Ob[h % 2][qh]], w=[R_rc[h % 2]])
                                P.op(DVE, lambda e, qh=qh: e.tensor_scalar(
                                    out=mix[:, qh, 512 + h * 64:512 + (h + 1) * 64], in0=Ob[h % 2][qh][:, 0:64],
                                    scalar1=rc[:, h % 2, qh:qh + 1], scalar2=None, op0=ALU.mult),
                                    r=[R_Ob[h % 2][qh], R_rc[h % 2]], w=[R_mixm[qh]])

                    scores(0)
                    for u in range(len(units)):
                        if u + 1 < len(units):
                            scores(u + 1)
                        pv(u)

                def phase_d(s, b, i):
                    tt = 2 * b + i
                    gt = s * 16 + tt
                    dbk = ((s * 8 + b) % 2) * 2 + i
                    X = xt[:, dbk, :]
                    RX = R_xt[dbk]
                    sl = gt % 2
                    if debug and l == 0:
                        P.dma(SP, lambda e: e.dma_start(out=dbg_d[gt * 128:(gt + 1) * 128, :], in_=mix[:, i, :]),
                              r=[R_mixr[i], R_mixm[i]], w=[Region("dbg%d" % gt)])
                    for k in range(8):
                        P.op(PE, lambda e, k=k: e.transpose(out=pT[:, k * 128:(k + 1) * 128], in_=mix[:, i, k * 128:(k + 1) * 128],
                                                            identity=ident),
                             r=[R_mixr[i], R_mixm[i], R_cB], w=[R_pT[k // 4]])
                    P.op(ACT, lambda e: e.copy(out=mixT[:, :], in_=pT[:, :]), r=R_pT, w=[R_mixT])
                    mixTv = mixT[:, :].rearrange("p (k n) -> p k n", k=8)
                    s2 = stat2[:, sl, :]
                    P.op(POOL, lambda e: e.memset(s2, 0.0), w=[R_stat2[sl]])
                    for hf in range(2):
                        for k in range(8):
                            P.op(PE, lambda e, k=k, hf=hf: e.matmul(pB[hf][:, :], lhsT=mixTv[:, k, :],
                                                                    rhs=w_out[:, k, hf * 512:(hf + 1) * 512],
                                                                    start=(k == 0), stop=(k == 7)),
                                 r=[R_mixT, R_wout[k]], w=[R_pB[hf]])
                        P.op(ACT, lambda e, hf=hf: e.activation(out=ytmp[:, hf * 512:(hf + 1) * 512], in_=pB[hf][:, :],
                                                                func=AF.Square, accum_out=s2[:, hf:hf + 1]),
                             r=[R_pB[hf]], w=[R_stat2[sl], R_ytmp])
                    P.op(DVE, lambda e: e.tensor_tensor(out=s2[:, 2:3], in0=s2[:, 0:1], in1=s2[:, 1:2], op=ALU.add),
                         r=[R_stat2[sl]], w=[R_stat2[sl]])
                    rstd_from_ssq(s2, 2, 3, 4, R_stat2[sl], NORM_EPS, 1.0 / DM)
                    for hf in range(2):
                        P.op(DVE, lambda e, hf=hf: e.scalar_tensor_tensor(
                            out=ytmp[:, hf * 512:(hf + 1) * 512], in0=pB[hf][:, :], scalar=s2[:, 4:5],
                            in1=gB[:, hf * 512:(hf + 1) * 512], op0=ALU.mult, op1=ALU.mult),
                            r=[R_pB[hf], R_stat2[sl], R_gB], w=[R_ytmp])
                    P.op(POOL, lambda e: e.tensor_tensor(out=X, in0=X, in1=ytmp[:, :], op=ALU.add), r=[RX, R_ytmp], w=[RX])
                    P.dma(SP, lambda e: e.dma_start(out=y_d[gt * 128:(gt + 1) * 128, :], in_=X), r=[RX], w=[R_y[gt]])

                for s in range(nseq):
                    P.op(POOL, lambda e: e.memset(state[:], 0.0), w=R_state)
                    P.op(POOL, lambda e: e.memset(stateb[:], 0.0), w=R_stateb)
                    for b in range(nblk):
                        if s == 0 and b == 0:
                            for i in range(2):
                                phase_a_pre(s, b, i)
                        if "a" in stages:
                            for i in range(2):
                                phase_a(s, b, i)
                                if "m" in stages:
                                    gate(s, b, i)
                        if "r" in stages:
                            for i in range(2):
                                retention(s, b, i)
                        nb_ = s * nblk + b + 1
                        if nb_ < nseq * nblk:
                            for i in range(2):
                                phase_a_pre(nb_ // nblk, nb_ % nblk, i)
                        if "m" in stages:
                            moba(s, b)
                        if "d" in stages:
                            for i in range(2):
                                phase_d(s, b, i)
                if debug and l == 0:
                    P.dma(SP, lambda e: e.dma_start(out=dbg_d[0:128, 0:512], in_=stateb[:].rearrange("p a b n -> p (a b n)")),
                          r=R_stateb, w=[Region("dbgs")])
                    P.dma(SP, lambda e: e.dma_start(out=dbg_d[128:256, 0:512], in_=rqdT[:].rearrange("p a n -> p (a n)")),
                          r=R_rqdT, w=[Region("dbgs3")])
                P.barrier()
                P.emit_block()

            if not do_ffn:
                continue
            with ExitStack() as ph:
                WB = sbt(ph, "WBf", [128, 65536], BF16)
                w_up = WB[:, 0:32768].rearrange("p (k n) -> p k n", k=8)
                w_dn = WB[:, 32768:65536].rearrange("p (c n) -> p c n", c=32)
                R_wup = [Region("wup%d" % k) for k in range(8)]
                R_wdn = [Region("wdn%d" % k) for k in range(8)]
                xt = sbt(ph, "xtf", [128, 6, DM], F32)
                R_xt = [Region("xtf%d" % i) for i in range(6)]
                hb = sbt(ph, "hbf", [128, 2, DM], BF16)
                R_hb = [Region("hbf0"), Region("hbf1")]
                hT = sbt(ph, "hTf", [128, 2, 8, 256], BF16)
                R_hT = [[Region("hTf%d_%d" % (d, i)) for i in range(2)] for d in range(2)]
                stat = sbt(ph, "statf", [128, 2, 8], F32)
                R_stat = [Region("statf0"), Region("statf1")]
                stat2 = sbt(ph, "stat2f", [128, 2, 8], F32)
                R_stat2 = [Region("stat2f0"), Region("stat2f1")]
                rl = sbt(ph, "rl", [128, 3, 256], BF16)
                R_rl = [Region("rl%d" % i) for i in range(3)]
                aT = sbt(ph, "aT", [128, 32, 256], BF16)
                R_aT = [Region("aT%d" % i) for i in range(32)]
                ytmp = sbt(ph, "ytmpf", [128, DM], F32)
                R_ytmp = Region("ytmpf")
                P.dma(SP, lambda e: e.dma_start(out=gA[:], in_=g_d["g_mlp_pre"][l].partition_broadcast(128)), w=[R_gA])
                P.dma(SP, lambda e: e.dma_start(out=gB[:], in_=g_d["g_mlp_post"][l].partition_broadcast(128)), w=[R_gB])
                for k in range(8):
                    for hh in range(2):
                        P.dma(POOL, lambda e, k=k, hh=hh: e.dma_start(
                            out=w_up[:, k, hh * 2048:(hh + 1) * 2048],
                            in_=w_up_d[l, k * 128:(k + 1) * 128, hh * 2048:(hh + 1) * 2048]), w=[R_wup[k]])
                wdv = w_dn_d[l].rearrange("(c p) n -> p c n", p=128)
                for k in range(8):
                    P.dma(POOL, lambda e, k=k: e.dma_start(out=w_dn[:, k * 4:(k + 1) * 4, :], in_=wdv[:, k * 4:(k + 1) * 4, :]),
                          w=[R_wdn[k]])
                upb = [pB[0][:, 0:256], pB[1][:, 0:256], pB[2][:, 0:256]]
                R_upb = R_pB
                dnb = [pR0, pR1, pM0, pM1]
                R_dnb = [Region("dnb%d" % i) for i in range(4)]
                ngrp = ntile // 2

                def prep_norm(G):
                    db = G % 2
                    for i in range(2):
                        gt = G * 2 + i
                        dbk = (G % 3) * 2 + i
                        X = xt[:, dbk, :]
                        RX = R_xt[dbk]
                        sl = i
                        P.dma(SP, lambda e, X=X, gt=gt: e.dma_start(out=X, in_=y_d[gt * 128:(gt + 1) * 128, :]),
                              r=[R_y[gt]], w=[RX])
                        stt_ = stat[:, sl, :]
                        P.op(POOL, lambda e, stt_=stt_: e.memset(stt_, 0.0), w=[R_stat[sl]])
                        P.op(ACT, lambda e, X=X, stt_=stt_, sl=sl: e.activation(out=hb[:, sl, :], in_=X, func=AF.Square,
                                                                                accum_out=stt_[:, 0:1]),
                             r=[RX], w=[R_stat[sl], R_hb[sl]])
                        rstd_from_ssq(stt_, 0, 1, 2, R_stat[sl], NORM_EPS, 1.0 / DM)
                        P.op(DVE, lambda e, X=X, stt_=stt_, sl=sl: e.scalar_tensor_tensor(
                            out=hb[:, sl, :], in0=X, scalar=stt_[:, 2:3], in1=gA[:], op0=ALU.mult, op1=ALU.mult),
                            r=[RX, R_stat[sl], R_gA], w=[R_hb[sl]])

                def prep_tr(G):
                    db = G % 2
                    for i in range(2):
                        sl = i
                        for k in range(8):
                            P.op(PE, lambda e, k=k, sl=sl: e.transpose(out=pT[:, k * 128:(k + 1) * 128],
                                                                       in_=hb[:, sl, k * 128:(k + 1) * 128], identity=ident),
                                 r=[R_hb[sl], R_cB], w=[R_pT[k // 4]])
                        P.op(ACT, lambda e, db=db, i=i: e.copy(out=hT[:, db, :, i * 128:(i + 1) * 128],
                                                               in_=pT[:, :].rearrange("p (k n) -> p k n", k=8)),
                             r=R_pT, w=[R_hT[db][i]])

                def up(G):
                    db = G % 2
                    for fc in range(32):
                        if fc == 4 and G + 1 < ngrp:
                            prep_norm(G + 1)
                        if fc == 24 and G + 1 < ngrp:
                            prep_tr(G + 1)
                        bank = upb[fc % 3]
                        Rb = R_upb[fc % 3]
                        for k in range(8):
                            P.op(PE, lambda e, k=k, fc=fc, bank=bank, db=db: e.matmul(
                                bank, lhsT=w_up[:, k, fc * 128:(fc + 1) * 128], rhs=hT[:, db, k, :],
                                start=(k == 0), stop=(k == 7)), r=[R_wup[k], R_hT[db][0], R_hT[db][1]], w=[Rb])
                        P.op(ACT, lambda e, fc=fc, bank=bank: e.activation(out=rl[:, fc % 3, :], in_=bank, func=AF.Relu),
                             r=[Rb], w=[R_rl[fc % 3]])
                        P.op(POOL, lambda e, fc=fc: e.tensor_tensor(out=aT[:, fc, :], in0=rl[:, fc % 3, :], in1=rl[:, fc % 3, :],
                                                                    op=ALU.mult), r=[R_rl[fc % 3]], w=[R_aT[fc]])

                def down(G):
                    for fc in range(32):
                        for i in range(2):
                            for hf in range(2):
                                bi = i * 2 + hf
                                P.op(PE, lambda e, fc=fc, i=i, hf=hf, bi=bi: e.matmul(
                                    dnb[bi][:, :], lhsT=aT[:, fc, i * 128:(i + 1) * 128], rhs=w_dn[:, fc, hf * 512:(hf + 1) * 512],
                                    start=(fc == 0), stop=(fc == 31)), r=[R_aT[fc], R_wdn[fc // 4]], w=[R_dnb[bi]])

                def post(G):
                    db = G % 2
                    for i in range(2):
                        gt = G * 2 + i
                        dbk = (G % 3) * 2 + i
                        X = xt[:, dbk, :]
                        RX = R_xt[dbk]
                        s2 = stat2[:, i, :]
                        R2 = R_stat2[i]
                        P.op(POOL, lambda e, s2=s2: e.memset(s2, 0.0), w=[R2])
                        for hf in range(2):
                            bi = i * 2 + hf
                            P.op(ACT, lambda e, hf=hf, bi=bi, s2=s2: e.activation(
                                out=ytmp[:, hf * 512:(hf + 1) * 512], in_=dnb[bi][:, :], func=AF.Square,
                                accum_out=s2[:, hf:hf + 1]), r=[R_dnb[bi]], w=[R2, R_ytmp])
                        P.op(DVE, lambda e, s2=s2: e.tensor_tensor(out=s2[:, 2:3], in0=s2[:, 0:1], in1=s2[:, 1:2], op=ALU.add),
                             r=[R2], w=[R2])
                        rstd_from_ssq(s2, 2, 3, 4, R2, NORM_EPS, 1.0 / DM)
                        for hf in range(2):
                            bi = i * 2 + hf
                            P.op(DVE, lambda e, hf=hf, bi=bi, s2=s2: e.scalar_tensor_tensor(
                                out=ytmp[:, hf * 512:(hf + 1) * 512], in0=dnb[bi][:, :], scalar=s2[:, 4:5],
                                in1=gB[:, hf * 512:(hf + 1) * 512], op0=ALU.mult, op1=ALU.mult),
                                r=[R_dnb[bi], R2, R_gB], w=[R_ytmp])
                        P.op(POOL, lambda e, X=X: e.tensor_tensor(out=X, in0=X, in1=ytmp[:, :], op=ALU.add),
                             r=[RX, R_ytmp], w=[RX])
                        P.dma(SP, lambda e, X=X, gt=gt: e.dma_start(out=y_d[gt * 128:(gt + 1) * 128, :], in_=X),
                              r=[RX], w=[R_y[gt]])

                prep_norm(0)
                prep_tr(0)
                for G in range(ngrp):
                    up(G)
                    down(G)
                    post(G)
                P.barrier()
                P.emit_block()
    return nc


_NC_CACHE = {}


def kernel(x, w_in, w_out, w_up, w_down, g_mix_pre, g_mix_post, g_mlp_pre, g_mlp_post):
    if "nc" not in _NC_CACHE:
        _NC_CACHE["nc"] = build_program()
    nc = _NC_CACHE["nc"]
    cF, cB, _ = host_constants()
    x = np.ascontiguousarray(x, dtype=np.float32)
    B = x.shape[0]
    per = B // NCORES
    common = {
        "w_in": np.ascontiguousarray(w_in, np.float32), "w_out": np.ascontiguousarray(w_out, np.float32),
        "w_up": np.ascontiguousarray(w_up, np.float32), "w_down": np.ascontiguousarray(w_down, np.float32),
        "g_mix_pre": np.ascontiguousarray(g_mix_pre, np.float32), "g_mix_post": np.ascontiguousarray(g_mix_post, np.float32),
        "g_mlp_pre": np.ascontiguousarray(g_mlp_pre, np.float32), "g_mlp_post": np.ascontiguousarray(g_mlp_post, np.float32),
        "cF": cF, "cB": cB,
    }
    in_maps = []
    for c in range(NCORES):
        d = dict(common)
        d["x"] = x[c * per:(c + 1) * per].reshape(per * SEQ, DM)
        in_maps.append(d)
    res = run_bass_kernel_spmd(nc, in_maps, core_ids=list(range(NCORES)))
    out = np.stack([np.asarray(r["y"]).reshape(per, SEQ, DM) for r in res.results], axis=0)
    return out.reshape(B, SEQ, DM).astype(np.float32)
```

```python
import math
from contextlib import ExitStack
import numpy as np
import ml_dtypes
import concourse.bass as bass
import concourse.mybir as mybir
from concourse.bass_utils import run_bass_kernel_spmd

F32 = mybir.dt.float32
BF16 = mybir.dt.bfloat16
ALU = mybir.AluOpType
AF = mybir.ActivationFunctionType
AX = mybir.AxisListType

PE, ACT, DVE, POOL, SP = "pe", "act", "dve", "pool", "sp"
ENGS = [PE, ACT, DVE, POOL, SP]
SAME_ENGINE_SYNC = {PE: False, ACT: True, DVE: True, POOL: True, SP: False}
N_DMA_SEMS = 24


class Region:
    __slots__ = ("name", "last_w", "reads")

    def __init__(self, name):
        self.name = name
        self.last_w = None
        self.reads = []


class Op:
    __slots__ = ("eng", "idx", "fn", "deps", "is_dma", "dma_slot", "dma_val", "signal", "semval")

    def __init__(self, eng, idx, fn, is_dma):
        self.eng = eng
        self.idx = idx
        self.fn = fn
        self.deps = []
        self.is_dma = is_dma
        self.dma_slot = None
        self.dma_val = None
        self.signal = False
        self.semval = None


class Prog:
    def __init__(self, nc):
        self.nc = nc
        self.ops = {e: [] for e in ENGS}
        self.known = {e: {} for e in ENGS}
        self.n_dma = 0
        self.dma_last = [None] * N_DMA_SEMS

    def _add(self, eng, fn, r, w, is_dma):
        op = Op(eng, self._next_idx(eng), fn, is_dma)
        deps = []
        for reg in r:
            if reg.last_w is not None:
                deps.append(reg.last_w)
        for reg in w:
            if reg.last_w is not None:
                deps.append(reg.last_w)
            deps.extend(reg.reads)
        if is_dma:
            slot = self.n_dma % N_DMA_SEMS
            op.dma_slot = slot
            op.dma_val = 16 * (self.n_dma // N_DMA_SEMS + 1)
            if self.dma_last[slot] is not None:
                deps.append(self.dma_last[slot])
            self.dma_last[slot] = op
            self.n_dma += 1
            op.signal = True
        kn = self.known[eng]
        for d in deps:
            if d.is_dma:
                key = ("dma", d.dma_slot)
                val = d.dma_val
            else:
                if d.eng == eng and not SAME_ENGINE_SYNC[eng]:
                    continue
                key = d.eng
                val = d.idx
            if kn.get(key, -1) >= val:
                continue
            kn[key] = val
            d.signal = True
            op.deps.append(d)
        for reg in r:
            reg.reads.append(op)
        for reg in w:
            reg.last_w = op
            reg.reads = []
        self.ops[eng].append(op)
        return op

    def _next_idx(self, eng):
        return len(self.ops[eng])

    def op(self, eng, fn, r=(), w=()):
        return self._add(eng, fn, r, w, False)

    def dma(self, eng, fn, r=(), w=()):
        return self._add(eng, fn, r, w, True)

    def emit(self, final_regions=()):
        nc = self.nc
        self._add(SP, None, list(final_regions), [], False)
        with ExitStack() as st:
            sems = {e: st.enter_context(nc.semaphore("sem_" + e)) for e in ENGS}
            dsems = [st.enter_context(nc.semaphore("sem_dma%d" % i)) for i in range(N_DMA_SEMS)]
            for e in ENGS:
                c = 0
                for op in self.ops[e]:
                    if op.is_dma:
                        continue
                    if op.signal:
                        c += 1
                        op.semval = c
            block = st.enter_context(nc.Block())

            def run(e):
                def body(eng):
                    for op in self.ops[e]:
                        for d in op.deps:
                            if d.is_dma:
                                eng.wait_ge(dsems[d.dma_slot], d.dma_val)
                            else:
                                eng.wait_ge(sems[d.eng], d.semval)
                        if op.fn is None:
                            continue
                        ins = op.fn(eng)
                        if op.is_dma:
                            ins.then_inc(dsems[op.dma_slot], 16)
                        elif op.signal:
                            ins.then_inc(sems[e], 1)
                return body

            block.tensor(run(PE))
            block.scalar(run(ACT))
            block.vector(run(DVE))
            block.gpsimd(run(POOL))
            block.sync(run(SP))


class Prog2(Prog):
    def __init__(self, nc, stack):
        super().__init__(nc)
        self.nidx = {e: 0 for e in ENGS}
        self.semcnt = {e: 0 for e in ENGS}
        self.last_compute = {e: None for e in ENGS}
        self.sems = {e: stack.enter_context(nc.semaphore("sem_" + e)) for e in ENGS}
        self.dsems = [stack.enter_context(nc.semaphore("sem_dma%d" % i)) for i in range(N_DMA_SEMS)]

    def _next_idx(self, eng):
        i = self.nidx[eng]
        self.nidx[eng] += 1
        return i

    def _add(self, eng, fn, r, w, is_dma):
        op = super()._add(eng, fn, r, w, is_dma)
        if not is_dma and fn is not None:
            self.last_compute[eng] = op
        return op

    def barrier(self):
        lasts = dict(self.last_compute)
        dl = list(self.dma_last)
        for e in ENGS:
            op = Op(e, self.nidx[e], None, False)
            self.nidx[e] += 1
            kn = self.known[e]
            for e2 in ENGS:
                d = lasts[e2]
                if d is None:
                    continue
                if kn.get(e2, -1) >= d.idx:
                    continue
                kn[e2] = d.idx
                d.signal = True
                op.deps.append(d)
            for d in dl:
                if d is None:
                    continue
                key = ("dma", d.dma_slot)
                if kn.get(key, -1) >= d.dma_val:
                    continue
                kn[key] = d.dma_val
                op.deps.append(d)
            self.ops[e].append(op)

    def emit_block(self):
        nc = self.nc
        for e in ENGS:
            for op in self.ops[e]:
                if op.is_dma:
                    continue
                if op.signal and op.semval is None:
                    self.semcnt[e] += 1
                    op.semval = self.semcnt[e]
        sems, dsems = self.sems, self.dsems
        ops = {e: self.ops[e] for e in ENGS}
        self.ops = {e: [] for e in ENGS}
        with nc.Block() as block:
            def run(e):
                def body(eng):
                    for op in ops[e]:
                        for d in op.deps:
                            if d.is_dma:
                                eng.wait_ge(dsems[d.dma_slot], d.dma_val)
                            else:
                                assert d.semval is not None
                                eng.wait_ge(sems[d.eng], d.semval)
                        if op.fn is None:
                            continue
                        ins = op.fn(eng)
                        if op.is_dma:
                            ins.then_inc(dsems[op.dma_slot], 16)
                        elif op.signal:
                            ins.then_inc(sems[e], 1)
                return body

            block.tensor(run(PE))
            block.scalar(run(ACT))
            block.vector(run(DVE))
            block.gpsimd(run(POOL))
            block.sync(run(SP))


NCORES = 8
SEQ = 2048
DM = 1024
NSEQ = 2
TOK = NSEQ * SEQ
NTILE = TOK // 128
DEPTH = 2
NORM_EPS = 1e-6
GN_EPS = 1e-5
BIGM = 32768.0

C_ROT = 0
C_DEC = C_ROT + 16 * 160
C_QDEC = C_DEC + 512
C_KDEC = C_QDEC + 256
C_GB = C_KDEC + 4
C_END = C_GB + 256
B_ID = 0
B_TRI = 128
B_IDB = 256
B_END = 384


def host_constants():
    cF = np.zeros((128, C_END), np.float32)
    p = np.arange(128)
    fr = (np.float32(10000.0) ** (-np.arange(0, 64, 2, dtype=np.float32) / np.float32(64))).astype(np.float32)
    fm = (np.float32(500000.0) ** (-np.arange(0, 16, 2, dtype=np.float32) / np.float32(16))).astype(np.float32)
    rot = np.zeros((128, 16, 160), np.float32)
    for t in range(16):
        pos = (t * 128 + p).astype(np.float32)
        ar = (pos[:, None] * fr[None, :]).astype(np.float32)
        am = (pos[:, None] * fm[None, :]).astype(np.float32)
        cr, sr = np.cos(ar).astype(np.float32), np.sin(ar).astype(np.float32)
        cm, sm = np.cos(am).astype(np.float32), np.sin(am).astype(np.float32)
        rot[:, t, 0:32] = cr
        rot[:, t, 32:64] = cr
        rot[:, t, 64:96] = -sr
        rot[:, t, 96:128] = sr
        rot[:, t, 128:136] = cm
        rot[:, t, 136:144] = cm
        rot[:, t, 144:152] = -sm
        rot[:, t, 152:160] = sm
    cF[:, C_ROT:C_DEC] = rot.reshape(128, -1)
    lg = np.log(1.0 - 2.0 ** (-5.0 - np.arange(4, dtype=np.float64)))
    m = np.arange(128)[:, None].astype(np.float64)
    c = np.arange(128)[None, :].astype(np.float64)
    dec = np.zeros((128, 4, 128), np.float64)
    for h in range(4):
        dec[:, h, :] = np.where(c >= m, np.exp(lg[h] * np.maximum(c - m, 0.0)), 0.0) * 0.125
    cF[:, C_DEC:C_QDEC] = dec.reshape(128, -1)
    qd = np.zeros((128, 2, 128), np.float64)
    for pr in range(2):
        for hh in range(2):
            h = pr * 2 + hh
            qd[hh * 64:(hh + 1) * 64, pr, :] = np.exp(lg[h] * (np.arange(128) + 1.0))[None, :]
    cF[:, C_QDEC:C_KDEC] = qd.reshape(128, -1)
    for h in range(4):
        cF[:, C_KDEC + h] = np.exp(lg[h] * (127.0 - np.arange(128))) * 0.125
    gb = np.zeros((4, 8, 8), np.float32)
    for j in range(4, 8):
        gb[j - 4, :, j:] = -1e30
    cF[:, C_GB:C_END] = gb.reshape(1, -1)
    cd = [float(np.exp(lg[h] * 128.0)) for h in range(4)]
    cB = np.zeros((128, B_END), np.float32)
    cB[:, B_ID:B_ID + 128] = np.eye(128)
    cB[:, B_TRI:B_TRI + 128] = (np.arange(128)[:, None] <= np.arange(128)[None, :]).astype(np.float32)
    cB[:, B_IDB:B_IDB + 128] = np.eye(128) * BIGM
    return cF, cB.astype(ml_dtypes.bfloat16), cd


def bc(ap, shape):
    return ap.broadcast_to(list(shape))


def build_program(n_layers=DEPTH, nseq=NSEQ, do_ffn=True, nblk=8, stages="armd", debug=False):
    nc = bass.Bass("TRN2", target_bir_lowering=False)
    cF_np, cB_np, cd = host_constants()
    tok = nseq * SEQ
    ntile = tok // 128
    x_d = nc.dram_tensor("x", [tok, DM], F32, kind="ExternalInput").ap()
    w_in_d = nc.dram_tensor("w_in", [DEPTH, DM, 3072], F32, kind="ExternalInput").ap()
    w_out_d = nc.dram_tensor("w_out", [DEPTH, DM, DM], F32, kind="ExternalInput").ap()
    w_up_d = nc.dram_tensor("w_up", [DEPTH, DM, 4096], F32, kind="ExternalInput").ap()
    w_dn_d = nc.dram_tensor("w_down", [DEPTH, 4096, DM], F32, kind="ExternalInput").ap()
    g_d = {n: nc.dram_tensor(n, [DEPTH, DM], F32, kind="ExternalInput").ap()
           for n in ["g_mix_pre", "g_mix_post", "g_mlp_pre", "g_mlp_post"]}
    cF_d = nc.dram_tensor("cF", [128, C_END], F32, kind="ExternalInput").ap()
    cB_d = nc.dram_tensor("cB", [128, B_END], BF16, kind="ExternalInput").ap()
    y_d = nc.dram_tensor("y", [tok, DM], F32, kind="ExternalOutput").ap()
    dbg_d = nc.dram_tensor("dbg", [tok, DM], BF16, kind="ExternalOutput").ap() if debug else None

    with ExitStack() as top:
        P = Prog2(nc, top)
        _cnt = [0]

        def sbt(st, n, s, d):
            _cnt[0] += 1
            return st.enter_context(nc.sbuf_tensor("sb_%s_%d" % (n, _cnt[0]), s, d))
        pB = [top.enter_context(nc.psum_tensor("pB%d" % i, [128, 512], F32)) for i in range(3)]
        pT = top.enter_context(nc.psum_tensor("pT", [128, 1024], BF16))
        pR0 = top.enter_context(nc.psum_tensor("pR0", [128, 512], F32))
        pR1 = top.enter_context(nc.psum_tensor("pR1", [128, 512], F32))
        pM0 = top.enter_context(nc.psum_tensor("pM0", [128, 512], F32))
        pM1 = top.enter_context(nc.psum_tensor("pM1", [128, 512], F32))
        R_pB = [Region("pB%d" % i) for i in range(3)]
        _rpt = Region("pT")
        R_pT = [_rpt, _rpt]
        R_R0a = Region("R0")
        R_R0b = R_R0a
        R_R1 = Region("R1")
        R_M0 = [Region("M0"), R_R0a]
        R_O = [Region("M1"), R_R1]
        R_G = R_pB[2]
        pT2 = pM1[:, :].bitcast(BF16)
        R_pT2 = R_O[0]
        cB = sbt(top, "cB", [128, B_END], BF16)
        gA = sbt(top, "gA", [128, DM], F32)
        gB = sbt(top, "gB", [128, DM], F32)
        R_cB, R_gA, R_gB = Region("cB"), Region("gA"), Region("gB")
        ident = cB[:, B_ID:B_ID + 128]
        tri = cB[:, B_TRI:B_TRI + 128]
        identBig = cB[:, B_IDB:B_IDB + 128]
        P.dma(SP, lambda e: e.dma_start(out=cB[:], in_=cB_d[:, :]), w=[R_cB])
        R_y = [Region("y%d" % i) for i in range(ntile)]

        def rstd_from_ssq(stat, col_in, col_tmp, col_out, R_stat, eps, inv_n):
            P.op(ACT, lambda e: e.activation(out=stat[:, col_tmp:col_tmp + 1], in_=stat[:, col_in:col_in + 1],
                                             func=AF.Ln, scale=inv_n, bias=eps), r=[R_stat], w=[R_stat])
            P.op(ACT, lambda e: e.activation(out=stat[:, col_out:col_out + 1], in_=stat[:, col_tmp:col_tmp + 1],
                                             func=AF.Exp, scale=-0.5), r=[R_stat], w=[R_stat])

        for l in range(n_layers):
            src_d = x_d if l == 0 else y_d
            with ExitStack() as ph:
                WB = sbt(ph, "WBm", [128, 32768], BF16)
                w_in = WB[:, 0:24576].rearrange("p (k n) -> p k n", k=8)
                w_out = WB[:, 24576:32768].rearrange("p (k n) -> p k n", k=8)
                R_win = [Region("win%d" % k) for k in range(6)]
                R_wout = [Region("wout%d" % k) for k in range(8)]
                cF = sbt(ph, "cF", [128, C_END], F32)
                R_cF = Region("cF")
                rot = cF[:, C_ROT:C_DEC].rearrange("p (t n) -> p t n", t=16)
                dec = cF[:, C_DEC:C_QDEC].rearrange("p (h n) -> p h n", h=4)
                qdec = cF[:, C_QDEC:C_KDEC].rearrange("p (h n) -> p h n", h=2)
                kdec = cF[:, C_KDEC:C_KDEC + 4]
                gbias = cF[:, C_GB:C_END].rearrange("p (j n) -> p j n", j=4)
                mkT = sbt(ph, "mkT", [128, 4, SEQ], BF16)
                mv = sbt(ph, "mv", [128, 16, 8, 65], BF16)
                kmT = sbt(ph, "kmT", [128, 4, 8], BF16)
                km32 = sbt(ph, "km32", [128, 4], F32)
                MT = sbt(ph, "MT", [128, 256], BF16)
                R_mkT = [Region("mkT%d" % i) for i in range(16)]
                R_mv = [Region("mv%d" % i) for i in range(16)]
                R_kmT, R_km32, R_MT = Region("kmT"), Region("km32"), Region("MT")
                xt = sbt(ph, "xt", [128, 4, DM], F32)
                R_xt = [Region("xt%d" % i) for i in range(4)]
                hb = sbt(ph, "hb", [128, 2, DM], BF16)
                R_hb = [Region("hb0"), Region("hb1")]
                hT = sbt(ph, "hT", [128, 2, DM], BF16)
                R_hT = [Region("hT0"), Region("hT1")]
                stat = sbt(ph, "stat", [128, 2, 8], F32)
                R_stat = [Region("stat0"), Region("stat1")]
                stat2 = sbt(ph, "stat2", [128, 2, 8], F32)
                R_stat2 = [Region("stat2_0"), Region("stat2_1")]
                rqa = sbt(ph, "rqa", [128, 512], F32)
                rqb = sbt(ph, "rqb", [128, 512], F32)
                rqk = sbt(ph, "rqk", [128, 512], BF16)
                rkd = sbt(ph, "rkd", [128, 2, 256], BF16)
                R_rqa, R_rqb, R_rqk = Region("rqa"), Region("rqb"), Region("rqk")
                R_rkd = [Region("rkd0"), Region("rkd1")]
                rv = sbt(ph, "rv", [128, 2, 512], BF16)
                sg = sbt(ph, "sg", [128, 2, 512], BF16)
                R_rv = [Region("rv0"), Region("rv1")]
                R_sg = [Region("sg0"), Region("sg1")]
                sgt = sbt(ph, "sgt", [128, 512], F32)
                R_sgt = Region("sgt")
                gs = sbt(ph, "gs", [128, 512], F32)
                R_gs = Region("gs")
                mqs = sbt(ph, "mqs", [128, 2, 512], BF16)
                R_mqs = [Region("mqs0"), Region("mqs1")]
                mta = sbt(ph, "mta", [128, 2, 128], F32)
                mtb = sbt(ph, "mtb", [128, 2, 128], F32)
                R_mta = [Region("mta0"), Region("mta1")]
                R_mtb = [Region("mtb0"), Region("mtb1")]
                rqT = sbt(ph, "rqT", [128, 2, 256], BF16)
                rqdT = sbt(ph, "rqdT", [128, 2, 256], BF16)
                rkT = sbt(ph, "rkT", [128, 2, 256], BF16)
                mqT = sbt(ph, "mqT", [128, 4, 256], BF16)
                R_rqT = [Region("rqT0"), Region("rqT1")]
                R_rqdT = [Region("rqdT0"), Region("rqdT1")]
                R_rkT = [Region("rkT0"), Region("rkT1")]
                R_mqT = [Region("mqT0"), Region("mqT1")]
                mqTz = sbt(ph, "mqTz", [128, 8, 256], BF16)
                ST = sbt(ph, "ST", [128, 4, 128], BF16)
                R_ST = [Region("ST0"), Region("ST1")]
                state = sbt(ph, "state", [128, 2, 128], F32)
                stateb = sbt(ph, "stateb", [128, 2, 2, 128], BF16)
                R_state = [Region("state0"), Region("state1")]
                R_stateb = [Region("stateb0"), Region("stateb1")]
                bst = sbt(ph, "bst", [128, 4, 6], F32)
                bag = sbt(ph, "bag", [128, 4, 2], F32)
                rs4 = sbt(ph, "rs4", [128, 8], F32)
                R_bst, R_bag, R_rs4 = Region("bst"), Region("bag"), Region("rs4")
                on = sbt(ph, "on", [128, 512], F32)
                R_on = Region("on")
                mix = sbt(ph, "mix", [128, 2, DM], BF16)
                R_mixr = [Region("mixr0"), Region("mixr1")]
                R_mixm = [Region("mixm0"), Region("mixm1")]
                mixT = sbt(ph, "mixT", [128, DM], BF16)
                R_mixT = Region("mixT")
                PT = sbt(ph, "PT", [128, 3, 256], BF16)
                R_PT = [Region("PT%d" % i) for i in range(3)]
                g2 = sbt(ph, "g2", [128, 2, 64], F32)
                m8 = sbt(ph, "m8", [128, 2, 64], F32)
                Mp = sbt(ph, "Mp", [128, 2, 128], BF16)
                rc = sbt(ph, "rc", [128, 2, 2], F32)
                R_g2 = [Region("g2_0"), Region("g2_1")]
                R_m8 = [Region("m8_0"), Region("m8_1")]
                R_Mp = [Region("Mp0"), Region("Mp1")]
                R_rc = [Region("rc0"), Region("rc1")]
                ytmp = sbt(ph, "ytmp", [128, DM], F32)
                R_ytmp = Region("ytmp")

                P.dma(SP, lambda e: e.dma_start(out=cF[:], in_=cF_d[:, :]), w=[R_cF])
                P.dma(SP, lambda e: e.dma_start(out=gA[:], in_=g_d["g_mix_pre"][l].partition_broadcast(128)), w=[R_gA])
                P.dma(SP, lambda e: e.dma_start(out=gB[:], in_=g_d["g_mix_post"][l].partition_broadcast(128)), w=[R_gB])
                wiv = w_in_d[l].rearrange("(k p) n -> p k n", p=128)
                for cb in [3, 4, 0, 5, 1, 2]:
                    P.dma(POOL, lambda e, cb=cb: e.dma_start(
                        out=w_in[:, :, cb * 512:(cb + 1) * 512], in_=wiv[:, :, cb * 512:(cb + 1) * 512]), w=[R_win[cb]])
                for k in range(8):
                    P.dma(POOL, lambda e, k=k: e.dma_start(out=w_out[:, k, :], in_=w_out_d[l, k * 128:(k + 1) * 128, :]),
                          w=[R_wout[k]])
                P.op(POOL, lambda e: e.memset(mv[:, :, :, 64:65], 1.0), w=R_mv)
                P.op(POOL, lambda e: e.memset(kmT[:], 0.0), w=[R_kmT])
                P.op(POOL, lambda e: e.memset(mqTz[:], 0.0), w=R_mqT)
                P.op(POOL, lambda e: e.memset(Mp[:], 0.0), w=R_Mp)

                def phase_a_pre(s, b, i):
                    tt = 2 * b + i
                    gt = s * 16 + tt
                    dbk = ((s * 8 + b) % 2) * 2 + i
                    X = xt[:, dbk, :]
                    RX = R_xt[dbk]
                    sl = gt % 2
                    P.dma(SP, lambda e: e.dma_start(out=X, in_=src_d[gt * 128:(gt + 1) * 128, :]),
                          r=([R_y[gt]] if l > 0 else []), w=[RX])
                    stt_ = stat[:, sl, :]
                    P.op(POOL, lambda e: e.memset(stt_, 0.0), w=[R_stat[sl]])
                    P.op(ACT, lambda e: e.activation(out=hb[:, sl, :], in_=X, func=AF.Square, accum_out=stt_[:, 0:1]),
                         r=[RX], w=[R_stat[sl], R_hb[sl]])
                    rstd_from_ssq(stt_, 0, 1, 2, R_stat[sl], NORM_EPS, 1.0 / DM)
                    P.op(DVE, lambda e: e.scalar_tensor_tensor(out=hb[:, sl, :], in0=X, scalar=stt_[:, 2:3], in1=gA[:],
                                                               op0=ALU.mult, op1=ALU.mult),
                         r=[RX, R_stat[sl], R_gA], w=[R_hb[sl]])
                    for k in range(8):
                        P.op(PE, lambda e, k=k: e.transpose(out=pT[:, k * 128:(k + 1) * 128], in_=hb[:, sl, k * 128:(k + 1) * 128],
                                                            identity=ident), r=[R_hb[sl], R_cB], w=[R_pT[k // 4]])
                    P.op(ACT, lambda e: e.copy(out=hT[:, sl, :], in_=pT[:, :]), r=R_pT, w=[R_hT[sl]])

                def phase_a(s, b, i):
                    tt = 2 * b + i
                    gt = s * 16 + tt
                    sl = gt % 2
                    import os
                    KCUT = int(os.environ.get("KCUT", "99"))
                    hTv = hT[:, sl, :].rearrange("p (k n) -> p k n", k=8)
                    rt = rot[:, tt, :]
                    for ci, cb in enumerate([3, 4, 0, 5, 1, 2]):
                        bank = pB[ci % 3]
                        Rb = R_pB[ci % 3]
                        for k in range(8):
                            P.op(PE, lambda e, k=k, cb=cb, bank=bank: e.matmul(
                                bank[:, :], lhsT=hTv[:, k, :], rhs=w_in[:, k, cb * 512:(cb + 1) * 512],
                                start=(k == 0), stop=(k == 7)), r=[R_hT[sl], R_win[cb]], w=[Rb])
                        if cb == 0:
                            ps = bank[:, :].rearrange("p (a n) -> p a n", a=8)
                            av = rqa[:, :].rearrange("p (a n) -> p a n", a=8)
                            bv = rqb[:, :].rearrange("p (a n) -> p a n", a=8)
                            P.op(ACT, lambda e, bank=bank: e.copy(out=rqa[:, :], in_=bank[:, :]), r=[Rb], w=[R_rqa])
                            P.op(DVE, lambda e, av=av, bv=bv: e.tensor_tensor(
                                out=bv[:, :, 0:32], in0=av[:, :, 32:64], in1=bc(rt[:, 64:96].unsqueeze(1), [128, 8, 32]),
                                op=ALU.mult), r=[R_rqa, R_cF], w=[R_rqb])
                            P.op(DVE, lambda e, av=av, bv=bv: e.tensor_tensor(
                                out=bv[:, :, 32:64], in0=av[:, :, 0:32], in1=bc(rt[:, 96:128].unsqueeze(1), [128, 8, 32]),
                                op=ALU.mult), r=[R_rqa, R_cF], w=[R_rqb])
                            P.op(DVE, lambda e, av=av: e.tensor_tensor(
                                out=av, in0=av, in1=bc(rt[:, 0:64].unsqueeze(1), [128, 8, 64]), op=ALU.mult),
                                r=[R_rqa, R_cF], w=[R_rqa])
                            P.op(POOL, lambda e: e.tensor_tensor(out=rqk[:, :], in0=rqa[:, :], in1=rqb[:, :], op=ALU.add),
                                 r=[R_rqa, R_rqb], w=[R_rqk])
                            P.op(POOL, lambda e: e.tensor_tensor(
                                out=rkd[:, i, :].rearrange("p (h n) -> p h n", h=4),
                                in0=rqk[:, 256:512].rearrange("p (h n) -> p h n", h=4),
                                in1=bc(kdec.unsqueeze(2), [128, 4, 64]), op=ALU.mult), r=[R_rqk, R_cF], w=[R_rkd[i]])
                        elif cb == 1:
                            P.op(ACT, lambda e, bank=bank: e.copy(out=rv[:, i, :], in_=bank[:, :]), r=[Rb], w=[R_rv[i]])
                        elif cb == 2:
                            P.op(ACT, lambda e, bank=bank: e.activation(out=sgt[:, :], in_=bank[:, :], func=AF.Exp, scale=-1.0),
                                 r=[Rb], w=[R_sgt])
                            P.op(ACT, lambda e, bank=bank: e.copy(out=gs[:, :], in_=bank[:, :]), r=[Rb], w=[R_gs])
                            P.op(POOL, lambda e: e.tensor_scalar_add(out=sgt[:, :], in0=sgt[:, :], scalar1=1.0),
                                 r=[R_sgt], w=[R_sgt])
                            P.op(DVE, lambda e: e.reciprocal(out=sgt[:, :], in_=sgt[:, :]), r=[R_sgt], w=[R_sgt])
                            P.op(DVE, lambda e: e.tensor_tensor(out=sg[:, i, :], in0=gs[:, :], in1=sgt[:, :],
                                                                op=ALU.mult), r=[R_gs, R_sgt], w=[R_sg[i]])
                        elif cb in (3, 4):
                            j = cb - 3
                            ps = bank[:, :].rearrange("p (a n) -> p a n", a=8)
                            ov = mqs[:, j, :].rearrange("p (a n) -> p a n", a=8)
                            av = mta[:, j, :].rearrange("p (a n) -> p a n", a=8)
                            bv = mtb[:, j, :].rearrange("p (a n) -> p a n", a=8)
                            P.op(ACT, lambda e, bank=bank, j=j: e.copy(out=mqs[:, j, :], in_=bank[:, :]), r=[Rb], w=[R_mqs[j]])
                            P.op(DVE, lambda e, ov=ov, av=av: e.tensor_tensor(
                                out=av, in0=ov[:, :, 0:16], in1=bc(rt[:, 128:144].unsqueeze(1), [128, 8, 16]), op=ALU.mult),
                                r=[R_mqs[j], R_cF], w=[R_mta[j]])
                            P.op(DVE, lambda e, ov=ov, bv=bv: e.tensor_tensor(
                                out=bv[:, :, 0:8], in0=ov[:, :, 8:16], in1=bc(rt[:, 144:152].unsqueeze(1), [128, 8, 8]),
                                op=ALU.mult), r=[R_mqs[j], R_cF], w=[R_mtb[j]])
                            P.op(DVE, lambda e, ov=ov, bv=bv: e.tensor_tensor(
                                out=bv[:, :, 8:16], in0=ov[:, :, 0:8], in1=bc(rt[:, 152:160].unsqueeze(1), [128, 8, 8]),
                                op=ALU.mult), r=[R_mqs[j], R_cF], w=[R_mtb[j]])
                            P.op(POOL, lambda e, ov=ov, av=av, bv=bv: e.tensor_tensor(
                                out=ov[:, :, 0:16], in0=av, in1=bv, op=ALU.add),
                                r=[R_mta[j], R_mtb[j], R_mqs[j]], w=[R_mqs[j]])
                        else:
                            P.op(ACT, lambda e, bank=bank: e.copy(
                                out=mv[:, tt, :, 0:64], in_=bank[:, :].rearrange("p (a n) -> p a n", a=8)),
                                r=[Rb], w=[R_mv[tt]])
                    if KCUT <= 9:
                        return
                    cs = slice(i * 128, (i + 1) * 128)
                    KSUB = int(os.environ.get("KSUB", "0"))
                    for c4 in range(4):
                        if KSUB == 2:
                            break
                        P.op(PE, lambda e, c4=c4: e.transpose(out=pT2[:, c4 * 128:(c4 + 1) * 128],
                                                              in_=rqk[:, c4 * 128:(c4 + 1) * 128], identity=ident),
                             r=[R_rqk, R_cB], w=[R_pT2])
                    if KSUB == 1:
                        return
                    for c4 in range(4):
                        P.op(PE, lambda e, c4=c4: e.transpose(out=pT2[:, 512 + c4 * 128:512 + (c4 + 1) * 128],
                                                              in_=mqs[:, 0, c4 * 128:(c4 + 1) * 128], identity=ident),
                             r=[R_mqs[0], R_cB], w=[R_pT2])
                    if KSUB == 3:
                        return
                    P.op(DVE, lambda e: e.tensor_copy(out=rqT[:, :, cs], in_=pT2[:, 0:256].rearrange("p (a n) -> p a n", a=2)),
                         r=[R_pT2], w=[R_rqT[i]])
                    P.op(DVE, lambda e: e.tensor_copy(out=rkT[:, :, cs], in_=pT2[:, 256:512].rearrange("p (a n) -> p a n", a=2)),
                         r=[R_pT2], w=[R_rkT[i]])
                    if KSUB == 4:
                        return
                    P.op(ACT, lambda e: e.copy(out=mqT[:, :, cs], in_=pT2[:, 512:1024].rearrange("p (a n) -> p a n", a=4)),
                         r=[R_pT2], w=[R_mqT[i]])
                    mzv = mqTz[:, :, :].rearrange("p (a c) n -> p a c n", c=2)
                    for hh in range(2):
                        hf = slice(hh * 64, (hh + 1) * 64)
                        P.op(DVE if hh == 0 else ACT, lambda e, hh=hh, hf=hf: (e.tensor_copy if hh == 0 else e.copy)(
                            out=mzv[hf, :, hh, cs], in_=pT2[hf, 512:1024].rearrange("p (a n) -> p a n", a=4)),
                            r=[R_pT2], w=[R_mqT[i]])
                    if KCUT <= 10:
                        return
                    for c4 in range(4):
                        P.op(PE, lambda e, c4=c4: e.transpose(out=pT[:, c4 * 128:(c4 + 1) * 128],
                                                              in_=mqs[:, 1, c4 * 128:(c4 + 1) * 128], identity=ident),
                             r=[R_mqs[1], R_cB], w=[R_pT[0]])
                    P.op(ACT, lambda e: e.copy(out=mkT[:, :, tt * 128:(tt + 1) * 128],
                                               in_=pT[:, 0:512].rearrange("p (a n) -> p a n", a=4)),
                         r=[R_pT[0]], w=[R_mkT[tt]])
                    if KCUT <= 11:
                        return
                    P.op(POOL, lambda e: e.tensor_tensor(out=rqdT[:, :, cs], in0=rqT[:, :, cs], in1=qdec, op=ALU.mult),
                         r=[R_rqT[i], R_cF], w=[R_rqdT[i]])

                def gate(s, b, i):
                    if b < 4:
                        return
                    cs = slice(i * 128, (i + 1) * 128)
                    gbk = [pR0, pM0]
                    Rgb = [R_R0a, R_M0[0]]
                    for h in range(8):
                        pr, hh = divmod(h, 2)
                        hf = slice(hh * 64, (hh + 1) * 64)
                        P.op(PE, lambda e, h=h, pr=pr, hh=hh, hf=hf: e.matmul(
                            gbk[hh][:, pr * 8:(pr + 1) * 8], lhsT=mqTz[:, h, cs], rhs=kmT[:, pr, :],
                            start=True, stop=True), r=[R_mqT[i], R_kmT], w=[Rgb[hh]])
                    g2v = g2[:, i, :].rearrange("p (a c n) -> p a c n", a=4, c=2)
                    gbv = gbias[:, b - 4, :].rearrange("p (a c n) -> p a c n", a=4, c=2)
                    for hh in range(2):
                        P.op(DVE, lambda e, hh=hh: e.tensor_tensor(
                            out=g2v[:, :, hh, :], in0=gbk[hh][:, 0:32].rearrange("p (a n) -> p a n", a=4),
                            in1=gbv[:, :, hh, :], op=ALU.add), r=[Rgb[hh], R_cF], w=[R_g2[i]])
                    for h in range(8):
                        P.op(DVE, lambda e, h=h: e.max(out=m8[:, i, h * 8:(h + 1) * 8], in_=g2[:, i, h * 8:(h + 1) * 8]),
                             r=[R_g2[i]], w=[R_m8[i]])
                    for h in range(8):
                        P.op(DVE, lambda e, h=h: e.tensor_scalar(
                            out=Mp[:, i, (h % 2) * 64 + (h // 2) * 8:(h % 2) * 64 + (h // 2) * 8 + 8],
                            in0=g2[:, i, h * 8:(h + 1) * 8],
                            scalar1=m8[:, i, h * 8 + 2:h * 8 + 3], scalar2=1.0, op0=ALU.is_ge, op1=ALU.subtract),
                            r=[R_g2[i], R_m8[i]], w=[R_Mp[i]])

                def retention(s, b, i):
                    import os
                    RC = int(os.environ.get("RCUT", "99"))
                    cs = slice(i * 128, (i + 1) * 128)
                    sbk = [[pR0, pM0], [pB[0], pB[1]]]
                    Rsb = [[R_R0a, R_M0[0]], [R_pB[0], R_pB[1]]]
                    kvb = [pR0[:, 256:512], pB[2][:, 0:256]]
                    Rkv = [R_R0b, R_pB[2]]
                    for pr in range(2):
                        for hh in range(2):
                            hf = slice(hh * 64, (hh + 1) * 64)
                            P.op(PE, lambda e, hh=hh, hf=hf, pr=pr: e.matmul(
                                sbk[pr][hh][:, 0:128], lhsT=rkT[hf, pr, cs], rhs=rqT[hf, pr, cs],
                                start=True, stop=True), r=[R_rkT[i], R_rqT[i]], w=[Rsb[pr][hh]])
                    for pr in range(2):
                        for hh in range(2):
                            P.op(DVE, lambda e, pr=pr, hh=hh: e.tensor_tensor(
                                out=ST[:, pr * 2 + hh, :], in0=sbk[pr][hh][:, 0:128],
                                in1=dec[:, pr * 2 + hh, :], op=ALU.mult),
                                r=[Rsb[pr][hh], R_cF], w=[R_ST[pr]])
                    for pr in range(2):
                        for hh in range(2):
                            h = pr * 2 + hh
                            P.op(PE, lambda e, h=h: e.matmul(pR1[:, h * 128:(h + 1) * 128], lhsT=ST[:, h, :],
                                                             rhs=rv[:, i, h * 128:(h + 1) * 128], start=True, stop=False),
                                 r=[R_ST[pr], R_rv[i]], w=[R_R1])
                            P.op(PE, lambda e, h=h, hh=hh, pr=pr: e.matmul(
                                pR1[:, h * 128:(h + 1) * 128], lhsT=rqdT[:, pr, cs], rhs=stateb[:, pr, hh, :],
                                start=False, stop=True), r=[R_rqdT[i], R_stateb[pr]], w=[R_R1])
                    for pr in range(2):
                        P.op(PE, lambda e, pr=pr: e.matmul(kvb[pr], lhsT=rkd[:, i, pr * 128:(pr + 1) * 128],
                                                           rhs=rv[:, i, pr * 256:(pr + 1) * 256], start=True, stop=True),
                             r=[R_rkd[i], R_rv[i]], w=[Rkv[pr]])
                    for pr in range(2):
                        for hh in range(2):
                            h = pr * 2 + hh
                            hf = slice(hh * 64, (hh + 1) * 64)
                            P.op(DVE, lambda e, h=h, hh=hh, hf=hf, pr=pr: e.scalar_tensor_tensor(
                                out=state[hf, pr, :], in0=state[hf, pr, :], scalar=cd[h],
                                in1=kvb[pr][hf, hh * 128:(hh + 1) * 128], op0=ALU.mult, op1=ALU.add),
                                r=[Rkv[pr], R_state[pr]], w=[R_state[pr]])
                        for hh in range(2):
                            hf = slice(hh * 64, (hh + 1) * 64)
                            P.op(ACT, lambda e, pr=pr, hh=hh, hf=hf: e.copy(out=stateb[hf, pr, hh, :], in_=state[hf, pr, :]),
                                 r=[R_state[pr]], w=[R_stateb[pr]])
                    if RC <= 3:
                        return
                    for h in range(4):
                        P.op(DVE, lambda e, h=h: e.bn_stats(out=bst[:, h, :], in_=pR1[:, h * 128:(h + 1) * 128]),
                             r=[R_R1], w=[R_bst])
                    for h in range(4):
                        P.op(DVE, lambda e, h=h: e.bn_aggr(out=bag[:, h, :], in_=bst[:, h, :]), r=[R_bst], w=[R_bag])
                    if RC <= 4:
                        return
                    P.op(ACT, lambda e: e.activation(out=rs4[:, 0:4], in_=bag[:, :, 1], func=AF.Ln, bias=GN_EPS),
                         r=[R_bag], w=[R_rs4])
                    P.op(ACT, lambda e: e.activation(out=rs4[:, 4:8], in_=rs4[:, 0:4], func=AF.Exp, scale=-0.5),
                         r=[R_rs4], w=[R_rs4])
                    if RC <= 5:
                        return
                    for h in range(4):
                        P.op(DVE, lambda e, h=h: e.tensor_scalar(
                            out=on[:, h * 128:(h + 1) * 128], in0=pR1[:, h * 128:(h + 1) * 128],
                            scalar1=bag[:, h, 0:1], scalar2=rs4[:, 4 + h:5 + h], op0=ALU.subtract, op1=ALU.mult),
                            r=[R_R1, R_bag, R_rs4], w=[R_on])
                    P.op(POOL, lambda e: e.tensor_tensor(out=mix[:, i, 0:512], in0=on[:, :], in1=sg[:, i, :], op=ALU.mult),
                         r=[R_on, R_sg[i]], w=[R_mixr[i]])

                def moba(s, b):
                    bs = slice(b * 256, (b + 1) * 256)
                    if b < 7:
                        P.op(DVE, lambda e: e.tensor_reduce(out=km32[:, :], in_=mkT[:, :, bs], axis=AX.X, op=ALU.add),
                             r=[R_mkT[2 * b], R_mkT[2 * b + 1]], w=[R_km32])
                        P.op(ACT, lambda e: e.mul(out=kmT[:, :, b], in_=km32[:, :], mul=1.0 / 256), r=[R_km32], w=[R_kmT])
                    if b >= 4:
                        for i in range(2):
                            cs = slice(i * 128, (i + 1) * 128)
                            P.op(PE, lambda e, i=i: e.transpose(out=pT[:, 0:128], in_=Mp[:, i, :], identity=ident),
                                 r=[R_Mp[i], R_cB], w=[R_pT[0]])
                            P.op(ACT, lambda e, cs=cs: e.copy(out=MT[:, cs], in_=pT[:, 0:128]), r=[R_pT[0]], w=[R_MT])
                    nk = 2 * b + 2
                    units = [(h, kt) for h in range(8) for kt in range(nk)]
                    Ob = [[pM1, pR1], [pB[0], pB[1]]]
                    R_Ob = [[R_O[0], R_R1], [R_pB[0], R_pB[1]]]
                    scb = [pM0, pR0]

                    def scores(u):
                        h, kt = units[u]
                        pr, hh = divmod(h, 2)
                        hf = slice(hh * 64, (hh + 1) * 64)
                        q0 = 128 if kt == 2 * b + 1 else 0
                        slot = u % 2
                        sc = scb[slot][:, q0:256]
                        masked = (b >= 4 and kt < 2 * b)
                        P.op(PE, lambda e: e.matmul(sc, lhsT=mkT[:, pr, kt * 128:(kt + 1) * 128], rhs=mqTz[:, h, q0:256],
                                                    start=True, stop=not masked),
                             r=[R_mkT[kt], R_mqT[0], R_mqT[1]], w=[R_M0[slot]])
                        if masked:
                            rr = hh * 64 + pr * 8 + kt // 2
                            P.op(PE, lambda e: e.matmul(sc, lhsT=bc(identBig[:, rr:rr + 1], [128, 128]),
                                                        rhs=MT[:, q0:256], start=False, stop=True),
                                 r=[R_MT, R_cB], w=[R_M0[slot]])
                        pt = PT[:, u % 3, :]
                        P.op(ACT, lambda e: e.activation(out=pt[:, q0:256], in_=sc, func=AF.Exp, scale=0.125),
                             r=[R_M0[slot]], w=[R_PT[u % 3]])
                        if kt >= 2 * b:
                            P.op(POOL, lambda e: e.tensor_tensor(out=pt[:, q0:q0 + 128], in0=pt[:, q0:q0 + 128], in1=tri,
                                                                 op=ALU.mult), r=[R_PT[u % 3], R_cB], w=[R_PT[u % 3]])

                    def pv(u):
                        h, kt = units[u]
                        q0 = 128 if kt == 2 * b + 1 else 0
                        pt = PT[:, u % 3, :]
                        for qh in range(q0 // 128, 2):
                            last = (2 * b) if qh == 0 else (2 * b + 1)
                            P.op(PE, lambda e, qh=qh, last=last: e.matmul(
                                Ob[h % 2][qh][:, 0:65], lhsT=pt[:, qh * 128:(qh + 1) * 128], rhs=mv[:, kt, h, :],
                                start=(kt == 0), stop=(kt == last)), r=[R_PT[u % 3], R_mv[kt]], w=[R_Ob[h % 2][qh]])
                        if kt == nk - 1:
                            for qh in range(2):
                                P.op(DVE, lambda e, qh=qh: e.reciprocal(out=rc[:, h % 2, qh:qh + 1], in_=Ob[h % 2][qh][:, 64:65]),
                                     r=[R_Ob[h % 2][qh]], w=[R_rc[h % 2]])
                                P.op(DVE, lambda e, qh=qh: e.tensor_scalar(
                                    out=mix[:, qh, 512 + h * 64:512 + (h + 1) * 64], in0=Ob[h % 2][qh][:, 0:64],
                                    scalar1=rc[:, h % 2, qh:qh + 1], scalar2=None, op0=ALU.mult),
                                    r=[R_Ob[h % 2][qh], R_rc[h % 2]], w=[R_mixm[qh]])

                    scores(0)
                    for u in range(len(units)):
                        if u + 1 < len(units):
                            scores(u + 1)
                        pv(u)

                def phase_d(s, b, i):
                    tt = 2 * b + i
                    gt = s * 16 + tt
                    dbk = ((s * 8 + b) % 2) * 2 + i
                    X = xt[:, dbk, :]
                    RX = R_xt[dbk]
                    sl = gt % 2
                    if debug and l == 0:
                        P.dma(SP, lambda e: e.dma_start(out=dbg_d[gt * 128:(gt + 1) * 128, :], in_=mix[:, i, :]),
                              r=[R_mixr[i], R_mixm[i]], w=[Region("dbg%d" % gt)])
                    for k in range(8):
                        P.op(PE, lambda e, k=k: e.transpose(out=pT[:, k * 128:(k + 1) * 128], in_=mix[:, i, k * 128:(k + 1) * 128],
                                                            identity=ident),
                             r=[R_mixr[i], R_mixm[i], R_cB], w=[R_pT[k // 4]])
                    P.op(ACT, lambda e: e.copy(out=mixT[:, :], in_=pT[:, :]), r=R_pT, w=[R_mixT])
                    mixTv = mixT[:, :].rearrange("p (k n) -> p k n", k=8)
                    s2 = stat2[:, sl, :]
                    P.op(POOL, lambda e: e.memset(s2, 0.0), w=[R_stat2[sl]])
                    for hf in range(2):
                        for k in range(8):
                            P.op(PE, lambda e, k=k, hf=hf: e.matmul(pB[hf][:, :], lhsT=mixTv[:, k, :],
                                                                    rhs=w_out[:, k, hf * 512:(hf + 1) * 512],
                                                                    start=(k == 0), stop=(k == 7)),
                                 r=[R_mixT, R_wout[k]], w=[R_pB[hf]])
                        P.op(ACT, lambda e, hf=hf: e.activation(out=ytmp[:, hf * 512:(hf + 1) * 512], in_=pB[hf][:, :],
                                                                func=AF.Square, accum_out=s2[:, hf:hf + 1]),
                             r=[R_pB[hf]], w=[R_stat2[sl], R_ytmp])
                    P.op(DVE, lambda e: e.tensor_tensor(out=s2[:, 2:3], in0=s2[:, 0:1], in1=s2[:, 1:2], op=ALU.add),
                         r=[R_stat2[sl]], w=[R_stat2[sl]])
                    rstd_from_ssq(s2, 2, 3, 4, R_stat2[sl], NORM_EPS, 1.0 / DM)
                    for hf in range(2):
                        P.op(DVE, lambda e, hf=hf: e.scalar_tensor_tensor(
                            out=ytmp[:, hf * 512:(hf + 1) * 512], in0=pB[hf][:, :], scalar=s2[:, 4:5],
                            in1=gB[:, hf * 512:(hf + 1) * 512], op0=ALU.mult, op1=ALU.mult),
                            r=[R_pB[hf], R_stat2[sl], R_gB], w=[R_ytmp])
                    P.op(POOL, lambda e: e.tensor_tensor(out=X, in0=X, in1=ytmp[:, :], op=ALU.add), r=[RX, R_ytmp], w=[RX])
                    P.dma(SP, lambda e: e.dma_start(out=y_d[gt * 128:(gt + 1) * 128, :], in_=X), r=[RX], w=[R_y[gt]])

                for s in range(nseq):
                    P.op(POOL, lambda e: e.memset(state[:], 0.0), w=R_state)
                    P.op(POOL, lambda e: e.memset(stateb[:], 0.0), w=R_stateb)
                    for b in range(nblk):
                        if s == 0 and b == 0:
                            for i in range(2):
                                phase_a_pre(s, b, i)
                        if "a" in stages:
                            for i in range(2):
                                phase_a(s, b, i)
                                if "m" in stages:
                                    gate(s, b, i)
                        if "r" in stages:
                            for i in range(2):
                                retention(s, b, i)
                        nb_ = s * nblk + b + 1
                        if nb_ < nseq * nblk:
                            for i in range(2):
                                phase_a_pre(nb_ // nblk, nb_ % nblk, i)
                        if "m" in stages:
                            moba(s, b)
                        if "d" in stages:
                            for i in range(2):
                                phase_d(s, b, i)
                if debug and l == 0:
                    P.dma(SP, lambda e: e.dma_start(out=dbg_d[0:128, 0:512], in_=stateb[:].rearrange("p a b n -> p (a b n)")),
                          r=R_stateb, w=[Region("dbgs")])
                    P.dma(SP, lambda e: e.dma_start(out=dbg_d[128:256, 0:512], in_=rqdT[:].rearrange("p a n -> p (a n)")),
                          r=R_rqdT, w=[Region("dbgs3")])
                P.barrier()
                P.emit_block()

            if not do_ffn:
                continue
            with ExitStack() as ph:
                WB = sbt(ph, "WBf", [128, 65536], BF16)
                w_up = WB[:, 0:32768].rearrange("p (k n) -> p k n", k=8)
                w_dn = WB[:, 32768:65536].rearrange("p (c n) -> p c n", c=32)
                R_wup = [Region("wup%d" % k) for k in range(8)]
                R_wdn = [Region("wdn%d" % k) for k in range(8)]
                xt = sbt(ph, "xtf", [128, 6, DM], F32)
                R_xt = [Region("xtf%d" % i) for i in range(6)]
                hb = sbt(ph, "hbf", [128, 2, DM], BF16)
                R_hb = [Region("hbf0"), Region("hbf1")]
                hT = sbt(ph, "hTf", [128, 2, 8, 256], BF16)
                R_hT = [[Region("hTf%d_%d" % (d, i)) for i in range(2)] for d in range(2)]
                stat = sbt(ph, "statf", [128, 2, 8], F32)
                R_stat = [Region("statf0"), Region("statf1")]
                stat2 = sbt(ph, "stat2f", [128, 2, 8], F32)
                R_stat2 = [Region("stat2f0"), Region("stat2f1")]
                rl = sbt(ph, "rl", [128, 3, 256], BF16)
                R_rl = [Region("rl%d" % i) for i in range(3)]
                aT = sbt(ph, "aT", [128, 32, 256], BF16)
                R_aT = [Region("aT%d" % i) for i in range(32)]
                ytmp = sbt(ph, "ytmpf", [128, DM], F32)
                R_ytmp = Region("ytmpf")
                P.dma(SP, lambda e: e.dma_start(out=gA[:], in_=g_d["g_mlp_pre"][l].partition_broadcast(128)), w=[R_gA])
                P.dma(SP, lambda e: e.dma_start(out=gB[:], in_=g_d["g_mlp_post"][l].partition_broadcast(128)), w=[R_gB])
                wuv = w_up_d[l].rearrange("(k p) n -> p k n", p=128)
                for cbk in range(8):
                    P.dma(POOL, lambda e, cbk=cbk: e.dma_start(
                        out=w_up[:, :, cbk * 512:(cbk + 1) * 512], in_=wuv[:, :, cbk * 512:(cbk + 1) * 512]), w=[R_wup[cbk]])
                wdv = w_dn_d[l].rearrange("(c p) n -> p c n", p=128)
                for k in range(8):
                    P.dma(POOL, lambda e, k=k: e.dma_start(out=w_dn[:, k * 4:(k + 1) * 4, :], in_=wdv[:, k * 4:(k + 1) * 4, :]),
                          w=[R_wdn[k]])
                upb = [pB[0][:, 0:256], pB[1][:, 0:256], pB[2][:, 0:256]]
                R_upb = R_pB
                dnb = [pR0, pR1, pM0, pM1]
                R_dnb = [Region("dnb%d" % i) for i in range(4)]
                ngrp = ntile // 2

                def prep_norm(G):
                    db = G % 2
                    for i in range(2):
                        gt = G * 2 + i
                        dbk = (G % 3) * 2 + i
                        X = xt[:, dbk, :]
                        RX = R_xt[dbk]
                        sl = i
                        P.dma(SP, lambda e, X=X, gt=gt: e.dma_start(out=X, in_=y_d[gt * 128:(gt + 1) * 128, :]),
                              r=[R_y[gt]], w=[RX])
                        stt_ = stat[:, sl, :]
                        P.op(POOL, lambda e, stt_=stt_: e.memset(stt_, 0.0), w=[R_stat[sl]])
                        P.op(ACT, lambda e, X=X, stt_=stt_, sl=sl: e.activation(out=hb[:, sl, :], in_=X, func=AF.Square,
                                                                                accum_out=stt_[:, 0:1]),
                             r=[RX], w=[R_stat[sl], R_hb[sl]])
                        rstd_from_ssq(stt_, 0, 1, 2, R_stat[sl], NORM_EPS, 1.0 / DM)
                        P.op(DVE, lambda e, X=X, stt_=stt_, sl=sl: e.scalar_tensor_tensor(
                            out=hb[:, sl, :], in0=X, scalar=stt_[:, 2:3], in1=gA[:], op0=ALU.mult, op1=ALU.mult),
                            r=[RX, R_stat[sl], R_gA], w=[R_hb[sl]])

                def prep_tr(G):
                    db = G % 2
                    for i in range(2):
                        sl = i
                        for k in range(8):
                            P.op(PE, lambda e, k=k, sl=sl: e.transpose(out=pT[:, k * 128:(k + 1) * 128],
                                                                       in_=hb[:, sl, k * 128:(k + 1) * 128], identity=ident),
                                 r=[R_hb[sl], R_cB], w=[R_pT[k // 4]])
                        P.op(ACT, lambda e, db=db, i=i: e.copy(out=hT[:, db, :, i * 128:(i + 1) * 128],
                                                               in_=pT[:, :].rearrange("p (k n) -> p k n", k=8)),
                             r=R_pT, w=[R_hT[db][i]])

                def up(G):
                    db = G % 2
                    for fc in range(32):
                        if fc == 4 and G + 1 < ngrp:
                            prep_norm(G + 1)
                        if fc == 24 and G + 1 < ngrp:
                            prep_tr(G + 1)
                        bank = upb[fc % 3]
                        Rb = R_upb[fc % 3]
                        for k in range(8):
                            P.op(PE, lambda e, k=k, fc=fc, bank=bank, db=db: e.matmul(
                                bank, lhsT=w_up[:, k, fc * 128:(fc + 1) * 128], rhs=hT[:, db, k, :],
                                start=(k == 0), stop=(k == 7)), r=[R_wup[fc // 4], R_hT[db][0], R_hT[db][1]], w=[Rb])
                        P.op(ACT, lambda e, fc=fc, bank=bank: e.activation(out=rl[:, fc % 3, :], in_=bank, func=AF.Relu),
                             r=[Rb], w=[R_rl[fc % 3]])
                        P.op(POOL, lambda e, fc=fc: e.tensor_tensor(out=aT[:, fc, :], in0=rl[:, fc % 3, :], in1=rl[:, fc % 3, :],
                                                                    op=ALU.mult), r=[R_rl[fc % 3]], w=[R_aT[fc]])

                def down(G):
                    for fc in range(32):
                        for i in range(2):
                            for hf in range(2):
                                bi = i * 2 + hf
                                P.op(PE, lambda e, fc=fc, i=i, hf=hf, bi=bi: e.matmul(
                                    dnb[bi][:, :], lhsT=aT[:, fc, i * 128:(i + 1) * 128], rhs=w_dn[:, fc, hf * 512:(hf + 1) * 512],
                                    start=(fc == 0), stop=(fc == 31)), r=[R_aT[fc], R_wdn[fc // 4]], w=[R_dnb[bi]])

                def post(G):
                    db = G % 2
                    for i in range(2):
                        gt = G * 2 + i
                        dbk = (G % 3) * 2 + i
                        X = xt[:, dbk, :]
                        RX = R_xt[dbk]
                        s2 = stat2[:, i, :]
                        R2 = R_stat2[i]
                        P.op(POOL, lambda e, s2=s2: e.memset(s2, 0.0), w=[R2])
                        for hf in range(2):
                            bi = i * 2 + hf
                            P.op(ACT, lambda e, hf=hf, bi=bi, s2=s2: e.activation(
                                out=ytmp[:, hf * 512:(hf + 1) * 512], in_=dnb[bi][:, :], func=AF.Square,
                                accum_out=s2[:, hf:hf + 1]), r=[R_dnb[bi]], w=[R2, R_ytmp])
                        P.op(DVE, lambda e, s2=s2: e.tensor_tensor(out=s2[:, 2:3], in0=s2[:, 0:1], in1=s2[:, 1:2], op=ALU.add),
                             r=[R2], w=[R2])
                        rstd_from_ssq(s2, 2, 3, 4, R2, NORM_EPS, 1.0 / DM)
                        for hf in range(2):
                            bi = i * 2 + hf
                            P.op(DVE, lambda e, hf=hf, bi=bi, s2=s2: e.scalar_tensor_tensor(
                                out=ytmp[:, hf * 512:(hf + 1) * 512], in0=dnb[bi][:, :], scalar=s2[:, 4:5],
                                in1=gB[:, hf * 512:(hf + 1) * 512], op0=ALU.mult, op1=ALU.mult),
                                r=[R_dnb[bi], R2, R_gB], w=[R_ytmp])
                        P.op(POOL, lambda e, X=X: e.tensor_tensor(out=X, in0=X, in1=ytmp[:, :], op=ALU.add),
                             r=[RX, R_ytmp], w=[RX])
                        P.dma(SP, lambda e, X=X, gt=gt: e.dma_start(out=y_d[gt * 128:(gt + 1) * 128, :], in_=X),
                              r=[RX], w=[R_y[gt]])

                prep_norm(0)
                prep_tr(0)
                for G in range(ngrp):
                    up(G)
                    down(G)
                    post(G)
                P.barrier()
                P.emit_block()
    return nc


_NC_CACHE = {}


def kernel(x, w_in, w_out, w_up, w_down, g_mix_pre, g_mix_post, g_mlp_pre, g_mlp_post):
    if "nc" not in _NC_CACHE:
        _NC_CACHE["nc"] = build_program()
    nc = _NC_CACHE["nc"]
    cF, cB, _ = host_constants()
    x = np.ascontiguousarray(x, dtype=np.float32)
    B = x.shape[0]
    per = B // NCORES
    common = {
        "w_in": np.ascontiguousarray(w_in, np.float32), "w_out": np.ascontiguousarray(w_out, np.float32),
        "w_up": np.ascontiguousarray(w_up, np.float32), "w_down": np.ascontiguousarray(w_down, np.float32),
        "g_mix_pre": np.ascontiguousarray(g_mix_pre, np.float32), "g_mix_post": np.ascontiguousarray(g_mix_post, np.float32),
        "g_mlp_pre": np.ascontiguousarray(g_mlp_pre, np.float32), "g_mlp_post": np.ascontiguousarray(g_mlp_post, np.float32),
        "cF": cF, "cB": cB,
    }
    in_maps = []
    for c in range(NCORES):
        d = dict(common)
        d["x"] = x[c * per:(c + 1) * per].reshape(per * SEQ, DM)
        in_maps.append(d)
    res = run_bass_kernel_spmd(nc, in_maps, core_ids=list(range(NCORES)))
    out = np.stack([np.asarray(r["y"]).reshape(per, SEQ, DM) for r in res.results], axis=0)
    return out.reshape(B, SEQ, DM).astype(np.float32)
```
